# Optimizing a Trainium2 kernel written in Bass

```python
import math
import numpy as np
import jax
import jax.numpy as jnp
from jax import lax

D_MODEL = 1024
BATCH = 4
SEQ = 4096
DEPTH = 4

HEAD_DIM = 64
ROPE_THETA = 10000.0
NORM_EPS = 1e-6
D_FF = 4 * D_MODEL
Q_BLOCK = 128

A_HEADS = D_MODEL // (2 * HEAD_DIM)
IDX_HEADS = 4
DSA_TOPK = 256

B_HEADS = D_MODEL // (2 * HEAD_DIM)
B_KV_GROUPS = 2
CMP_BLOCK = 32
CMP_STRIDE = 16
CMP_HIDDEN = 256
SLC_BLOCK = 64
SLC_TOPN = 16
WINDOW = 512

C_HEADS = D_MODEL // (2 * HEAD_DIM)

EVEN_SPLITS = (A_HEADS * HEAD_DIM, HEAD_DIM, HEAD_DIM, IDX_HEADS * HEAD_DIM, HEAD_DIM, IDX_HEADS, B_HEADS * HEAD_DIM, 6 * B_KV_GROUPS * HEAD_DIM, 3 * B_HEADS)
EVEN_IN = sum(EVEN_SPLITS)
EVEN_MIX = (A_HEADS + B_HEADS) * HEAD_DIM
ODD_MIX = C_HEADS * 2 * HEAD_DIM
ODD_IN = 3 * ODD_MIX

kernel_name = 'hybrid_dsa_nsa_diffattn_trunk'


def rms_norm(x, g):
    xf = x.astype(jnp.float32)
    y = xf * lax.rsqrt(jnp.mean(xf * xf, axis=-1, keepdims=True) + NORM_EPS)
    return (y * g.astype(jnp.float32)).astype(x.dtype)


def rope_tables(T):
    inv = 1.0 / (ROPE_THETA ** (jnp.arange(0, HEAD_DIM, 2, dtype=jnp.float32) / HEAD_DIM))
    ang = jnp.arange(T, dtype=jnp.float32)[:, None] * inv[None, :]
    return jnp.cos(ang), jnp.sin(ang)


def apply_rope(x, cos, sin):
    x1, x2 = jnp.split(x, 2, axis=-1)
    shape = (1, cos.shape[0]) + (1,) * (x.ndim - 3) + (cos.shape[1],)
    c = cos.reshape(shape).astype(x.dtype)
    s = sin.reshape(shape).astype(x.dtype)
    return jnp.concatenate([x1 * c - x2 * s, x2 * c + x1 * s], axis=-1)


def masked_softmax(s, mask):
    s = jnp.where(mask, s.astype(jnp.float32), -jnp.inf)
    m = jnp.max(s, axis=-1, keepdims=True)
    m = jnp.where(jnp.isfinite(m), m, 0.0)
    e = jnp.exp(s - m)
    return e / jnp.maximum(jnp.sum(e, axis=-1, keepdims=True), 1e-30)


def split_cols(a, sizes):
    offs = np.cumsum(sizes)[:-1].tolist()
    return jnp.split(a, offs, axis=-1)


def sweep_query_blocks(fn, T):
    nb = T // Q_BLOCK
    out = lax.map(fn, jnp.arange(nb))
    out = jnp.moveaxis(out, 0, 1)
    return out.reshape((out.shape[0], T) + out.shape[3:])


def dsa_attention(q, k, v, q_idx, k_idx, w_idx):
    T = q.shape[1]
    topk = min(DSA_TOPK, T // 4)
    scale = HEAD_DIM ** -0.5
    key_pos = jnp.arange(T)

    def block(i):
        t0 = i * Q_BLOCK
        tq = t0 + jnp.arange(Q_BLOCK)
        qb = lax.dynamic_slice_in_dim(q, t0, Q_BLOCK, 1)
        qib = lax.dynamic_slice_in_dim(q_idx, t0, Q_BLOCK, 1)
        wb = lax.dynamic_slice_in_dim(w_idx, t0, Q_BLOCK, 1)
        rel = jax.nn.relu(jnp.einsum('bqhd,bsd->bqhs', qib, k_idx).astype(jnp.float32))
        score = jnp.einsum('bqh,bqhs->bqs', wb.astype(jnp.float32), rel)
        score = jnp.where(key_pos[None, :] <= tq[:, None], score, -jnp.inf)
        _, idx = lax.top_k(score, topk)
        valid = idx <= tq[None, :, None]
        kg = jax.vmap(lambda kk, ii: kk[ii])(k, idx)
        vg = jax.vmap(lambda vv, ii: vv[ii])(v, idx)
        s = jnp.einsum('bqhd,bqkd->bhqk', qb, kg) * scale
        p = masked_softmax(s, valid[:, None])
        return jnp.einsum('bhqk,bqkd->bqhd', p.astype(vg.dtype), vg)

    return sweep_query_blocks(block, T)


def compress_blocks(x, pe, w1, w2):
    bsz, T, G, D = x.shape
    n_ch = T // CMP_STRIDE
    r = CMP_BLOCK // CMP_STRIDE
    n_c = n_ch - r + 1
    ch = x.reshape(bsz, n_ch, CMP_STRIDE, G, D)
    blk = jnp.concatenate([ch[:, j:j + n_c] for j in range(r)], axis=2)
    blk = blk + pe[None, None, :, None, :].astype(x.dtype)
    h = jax.nn.gelu(jnp.einsum('bnlgd,ldf->bngf', blk, w1.reshape(CMP_BLOCK, D, CMP_HIDDEN)))
    return jnp.einsum('bngf,fd->bngd', h, w2)


def slc_overlap(n_c, n_s):
    c0 = np.arange(n_c) * CMP_STRIDE
    c1 = c0 + CMP_BLOCK
    s0 = np.arange(n_s) * SLC_BLOCK
    s1 = s0 + SLC_BLOCK
    return ((c0[:, None] < s1[None, :]) & (c1[:, None] > s0[None, :])).astype(np.float32)


def nsa_attention(q, k_c, v_c, k_s, v_s, k_w, v_w, gate_logits, cmp_pe, cmp_w1, cmp_w2):
    bsz, T = q.shape[0], q.shape[1]
    G, J, D = B_KV_GROUPS, B_HEADS // B_KV_GROUPS, HEAD_DIM
    scale = D ** -0.5
    qg = q.reshape(bsz, T, G, J, D)
    g = jax.nn.sigmoid(gate_logits.astype(jnp.float32)).astype(q.dtype).reshape(bsz, T, G, J, 3)
    t_all = jnp.arange(T)

    kc = compress_blocks(k_c, cmp_pe[0], cmp_w1[0], cmp_w2[0])
    vc = compress_blocks(v_c, cmp_pe[1], cmp_w1[1], cmp_w2[1])
    n_c = kc.shape[1]
    cmp_end = jnp.arange(n_c) * CMP_STRIDE + CMP_BLOCK - 1
    s_c = jnp.einsum('btgjd,bngd->bgjtn', qg, kc) * scale
    p_c = masked_softmax(s_c, cmp_end[None, :] <= t_all[:, None])
    o_c = jnp.einsum('bgjtn,bngd->btgjd', p_c.astype(vc.dtype), vc)

    n_s = T // SLC_BLOCK
    n_sel = min(SLC_TOPN, n_s)
    imp = jnp.einsum('bgjtn,nm->bgtm', p_c, jnp.asarray(slc_overlap(n_c, n_s)))
    ks_blk = k_s.reshape(bsz, n_s, SLC_BLOCK, G, D).transpose(0, 3, 1, 2, 4)
    vs_blk = v_s.reshape(bsz, n_s, SLC_BLOCK, G, D).transpose(0, 3, 1, 2, 4)
    kw_pad = jnp.pad(k_w, ((0, 0), (WINDOW, 0), (0, 0), (0, 0)))
    vw_pad = jnp.pad(v_w, ((0, 0), (WINDOW, 0), (0, 0), (0, 0)))
    b_ix = jnp.arange(bsz)[:, None, None, None]
    g_ix = jnp.arange(G)[None, :, None, None]
    blk_ids = jnp.arange(n_s)

    def block(i):
        t0 = i * Q_BLOCK
        tq = t0 + jnp.arange(Q_BLOCK)
        qb = lax.dynamic_slice_in_dim(qg, t0, Q_BLOCK, 1)
        gq = lax.dynamic_slice_in_dim(g, t0, Q_BLOCK, 1)
        sc = lax.dynamic_slice_in_dim(imp, t0, Q_BLOCK, 2)
        cur = tq // SLC_BLOCK
        forced = (blk_ids[None, :] == 0) | (blk_ids[None, :] == cur[:, None]) | (blk_ids[None, :] == cur[:, None] - 1)
        admissible = blk_ids[None, :] * SLC_BLOCK <= tq[:, None]
        sc = jnp.where(forced, jnp.inf, sc)
        sc = jnp.where(admissible, sc, -jnp.inf)
        _, idx = lax.top_k(sc, n_sel)
        kg = ks_blk[b_ix, g_ix, idx]
        vg = vs_blk[b_ix, g_ix, idx]
        pos = idx[..., None] * SLC_BLOCK + jnp.arange(SLC_BLOCK)
        valid = (pos <= tq[None, None, :, None, None]).reshape(bsz, G, Q_BLOCK, n_sel * SLC_BLOCK)[:, :, None]
        s_s = jnp.einsum('bqgjd,bgqnsd->bgjqns', qb, kg).reshape(bsz, G, J, Q_BLOCK, n_sel * SLC_BLOCK) * scale
        p_s = masked_softmax(s_s, valid)
        o_s = jnp.einsum('bgjqk,bgqkd->bqgjd', p_s.astype(vg.dtype), vg.reshape(bsz, G, Q_BLOCK, n_sel * SLC_BLOCK, D))
        kw = lax.dynamic_slice_in_dim(kw_pad, t0, WINDOW + Q_BLOCK, 1)
        vw = lax.dynamic_slice_in_dim(vw_pad, t0, WINDOW + Q_BLOCK, 1)
        pos_w = t0 - WINDOW + jnp.arange(WINDOW + Q_BLOCK)
        valid_w = (pos_w[None, :] >= 0) & (pos_w[None, :] <= tq[:, None]) & (pos_w[None, :] > tq[:, None] - WINDOW)
        s_w = jnp.einsum('bqgjd,bsgd->bgjqs', qb, kw) * scale
        p_w = masked_softmax(s_w, valid_w)
        o_w = jnp.einsum('bgjqs,bsgd->bqgjd', p_w.astype(vw.dtype), vw)
        return gq[..., 1:2] * o_s + gq[..., 2:3] * o_w

    o_sw = sweep_query_blocks(block, T)
    o = g[..., 0:1] * o_c + o_sw
    return o.reshape(bsz, T, B_HEADS, D)


def even_mixer(h, w_in, cmp_pe, cmp_w1, cmp_w2, w_out, cos, sin):
    bsz, T, _ = h.shape
    proj = jnp.einsum('btd,de->bte', h, w_in)
    qa, ka, va, qi, ki, wi, qb, kvb, gb = split_cols(proj, EVEN_SPLITS)
    qa = apply_rope(qa.reshape(bsz, T, A_HEADS, HEAD_DIM), cos, sin)
    ka = apply_rope(ka, cos, sin)
    qi = apply_rope(qi.reshape(bsz, T, IDX_HEADS, HEAD_DIM), cos, sin)
    ki = apply_rope(ki, cos, sin)
    o_a = dsa_attention(qa, ka, va, qi, ki, wi)
    qb = apply_rope(qb.reshape(bsz, T, B_HEADS, HEAD_DIM), cos, sin)
    kvb = kvb.reshape(bsz, T, 6, B_KV_GROUPS, HEAD_DIM)
    k_c = apply_rope(kvb[:, :, 0], cos, sin)
    v_c = kvb[:, :, 1]
    k_s = apply_rope(kvb[:, :, 2], cos, sin)
    v_s = kvb[:, :, 3]
    k_w = apply_rope(kvb[:, :, 4], cos, sin)
    v_w = kvb[:, :, 5]
    o_b = nsa_attention(qb, k_c, v_c, k_s, v_s, k_w, v_w, gb, cmp_pe, cmp_w1, cmp_w2)
    o = jnp.concatenate([o_a.reshape(bsz, T, -1), o_b.reshape(bsz, T, -1)], axis=-1)
    return jnp.einsum('bte,ed->btd', o, w_out)


def diff_mixer(h, w_in, lam, subln_g, w_out, cos, sin, lambda_init):
    bsz, T, _ = h.shape
    proj = jnp.einsum('btd,de->bte', h, w_in)
    q, k, v = jnp.split(proj, 3, axis=-1)
    q = apply_rope(q.reshape(bsz, T, C_HEADS * 2, HEAD_DIM), cos, sin).reshape(bsz, T, C_HEADS, 2, HEAD_DIM)
    k = apply_rope(k.reshape(bsz, T, C_HEADS * 2, HEAD_DIM), cos, sin).reshape(bsz, T, C_HEADS, 2, HEAD_DIM)
    v = v.reshape(bsz, T, C_HEADS, 2 * HEAD_DIM)
    lf = lam.astype(jnp.float32)
    lam_val = jnp.exp(jnp.sum(lf[0] * lf[1])) - jnp.exp(jnp.sum(lf[2] * lf[3])) + lambda_init
    scale = HEAD_DIM ** -0.5
    key_pos = jnp.arange(T)

    def block(i):
        t0 = i * Q_BLOCK
        tq = t0 + jnp.arange(Q_BLOCK)
        qb = lax.dynamic_slice_in_dim(q, t0, Q_BLOCK, 1)
        s = jnp.einsum('bqhcd,bshcd->bhcqs', qb, k) * scale
        p = masked_softmax(s, key_pos[None, :] <= tq[:, None])
        a = p[:, :, 0] - lam_val * p[:, :, 1]
        return jnp.einsum('bhqs,bshe->bqhe', a.astype(v.dtype), v)

    o = sweep_query_blocks(block, T)
    o = rms_norm(o, subln_g) * (1.0 - lambda_init)
    return jnp.einsum('bte,ed->btd', o.reshape(bsz, T, ODD_MIX), w_out)


def sq_relu_mlp(h, w_up, w_down):
    u = jnp.einsum('btd,df->btf', h, w_up)
    return jnp.einsum('btf,fd->btd', jnp.square(jax.nn.relu(u)), w_down)


def setup_inputs(seed: int = 0) -> dict:
    key = jax.random.key(seed)
    ks = jax.random.split(key, 16)
    ne = (DEPTH + 1) // 2
    no = DEPTH // 2

    def nrm(k, shape, fan_in):
        return jax.random.normal(k, shape, jnp.float32) * (fan_in ** -0.5)

    def gain(k, shape):
        return 1.0 + 0.05 * jax.random.normal(k, shape, jnp.float32)

    return {
        'x': jax.random.normal(ks[0], (BATCH, SEQ, D_MODEL), jnp.float32),
        'mix_norm_g': gain(ks[1], (DEPTH, D_MODEL)),
        'mlp_norm_g': gain(ks[2], (DEPTH, D_MODEL)),
        'even_w_in': nrm(ks[3], (ne, D_MODEL, EVEN_IN), D_MODEL),
        'even_cmp_pe': 0.02 * jax.random.normal(ks[4], (ne, 2, CMP_BLOCK, HEAD_DIM), jnp.float32),
        'even_cmp_w1': nrm(ks[5], (ne, 2, CMP_BLOCK * HEAD_DIM, CMP_HIDDEN), CMP_BLOCK * HEAD_DIM),
        'even_cmp_w2': nrm(ks[6], (ne, 2, CMP_HIDDEN, HEAD_DIM), CMP_HIDDEN),
        'even_w_out': nrm(ks[7], (ne, EVEN_MIX, D_MODEL), EVEN_MIX),
        'odd_w_in': nrm(ks[8], (no, D_MODEL, ODD_IN), D_MODEL),
        'odd_lambda': 0.1 * jax.random.normal(ks[9], (no, 4, HEAD_DIM), jnp.float32),
        'odd_subln_g': gain(ks[10], (no, 2 * HEAD_DIM)),
        'odd_w_out': nrm(ks[11], (no, ODD_MIX, D_MODEL), ODD_MIX),
        'mlp_w_up': nrm(ks[12], (DEPTH, D_MODEL, D_FF), D_MODEL),
        'mlp_w_down': nrm(ks[13], (DEPTH, D_FF, D_MODEL), D_FF),
        'final_norm_g': gain(ks[14], (D_MODEL,)),
    }


def reference(x, mix_norm_g, mlp_norm_g, even_w_in, even_cmp_pe, even_cmp_w1, even_cmp_w2, even_w_out, odd_w_in, odd_lambda, odd_subln_g, odd_w_out, mlp_w_up, mlp_w_down, final_norm_g):
    T = x.shape[1]
    cos, sin = rope_tables(T)
    h = x
    for layer in range(DEPTH):
        u = rms_norm(h, mix_norm_g[layer])
        if layer % 2 == 0:
            e = layer // 2
            h = h + even_mixer(u, even_w_in[e], even_cmp_pe[e], even_cmp_w1[e], even_cmp_w2[e], even_w_out[e], cos, sin)
        else:
            o = layer // 2
            lambda_init = 0.8 - 0.6 * math.exp(-0.3 * layer)
            h = h + diff_mixer(u, odd_w_in[o], odd_lambda[o], odd_subln_g[o], odd_w_out[o], cos, sin, lambda_init)
        h = h + sq_relu_mlp(rms_norm(h, mlp_norm_g[layer]), mlp_w_up[layer], mlp_w_down[layer])
    return rms_norm(h, final_norm_g)
```

```python
import math
import numpy as np
from contextlib import ExitStack
import ml_dtypes
import concourse.bass as bass
import concourse.mybir as mybir
from concourse.bass_utils import run_bass_kernel_spmd

F32 = mybir.dt.float32
BF16 = mybir.dt.bfloat16
AF = mybir.ActivationFunctionType
ALU = mybir.AluOpType
AX = mybir.AxisListType

T = 4096
D = 1024
NT = 32
DFF = 4096
EPS = 1e-6
NEG = -30000.0
N_CORES = 8


class Buf:
    __slots__ = ('name', 'lw', 'rd')

    def __init__(self, name=''):
        self.name = name
        self.lw = None
        self.rd = {}


class Tl:
    def __init__(self, t, name=''):
        self.t = t
        self.b = Buf(name)

    def __getitem__(self, k):
        return self.t[k]


def _bufs(lst):
    out = []
    for x in lst:
        if x is None:
            continue
        out.append(x.b if isinstance(x, Tl) else x)
    return out


class Prog:
    CE = ('pe', 'act', 'dve', 'pool')

    def __init__(self, nc, es, n_dma_sems=12):
        self.nc = nc
        self.ops = {e: [] for e in ('pe', 'act', 'dve', 'pool', 'sp')}
        self.sems = {}
        for e in self.CE:
            self.sems[e] = es.enter_context(nc.semaphore('s_' + e))
        self.cnt = {e: 0 for e in self.CE}
        self.dq = {}
        for q in ('sp', 'pool', 'act'):
            n = n_dma_sems if q == 'sp' else 6
            ss = []
            for i in range(n):
                k = 'd_%s_%d' % (q, i)
                self.sems[k] = es.enter_context(nc.semaphore(k))
                self.cnt[k] = 0
                ss.append(k)
            self.dq[q] = [ss, 0]
        self.waited = {e: {} for e in self.ops}
        self.same_engine_sync = {'pe': False, 'act': True, 'dve': True, 'pool': True, 'sp': True}
        self.total = 0

    def _deps(self, reads, writes):
        deps = {}
        for b in reads:
            if b.lw is not None:
                k, v = b.lw
                if deps.get(k, 0) < v:
                    deps[k] = v
        for b in writes:
            if b.lw is not None:
                k, v = b.lw
                if deps.get(k, 0) < v:
                    deps[k] = v
            for k, v in b.rd.items():
                if deps.get(k, 0) < v:
                    deps[k] = v
        return deps

    def _mark(self, tok, reads, writes):
        k, v = tok
        for b in reads:
            if b.rd.get(k, 0) < v:
                b.rd[k] = v
        for b in writes:
            b.lw = tok
            b.rd = {}

    def op(self, eng, fn, reads=(), writes=()):
        reads = _bufs(reads)
        writes = _bufs(writes)
        deps = self._deps(reads, writes)
        waits = []
        wd = self.waited[eng]
        for k, v in deps.items():
            if k == eng and not self.same_engine_sync[eng]:
                continue
            if wd.get(k, 0) >= v:
                continue
            wd[k] = v
            waits.append((k, v))
        self.cnt[eng] += 1
        tok = (eng, self.cnt[eng])
        self.ops[eng].append((waits, fn, eng, 1))
        self._mark(tok, reads, writes)
        self.total += 1

    def dma(self, fn, reads=(), writes=(), q='sp'):
        reads = _bufs(reads)
        writes = _bufs(writes)
        ss, i = self.dq[q]
        k = ss[i % len(ss)]
        self.dq[q][1] = i + 1
        deps = self._deps(reads, writes)
        if self.cnt[k] > 0 and deps.get(k, 0) < self.cnt[k]:
            deps[k] = self.cnt[k]
        waits = []
        wd = self.waited[q]
        for kk, v in deps.items():
            if wd.get(kk, 0) >= v:
                continue
            wd[kk] = v
            waits.append((kk, v))
        self.cnt[k] += 16
        tok = (k, self.cnt[k])
        self.ops[q].append((waits, fn, k, 16))
        self._mark(tok, reads, writes)
        self.total += 1

    def barrier(self):
        for e in self.ops:
            waits = []
            wd = self.waited[e]
            for k, v in self.cnt.items():
                if v > 0 and wd.get(k, 0) < v:
                    wd[k] = v
                    waits.append((k, v))
            if waits:
                self.ops[e].append((waits, None, None, 0))

    def emit(self):
        nc = self.nc
        sems = self.sems

        def mk(name):
            lst = self.ops[name]

            def body(e):
                for waits, fn, sk, inc in lst:
                    for k, v in waits:
                        e.wait_ge(sems[k], v)
                    if fn is not None:
                        fn(e).then_inc(sems[sk], inc)
            return body

        with nc.Block() as block:
            block.tensor(mk('pe'))
            block.scalar(mk('act'))
            block.vector(mk('dve'))
            block.gpsimd(mk('pool'))
            block.sync(mk('sp'))
        self.ops = {e: [] for e in self.ops}


class KB:
    def __init__(self, nc, es):
        self.nc = nc
        self.P = Prog(nc, es)
        self.dbufs = {}
        self.uid = 0

    def sb(self, es, shape, dt, name=None):
        self.uid += 1
        name = (name or 't') + '_%d' % self.uid
        return Tl(es.enter_context(self.nc.sbuf_tensor(name, list(shape), dt)), name)

    def ps(self, es, shape, dt, name=None):
        self.uid += 1
        name = (name or 'p') + '_%d' % self.uid
        return Tl(es.enter_context(self.nc.psum_tensor(name, list(shape), dt)), name)

    def db(self, *key):
        b = self.dbufs.get(key)
        if b is None:
            b = Buf(str(key))
            self.dbufs[key] = b
        return b

    def mm(self, out, lhsT, rhs, start, stop, r, w):
        self.P.op('pe', lambda e: e.matmul(out, lhsT=lhsT, rhs=rhs, start=start, stop=stop,
                                           skip_group_check=True), r, w)

    def tr(self, out, in_, ident, r, w):
        self.P.op('pe', lambda e: e.transpose(out=out, in_=in_, identity=ident), r, w)

    def act(self, out, in_, func, r, w, scale=None, bias=None, accum_out=None, eng='act'):
        kw = {}
        if scale is not None:
            kw['scale'] = scale
        if bias is not None:
            kw['bias'] = bias
        if accum_out is not None:
            kw['accum_out'] = accum_out
        self.P.op('act', lambda e: e.activation(out=out, in_=in_, func=func, **kw), r, w)

    def ts(self, eng, out, in0, s1, op0, r, w, s2=None, op1=None, accum_out=None):
        kw = {}
        if op1 is not None:
            kw['op1'] = op1
        if accum_out is not None:
            kw['accum_out'] = accum_out
        self.P.op(eng, lambda e: e.tensor_scalar(out=out, in0=in0, scalar1=s1, scalar2=s2, op0=op0, **kw), r, w)

    def tt(self, eng, out, in0, in1, op, r, w):
        self.P.op(eng, lambda e: e.tensor_tensor(out=out, in0=in0, in1=in1, op=op), r, w)

    def stt(self, eng, out, in0, scalar, in1, op0, op1, r, w):
        self.P.op(eng, lambda e: e.scalar_tensor_tensor(out=out, in0=in0, scalar=scalar, in1=in1, op0=op0, op1=op1), r, w)

    def cp(self, eng, out, in_, r, w):
        if eng == 'act':
            self.P.op('act', lambda e: e.copy(out=out, in_=in_), r, w)
        else:
            self.P.op(eng, lambda e: e.tensor_copy(out=out, in_=in_), r, w)

    def memset(self, eng, ap, val, w):
        self.P.op(eng, lambda e: e.memset(ap, val), (), w)

    def recip(self, out, in_, r, w):
        self.P.op('dve', lambda e: e.reciprocal(out=out, in_=in_), r, w)

    def dma(self, out, in_, r, w, q='sp'):
        self.P.dma(lambda e: e.dma_start(out=out, in_=in_), r, w, q=q)

    def rms_rstd(self, h, junk, ss, rstd, width):
        self.act(junk[:, 0:width], h[:, 0:width], AF.Square, [h], [junk, ss], accum_out=ss[:, 0:1])
        self.ts('dve', ss[:, 0:1], ss[:, 0:1], 1.0 / width, ALU.mult, [ss], [ss], s2=EPS, op1=ALU.add)
        self.act(ss[:, 0:1], ss[:, 0:1], AF.Sqrt, [ss], [ss])
        self.recip(rstd[:, 0:1], ss[:, 0:1], [ss], [rstd])


def build_program(cfg):
    nc = bass.Bass("TRN2", target_bir_lowering=False)
    layers = cfg['layers']
    dram = {}

    def din(name, shape, dt=F32):
        dram[name] = nc.dram_tensor(name, list(shape), dt, kind="ExternalInput").ap()
        return dram[name]

    def dscr(name, shape, dt):
        dram[name] = nc.dram_tensor(name, list(shape), dt, kind="Internal").ap()
        return dram[name]

    x = din('x', [T, D])
    y = nc.dram_tensor('y', [T, D], F32, kind="ExternalOutput").ap()
    din('mix_g', [4, 128, 8])
    din('mlp_g', [4, 128, 8])
    din('final_g', [1, D])
    din('ident', [128, 128], BF16)
    din('i4', [128, 512], BF16)
    din('tri', [128, 128], BF16)
    din('ropeC', [128, T])
    din('ropeS', [128, T])
    din('winfar', [128, 128], BF16)
    din('negm', [128, 128])
    din('cmask', [T, 256], BF16)
    din('fbias', [T, 64])
    din('ovl', [256, 64], BF16)
    for l in layers:
        din('w_up%d' % l, [D, DFF])
        din('w_down%d' % l, [DFF, D])
        din('w_out%d' % l, [D, D])
        if l % 2 == 0:
            din('wqk%d' % l, [D, 2048])
            din('wqks%d' % l, [D, 1920])
            din('wv%d' % l, [D, 348])
            din('w1_%d' % l, [2, 2048, 256])
            din('w2_%d' % l, [2, 256, 64])
            din('peT%d' % l, [2, 128, 32])
        if l % 2 == 1:
            din('wqk%d' % l, [D, 2048])
            din('wqks%d' % l, [D, 2048])
            din('wv%d' % l, [D, 1024])
            din('lam%d' % l, [1, 256])
            din('subg%d' % l, [1, 128])
    hs = dscr('hs', [T, D], F32)
    qk = dscr('qk', [20, 128, T], BF16)
    vs = dscr('vs', [T, 1024], BF16)
    os_ = dscr('os', [T, D], BF16)
    dscr('tm32', [T, 32], F32)
    dscr('kcs', [128, 256], BF16)
    dscr('vcs', [128, 256], BF16)

    with ExitStack() as es:
        kb = KB(nc, es)
        P = kb.P
        ident = kb.sb(es, [128, 128], BF16, 'ident')
        kb.dma(ident[:], dram['ident'][:, :], [], [ident])
        hsrc = x
        for li, l in enumerate(layers):
            if l % 2 == 1:
                phase_a(kb, dram, l, hsrc, ident)
                phase_b_odd(kb, dram, l, ident)
            else:
                phase_a(kb, dram, l, hsrc, ident)
                phase_b0_even(kb, dram, l)
                phase_b_even(kb, dram, l)
            last = (li == len(layers) - 1)
            phase_c(kb, dram, l, hsrc, ident, y if last else hs, last and cfg.get('final_norm', True))
            hsrc = hs
    return nc


def load_norm_transpose(kb, pes, h_ap, hb, g_sb, ident, ht, junk, ss, rstd, xn, pst, uT_ap, uT_tl):
    kb.dma(ht[:], h_ap, [hb], [ht])
    kb.rms_rstd(ht, junk, ss, rstd, D)
    kb.ts('dve', xn[:], ht[:], rstd[:, 0:1], ALU.mult, [ht, rstd], [xn])
    for c in range(8):
        kb.tr(pst[:, c * 128:(c + 1) * 128], xn[:, c * 128:(c + 1) * 128], ident[:], [xn, ident], [pst])
    kb.tt('dve', uT_ap, pst[:].rearrange("p (c t) -> p c t", c=8),
          g_sb[:].unsqueeze(2).broadcast_to([128, 8, 128]), ALU.mult, [pst, g_sb], [uT_tl])


def phase_a(kb, dram, l, hsrc, ident):
    P = kb.P
    even = (l % 2 == 0)
    nfm = 16
    nsw = 15 if even else 16
    ntm = 348 if even else 1024
    with ExitStack() as pes:
        wqk = kb.sb(pes, [128, 8, nfm * 128], BF16, 'wqk')
        wqks = kb.sb(pes, [128, 8, nsw * 128], BF16, 'wqks')
        wv = kb.sb(pes, [128, 8, ntm], BF16, 'wv')
        g_sb = kb.sb(pes, [128, 8], F32, 'g')
        kb.dma(g_sb[:], dram['mix_g'][l], [], [g_sb])
        for c in range(8):
            kb.dma(wqk[:, c, :], dram['wqk%d' % l][c * 128:(c + 1) * 128, :], [], [wqk], q='pool')
            kb.dma(wqks[:, c, :], dram['wqks%d' % l][c * 128:(c + 1) * 128, :], [], [wqks], q='pool')
            kb.dma(wv[:, c, :], dram['wv%d' % l][c * 128:(c + 1) * 128, :], [], [wv], q='pool')
        ht = [kb.sb(pes, [128, D], F32, 'ht') for _ in range(2)]
        junk = kb.sb(pes, [128, D], BF16, 'junk')
        ss = [kb.sb(pes, [128, 1], F32, 'ss') for _ in range(2)]
        rstd = [kb.sb(pes, [128, 1], F32, 'rstd') for _ in range(2)]
        xn = [kb.sb(pes, [128, D], BF16, 'xn') for _ in range(2)]
        uT = [kb.sb(pes, [128, 8, 512], BF16, 'uT') for _ in range(2)]
        cc = [kb.sb(pes, [128, 512], F32, 'cc') for _ in range(2)]
        sn = [kb.sb(pes, [128, 512], F32, 'sn') for _ in range(2)]
        t1 = [kb.sb(pes, [128, 512], F32, 't1') for _ in range(2)]
        t2 = [kb.sb(pes, [128, 512], F32, 't2') for _ in range(2)]
        stg = [kb.sb(pes, [128, 512], BF16, 'stg') for _ in range(3)]
        vst = [kb.sb(pes, [128, 1024], BF16, 'vst') for _ in range(2)]
        tmf = [kb.sb(pes, [128, 32], F32, 'tmf') for _ in range(2)]
        pst = [kb.ps(pes, [128, 1024], BF16, 'pst') for _ in range(2)]
        pa = [kb.ps(pes, [128, 512], F32, 'pa') for _ in range(2)]
        pb = [kb.ps(pes, [128, 512], F32, 'pb') for _ in range(2)]
        pv = [kb.ps(pes, [128, 512], F32, 'pv') for _ in range(2)]
        n = 0
        for G in range(8):
            u = uT[G % 2]
            kb.dma(cc[G % 2][:], dram['ropeC'][:, G * 512:(G + 1) * 512], [], [cc[G % 2]])
            kb.dma(sn[G % 2][:], dram['ropeS'][:, G * 512:(G + 1) * 512], [], [sn[G % 2]])
            for tt_ in range(4):
                i = G * 4 + tt_
                load_norm_transpose(kb, pes, hsrc[i * 128:(i + 1) * 128, :], kb.db('h', i), g_sb, ident,
                                    ht[i % 2], junk, ss[i % 2], rstd[i % 2], xn[i % 2], pst[i % 2],
                                    u[:, :, tt_ * 128:(tt_ + 1) * 128], u)
                us = u[:, :, tt_ * 128:(tt_ + 1) * 128]
                if not even:
                    for half in range(2):
                        pvt = pv[half]
                        for c in range(8):
                            kb.mm(pvt[:, :], u[:, c, tt_ * 128:(tt_ + 1) * 128], wv[:, c, half * 512:(half + 1) * 512],
                                  c == 0, c == 7, [u, wv], [pvt])
                        kb.cp('act', vst[i % 2][:, half * 512:(half + 1) * 512], pvt[:, :], [pvt], [vst[i % 2]])
                    kb.dma(dram['vs'][i * 128:(i + 1) * 128, :], vst[i % 2][:], [vst[i % 2]], [kb.db('vs', i)])
                else:
                    pvt = pv[i % 2]
                    for c in range(8):
                        kb.mm(pvt[:, 0:ntm], u[:, c, tt_ * 128:(tt_ + 1) * 128], wv[:, c, :], c == 0, c == 7, [u, wv], [pvt])
                    kb.cp('act', vst[i % 2][:, 0:320], pvt[:, 0:320], [pvt], [vst[i % 2]])
                    kb.cp('act', tmf[i % 2][:, 0:4], pvt[:, 320:324], [pvt], [tmf[i % 2]])
                    kb.act(tmf[i % 2][:, 4:28], pvt[:, 324:348], AF.Sigmoid, [pvt], [tmf[i % 2]])
                    kb.dma(dram['vs'][i * 128:(i + 1) * 128, 0:320], vst[i % 2][:, 0:320], [vst[i % 2]], [kb.db('vs', i)])
                    kb.dma(dram['tm32'][i * 128:(i + 1) * 128, :], tmf[i % 2][:, :], [tmf[i % 2]], [kb.db('tm32', i)])
            for rb in range(nfm):
                pat = pa[rb % 2]
                pbt = pb[rb % 2]
                for c in range(8):
                    kb.mm(pat[:, :], wqk[:, c, rb * 128:(rb + 1) * 128], u[:, c, :], c == 0, c == 7, [u, wqk], [pat])
                st = stg[n % 3]
                n += 1
                if rb < nsw:
                    for c in range(8):
                        kb.mm(pbt[:, :], wqks[:, c, rb * 128:(rb + 1) * 128], u[:, c, :], c == 0, c == 7, [u, wqks], [pbt])
                    a1 = t1[rb % 2]
                    a2 = t2[rb % 2]
                    kb.tt('dve', a1[:], pat[:, :], cc[G % 2][:], ALU.mult, [pat, cc[G % 2]], [a1])
                    kb.tt('dve', a2[:], pbt[:, :], sn[G % 2][:], ALU.mult, [pbt, sn[G % 2]], [a2])
                    kb.tt('pool', st[:], a1[:], a2[:], ALU.add, [a1, a2], [st])
                else:
                    kb.cp('act', st[:], pat[:, :], [pat], [st])
                kb.dma(dram['qk'][rb, :, G * 512:(G + 1) * 512], st[:], [st], [kb.db('qk', rb, G)])
        P.barrier()
        P.emit()


def phase_b_odd(kb, dram, l, ident):
    P = kb.P
    lambda_init = 0.8 - 0.6 * math.exp(-0.3 * l)
    with ExitStack() as pes:
        i4 = kb.sb(pes, [128, 512], BF16, 'i4')
        tri = kb.sb(pes, [128, 128], BF16, 'tri')
        zer = kb.sb(pes, [128, 512], BF16, 'zer')
        kb.dma(i4[:], dram['i4'][:, :], [], [i4])
        kb.dma(tri[:], dram['tri'][:, :], [], [tri])
        kb.memset('pool', zer[:], 0.0, [zer])
        lam = kb.sb(pes, [128, 256], F32, 'lam')
        lw = kb.sb(pes, [128, 128], F32, 'lw')
        lv = kb.sb(pes, [128, 4], F32, 'lv')
        subg = kb.sb(pes, [128, 128], F32, 'subg')
        kb.dma(lam[:], dram['lam%d' % l][0:1, :].partition_broadcast(128), [], [lam])
        kb.dma(subg[:], dram['subg%d' % l][0:1, :].partition_broadcast(128), [], [subg])
        kb.tt('dve', lw[:, 0:64], lam[:, 0:64], lam[:, 64:128], ALU.mult, [lam], [lw])
        kb.tt('dve', lw[:, 64:128], lam[:, 128:192], lam[:, 192:256], ALU.mult, [lam, lw], [lw])
        kb.P.op('dve', lambda e: e.reduce_sum(out=lv[:, 0:2], in_=lw[:].rearrange("p (a b) -> p a b", a=2), axis=AX.X), _bufs([lw]), _bufs([lv]))
        kb.act(lv[:, 0:2], lv[:, 0:2], AF.Exp, [lv], [lv])
        kb.tt('dve', lv[:, 2:3], lv[:, 1:2], lv[:, 0:1], ALU.subtract, [lv], [lv])
        kb.ts('dve', lv[:, 2:3], lv[:, 2:3], -lambda_init, ALU.add, [lv], [lv])
        kb.ts('dve', subg[:], subg[:], 1.0 - lambda_init, ALU.mult, [subg], [subg])

        kT = [kb.sb(pes, [128, T], BF16, 'kT') for _ in range(2)]
        vh = [kb.sb(pes, [128, NT, 130], BF16, 'vh') for _ in range(2)]
        for v_ in vh:
            kb.memset('pool', v_[:, :, 128:129], 1.0, [v_])
        qT = [kb.sb(pes, [128, 512], BF16, 'qT') for _ in range(2)]
        pT = [kb.sb(pes, [128, 512], BF16, 'pT') for _ in range(3)]
        oc = [kb.sb(pes, [128, 4, 128], F32, 'oc') for _ in range(2)]
        rd = [kb.sb(pes, [128, 4], F32, 'rd') for _ in range(2)]
        ao = kb.sb(pes, [128, 4, 128], F32, 'ao')
        junk = kb.sb(pes, [128, 128], F32, 'junk')
        s4 = kb.sb(pes, [128, 4], F32, 's4')
        ob = [kb.sb(pes, [128, 4, 128], BF16, 'ob') for _ in range(2)]
        psS = [kb.ps(pes, [128, 512], F32, 'psS') for _ in range(3)]
        pvb = [[kb.ps(pes, [128, 512], F32, 'pvb') for _ in range(2)] for _ in range(2)]
        ns = 0
        nq = 0
        for h in range(8):
            kt = kT[h % 2]
            vt = vh[h % 2]
            kb.dma(kt[:], dram['qk'][8 + h, :, :], [kb.db('qk', 8 + h, G) for G in range(8)], [kt])
            kb.dma(vt[:, :, 0:128], dram['vs'][:, h * 128:(h + 1) * 128].rearrange("(n p) d -> p n d", p=128),
                   [kb.db('vs', i) for i in range(NT)], [vt])
            for Q in range(8):
                qt = qT[nq % 2]
                nq += 1
                kb.dma(qt[:], dram['qk'][h, :, Q * 512:(Q + 1) * 512], [kb.db('qk', h, Q)], [qt])
                for c in range(2):
                    lo = c * 64
                    hi = lo + 64
                    pv2 = pvb[c]
                    for bnk in pv2:
                        kb.mm(bnk[:, 0:258], zer[:, 0:128], zer[:, 0:258], True, False, [zer], [bnk])
                    for j in range(4 * Q + 4):
                        t0 = max(0, j - 4 * Q)
                        pS = psS[ns % 3]
                        pt = pT[ns % 3]
                        ns += 1
                        if j >= 4 * Q:
                            kb.mm(pS[:, t0 * 128:(t0 + 1) * 128], tri[:], i4[:, 0:128], True, False, [tri, i4], [pS])
                            kb.mm(pS[:, t0 * 128:512], kt[lo:hi, j * 128:(j + 1) * 128], qt[lo:hi, t0 * 128:512],
                                  False, True, [kt, qt], [pS])
                        else:
                            kb.mm(pS[:, :], kt[lo:hi, j * 128:(j + 1) * 128], qt[lo:hi, :], True, True, [kt, qt], [pS])
                        kb.act(pt[:, t0 * 128:512], pS[:, t0 * 128:512], AF.Exp, [pS], [pt], scale=0.125)
                        for t_ in range(t0, 4):
                            bnk = pv2[t_ // 2]
                            col = (t_ % 2) * 129
                            kb.mm(bnk[:, col:col + 129], pt[:, t_ * 128:(t_ + 1) * 128], vt[:, j, 0:129],
                                  False, False, [pt, vt], [bnk])
                    for t_ in range(4):
                        bnk = pv2[t_ // 2]
                        col = (t_ % 2) * 129
                        kb.recip(rd[c][:, t_:t_ + 1], bnk[:, col + 128:col + 129], [bnk], [rd[c]])
                    if c == 1:
                        kb.ts('dve', rd[c][:], rd[c][:], lv[:, 2:3], ALU.mult, [rd[c], lv], [rd[c]])
                    for t_ in range(4):
                        bnk = pv2[t_ // 2]
                        col = (t_ % 2) * 129
                        kb.act(oc[c][:, t_, :], bnk[:, col:col + 128], AF.Copy, [bnk, rd[c]], [oc[c]], scale=rd[c][:, t_:t_ + 1])
                kb.tt('dve', ao[:], oc[0][:], oc[1][:], ALU.add, [oc[0], oc[1]], [ao])
                for t_ in range(4):
                    kb.act(junk[:], ao[:, t_, :], AF.Square, [ao], [junk, s4], accum_out=s4[:, t_:t_ + 1])
                kb.ts('dve', s4[:], s4[:], 1.0 / 128, ALU.mult, [s4], [s4], s2=EPS, op1=ALU.add)
                kb.act(s4[:], s4[:], AF.Sqrt, [s4], [s4])
                kb.recip(s4[:], s4[:], [s4], [s4])
                kb.tt('dve', ao[:], ao[:], s4[:].unsqueeze(2).broadcast_to([128, 4, 128]), ALU.mult, [ao, s4], [ao])
                o_ = ob[Q % 2]
                kb.tt('pool', o_[:], ao[:], subg[:].unsqueeze(1).broadcast_to([128, 4, 128]), ALU.mult, [ao, subg], [o_])
                kb.dma(dram['os'][Q * 512:(Q + 1) * 512, h * 128:(h + 1) * 128].rearrange("(n p) d -> p n d", p=128),
                       o_[:], [o_], [kb.db('os', Q, h)])
        P.barrier()
        P.emit()


def phase_b0_even(kb, dram, l):
    P = kb.P
    with ExitStack() as pes:
        w1 = [kb.sb(pes, [128, 32, 256], BF16, 'w1') for _ in range(2)]
        w2k = kb.sb(pes, [128, 2, 128], BF16, 'w2k')
        w2v = kb.sb(pes, [128, 2, 64], BF16, 'w2v')
        peT = kb.sb(pes, [128, 2, 32], F32, 'peT')
        srcs = [kb.sb(pes, [128, T], BF16, 'src') for _ in range(2)]
        xlo = [kb.sb(pes, [128, T], BF16, 'xlo') for _ in range(2)]
        xhi = [kb.sb(pes, [128, T], BF16, 'xhi') for _ in range(2)]
        hT = [kb.sb(pes, [128, 2, 2, 256], BF16, 'hT') for _ in range(2)]
        g0 = [kb.sb(pes, [128, 256], F32, 'g0') for _ in range(2)]
        g1 = [kb.sb(pes, [128, 256], F32, 'g1') for _ in range(2)]
        kcs = kb.sb(pes, [128, 256], BF16, 'kcs')
        vcs = kb.sb(pes, [128, 2, 2, 64], BF16, 'vcs')
        ph = [kb.ps(pes, [128, 512], F32, 'ph') for _ in range(2)]
        pk = [kb.ps(pes, [128, 512], F32, 'pk') for _ in range(2)]
        for kv in range(2):
            wv_ = dram['w1_%d' % l][kv].rearrange("(l d) f -> d l f", d=64)
            kb.dma(w1[kv][0:64, :, :], wv_, [], [w1[kv]], q='pool')
            kb.dma(w1[kv][64:128, :, :], wv_, [], [w1[kv]], q='pool')
            kb.dma(srcs[kv][:], dram['qk'][12 if kv == 0 else 15, :, :], [kb.db('qk', 12 if kv == 0 else 15, G) for G in range(8)], [srcs[kv]])
            kb.memset('pool', hT[kv][:, :, :, 255:256], 0.0, [hT[kv]])
        w2kv = dram['w2_%d' % l][0].rearrange("(c p) d -> p c d", p=128)
        kb.dma(w2k[:, :, 0:64], w2kv, [], [w2k], q='pool')
        kb.dma(w2k[:, :, 64:128], w2kv, [], [w2k], q='pool')
        kb.dma(w2v[:, :, :], dram['w2_%d' % l][1].rearrange("(c p) d -> p c d", p=128), [], [w2v], q='pool')
        kb.dma(peT[:, :, :], dram['peT%d' % l].rearrange("k p l -> p k l"), [], [peT])
        n = 0
        for kv in range(2):
            sv = srcs[kv][:].rearrange("p (n r) -> p n r", r=16)
            kb.tt('dve', xlo[kv][:].rearrange("p (n r) -> p n r", r=16), sv,
                  peT[:, kv, 0:16].unsqueeze(1).broadcast_to([128, 256, 16]), ALU.add, [srcs[kv], peT], [xlo[kv]])
            kb.tt('dve', xhi[kv][:].rearrange("p (n r) -> p n r", r=16), sv,
                  peT[:, kv, 16:32].unsqueeze(1).broadcast_to([128, 256, 16]), ALU.add, [srcs[kv], peT], [xhi[kv]])
            lov = xlo[kv][:].rearrange("p (n r) -> p n r", r=16)
            hiv = xhi[kv][:].rearrange("p (n r) -> p n r", r=16)
            for g in range(2):
                rows = slice(g * 64, g * 64 + 64)
                for fc in range(2):
                    p_ = ph[n % 2]
                    a0 = g0[n % 2]
                    a1 = g1[n % 2]
                    n += 1
                    for l_ in range(32):
                        rhs = lov[rows, 0:255, l_] if l_ < 16 else hiv[rows, 1:256, l_ - 16]
                        kb.mm(p_[:, 0:255], w1[kv][rows, l_, fc * 128:(fc + 1) * 128], rhs, l_ == 0, l_ == 31,
                              [w1[kv], xlo[kv], xhi[kv]], [p_])
                    kb.cp('act', a0[:, 0:255], p_[:, 0:255], [p_], [a0])
                    kb.tt('dve', a1[:, 0:255], a0[:, 0:255], a0[:, 0:255], ALU.mult, [a0], [a1])
                    kb.ts('dve', a1[:, 0:255], a1[:, 0:255], 0.044715, ALU.mult, [a1], [a1], s2=1.0, op1=ALU.add)
                    kb.tt('dve', a1[:, 0:255], a1[:, 0:255], a0[:, 0:255], ALU.mult, [a1, a0], [a1])
                    kb.act(a1[:, 0:255], a1[:, 0:255], AF.Tanh, [a1], [a1], scale=0.7978845608028654)
                    kb.ts('dve', a1[:, 0:255], a1[:, 0:255], 0.5, ALU.mult, [a1], [a1], s2=0.5, op1=ALU.add)
                    kb.tt('dve', hT[kv][:, g, fc, 0:255], a1[:, 0:255], a0[:, 0:255], ALU.mult, [a1, a0], [hT[kv]])
                if kv == 0:
                    p2 = pk[g]
                    for fc in range(2):
                        kb.mm(p2[:, 0:256], w2k[:, fc, :], hT[0][:, g, fc, :], fc == 0, fc == 1, [w2k, hT[0]], [p2])
                    kb.cp('act', kcs[rows, :], p2[rows, 0:256], [p2], [kcs])
                else:
                    for nch in range(2):
                        p2 = pk[nch]
                        for fc in range(2):
                            kb.mm(p2[:, 0:64], hT[1][:, g, fc, nch * 128:(nch + 1) * 128], w2v[:, fc, :], fc == 0, fc == 1,
                                  [w2v, hT[1]], [p2])
                        kb.cp('act', vcs[:, nch, g, :], p2[:, 0:64], [p2], [vcs])
        kb.dma(dram['kcs'][:, :], kcs[:], [kcs], [kb.db('kcs')])
        kb.dma(dram['vcs'][:, :], vcs[:].rearrange("p a g d -> p (a g d)"), [vcs], [kb.db('vcs')])
        P.barrier()
        P.emit()


def phase_b_even(kb, dram, l):
    P = kb.P
    with ExitStack() as pes:
        i4 = kb.sb(pes, [128, 512], BF16, 'i4')
        tri = kb.sb(pes, [128, 128], BF16, 'tri')
        wfar = kb.sb(pes, [128, 128], BF16, 'wfar')
        negm = kb.sb(pes, [128, 128], F32, 'negm')
        zer = kb.sb(pes, [128, 512], BF16, 'zer')
        kb.dma(i4[:], dram['i4'][:, :], [], [i4])
        kb.dma(tri[:], dram['tri'][:, :], [], [tri])
        kb.dma(wfar[:], dram['winfar'][:, :], [], [wfar])
        kb.dma(negm[:], dram['negm'][:, :], [], [negm])
        kb.memset('pool', zer[:], 0.0, [zer])
        kres = {}
        for nm, blk in (('ka', 4), ('ki', 7), ('ks', 13), ('kw', 14)):
            kres[nm] = kb.sb(pes, [128, T], BF16, nm)
            kb.dma(kres[nm][:], dram['qk'][blk, :, :], [kb.db('qk', blk, G) for G in range(8)], [kres[nm]])
        kcT = kb.sb(pes, [128, 256], BF16, 'kcT')
        kb.dma(kcT[:], dram['kcs'][:, :], [kb.db('kcs')], [kcT])
        vc = kb.sb(pes, [128, 2, 2, 130], BF16, 'vc')
        kb.dma(vc[:, :, :, 0:64], dram['vcs'].rearrange("p (a g d) -> p a g d", a=2, g=2), [kb.db('vcs')], [vc])
        kb.memset('pool', vc[:, :, :, 64:65], 1.0, [vc])
        for g in range(2):
            kb.dma(vc[:, :, g, 65:129], dram['ovl'].rearrange("(a p) m -> p a m", p=128), [], [vc])
        vt = kb.sb(pes, [128, NT, 5, 66], BF16, 'vt')
        kb.memset('pool', vt[:, :, :, 64:65], 1.0, [vt])
        for h5 in range(5):
            vsv = dram['vs'][:, h5 * 64:(h5 + 1) * 64].rearrange("(n p) d -> p n d", p=128)
            for q2 in range(2):
                kb.dma(vt[:, q2 * 16:(q2 + 1) * 16, h5, 0:64], vsv[:, q2 * 16:(q2 + 1) * 16, :], [kb.db('vs', i) for i in range(NT)], [vt])

        qs = [kb.sb(pes, [128, 10, 128], BF16, 'qs') for _ in range(2)]
        tmt = [kb.sb(pes, [128, 32], F32, 'tm') for _ in range(2)]
        cmt = [kb.sb(pes, [128, 256], BF16, 'cm') for _ in range(2)]
        fbt = [kb.sb(pes, [128, 64], F32, 'fb') for _ in range(2)]
        sc2 = [kb.sb(pes, [128, T], F32, 'sc') for _ in range(2)]
        wk = kb.sb(pes, [128, T], F32, 'wk')
        rtmp = [kb.sb(pes, [128, 512], F32, 'rt') for _ in range(2)]
        m8 = kb.sb(pes, [128, 8], F32, 'm8')
        mb2 = [kb.sb(pes, [128, T], BF16, 'mb') for _ in range(2)]
        msel = [kb.sb(pes, [128, T], BF16, 'msel') for _ in range(2)]
        pT = [kb.sb(pes, [128, 512], BF16, 'pT') for _ in range(3)]
        rden = kb.sb(pes, [128, 4], F32, 'rden')
        coef = kb.sb(pes, [128, 4], F32, 'coef')
        imp = kb.sb(pes, [128, 64], F32, 'imp')
        scs = kb.sb(pes, [128, 64], F32, 'scs')
        scw = kb.sb(pes, [128, 64], F32, 'scw')
        selb = kb.sb(pes, [128, 64], BF16, 'selb')
        m8b = kb.sb(pes, [128, 8], F32, 'm8b')
        thr = kb.sb(pes, [128, 1], F32, 'thr')
        oacc = kb.sb(pes, [128, 4, 64], F32, 'oacc')
        ot = [kb.sb(pes, [128, D], BF16, 'ot') for _ in range(2)]
        psS = [kb.ps(pes, [128, 512], F32, 'psS') for _ in range(2)]
        pvb = [kb.ps(pes, [128, 512], F32, 'pvb') for _ in range(2)]
        pimp = kb.ps(pes, [128, 512], F32, 'pimp')
        pidx = [kb.ps(pes, [128, 512], F32, 'pidx') for _ in range(2)]
        cnt = {'s': 0, 'v': 0, 'x': 0}

        def attn_block(mask_lhsT, mask_r, kT_ap, k_r, q_ap, q_r, v_aps, pv, extra=None):
            pS = psS[cnt['s'] % 2]
            pt = pT[cnt['s'] % 3]
            cnt['s'] += 1
            if mask_lhsT is not None:
                kb.mm(pS[:, :], mask_lhsT, i4[:], True, False, [i4] + mask_r, [pS])
                kb.mm(pS[:, :], kT_ap, q_ap, False, True, k_r + q_r, [pS])
            else:
                kb.mm(pS[:, :], kT_ap, q_ap, True, True, k_r + q_r, [pS])
            kb.act(pt[:], pS[:, :], AF.Exp, [pS], [pt], scale=0.125)
            for hh in range(4):
                kb.mm(pv[:, hh * 65:hh * 65 + 65], pt[:, hh * 128:(hh + 1) * 128], v_aps[0], False, False, [pt] + v_aps[1], [pv])
                if extra is not None:
                    kb.mm(extra[0][:, hh * 64:hh * 64 + 64], pt[:, hh * 128:(hh + 1) * 128], extra[1], False, False,
                          [pt] + v_aps[1], [extra[0]])

        def new_pv():
            pv = pvb[cnt['v'] % 2]
            cnt['v'] += 1
            kb.mm(pv[:, 0:260], zer[:, 0:128], zer[:, 0:260], True, False, [zer], [pv])
            return pv

        def get_rden(pv):
            dv = pv[:, 0:260].rearrange("p (h c) -> p h c", c=65)[:, :, 64]
            kb.ts('dve', rden[:, 0:4], dv, 1e-30, ALU.max, [pv], [rden])
            kb.recip(rden[:, 0:4], rden[:, 0:4], [rden], [rden])

        for i in range(NT):
            N = (i + 1) * 128
            cols = slice(i * 128, (i + 1) * 128)
            rows_t = slice(i * 128, (i + 1) * 128)
            q_ = qs[i % 2]
            tm = tmt[i % 2]
            cm = cmt[i % 2]
            fb = fbt[i % 2]
            sc = sc2[i % 2]
            mb = mb2[i % 2]
            o_ = ot[i % 2]
            Gq = i // 4
            kb.dma(q_[:, 0:4, :], dram['qk'][0:4, :, cols].rearrange("b p t -> p b t"), [kb.db('qk', b_, Gq) for b_ in range(4)], [q_])
            kb.dma(q_[:, 4:6, :], dram['qk'][5:7, :, cols].rearrange("b p t -> p b t"), [kb.db('qk', b_, Gq) for b_ in (5, 6)], [q_])
            kb.dma(q_[:, 6:10, :], dram['qk'][8:12, :, cols].rearrange("b p t -> p b t"), [kb.db('qk', b_, Gq) for b_ in range(8, 12)], [q_])
            kb.dma(tm[:], dram['tm32'][rows_t, :], [kb.db('tm32', i)], [tm])
            kb.dma(cm[:], dram['cmask'][rows_t, :], [], [cm])
            kb.dma(fb[:], dram['fbias'][rows_t, :], [], [fb])
            ki = kres['ki']
            for ck in range((N + 511) // 512):
                c0 = ck * 512
                c1 = min(N, c0 + 512)
                w_ = c1 - c0
                for hi in range(4):
                    rows = slice((hi // 2) * 64, (hi // 2) * 64 + 64)
                    blk = 4 + (hi % 2)
                    pI = pidx[cnt['x'] % 2]
                    rt = rtmp[cnt['x'] % 2]
                    cnt['x'] += 1
                    kb.mm(pI[:, 0:w_], q_[rows, blk, :], ki[rows, c0:c1], True, True, [q_, ki], [pI])
                    kb.act(rt[:, 0:w_], pI[:, 0:w_], AF.Relu, [pI], [rt])
                    if hi == 0:
                        kb.ts('pool', sc[:, c0:c1], rt[:, 0:w_], tm[:, 0:1], ALU.mult, [rt, tm], [sc], s2=0.0, op1=ALU.add)
                    else:
                        kb.ts('pool', rt[:, 0:w_], rt[:, 0:w_], tm[:, hi:hi + 1], ALU.mult, [rt, tm], [rt], s2=0.0, op1=ALU.add)
                        kb.tt('pool', sc[:, c0:c1], sc[:, c0:c1], rt[:, 0:w_], ALU.add, [rt, sc], [sc])
            kb.tt('pool', sc[:, cols], sc[:, cols], negm[:], ALU.add, [sc, negm], [sc])
            if i >= 2:
                kb.P.op('dve', lambda e, a=m8[:], b=sc[:, 0:N]: e.max(out=a, in_=b), _bufs([sc]), _bufs([m8]))
                kb.P.op('dve', lambda e, a=wk[:, 0:N], b=m8[:], c=sc[:, 0:N]: e.match_replace(out=a, in_to_replace=b, in_values=c, imm_value=-1e30),
                        _bufs([sc, m8]), _bufs([wk]))
                for r_ in range(1, 32):
                    kb.P.op('dve', lambda e, a=m8[:], b=wk[:, 0:N]: e.max(out=a, in_=b), _bufs([wk]), _bufs([m8]))
                    if r_ < 31:
                        kb.P.op('dve', lambda e, a=wk[:, 0:N], b=m8[:]: e.match_replace(out=a, in_to_replace=b, in_values=a, imm_value=-1e30),
                                _bufs([wk, m8]), _bufs([wk]))
                kb.ts('dve', mb[:, 0:N], sc[:, 0:N], m8[:, 7:8], ALU.is_lt, [sc, m8], [mb], s2=NEG, op1=ALU.mult)
            else:
                kb.ts('dve', mb[:, 0:N], sc[:, 0:N], -1e29, ALU.is_lt, [sc], [mb], s2=NEG, op1=ALU.mult)
            ka = kres['ka']
            for grp in range(2):
                rows = slice(grp * 64, grp * 64 + 64)
                pv = new_pv()
                for j in range(i + 1):
                    attn_block(mb[:, j * 128:(j + 1) * 128], [mb], ka[rows, j * 128:(j + 1) * 128], [ka],
                               q_[rows, 0:4, :], [q_], (vt[:, j, 0, 0:65], [vt]), pv)
                get_rden(pv)
                for hh in range(4):
                    hd = grp * 4 + hh
                    kb.act(o_[:, hd * 64:(hd + 1) * 64], pv[:, hh * 65:hh * 65 + 64], AF.Copy, [pv, rden], [o_], scale=rden[:, hh:hh + 1])
            gates = tm[:, 4:28].rearrange("p (g h b) -> p g h b", g=2, h=4)
            nchk = 1 if i < 16 else 2
            for g in range(2):
                rows = slice(g * 64, g * 64 + 64)
                qg = q_[rows, 6:10, :]
                pv = new_pv()
                kb.mm(pimp[:, 0:256], zer[:, 0:128], zer[:, 0:256], True, False, [zer], [pimp])
                for nch in range(nchk):
                    attn_block(cm[:, nch * 128:(nch + 1) * 128], [cm], kcT[rows, nch * 128:(nch + 1) * 128], [kcT],
                               qg, [q_], (vc[:, nch, g, 0:65], [vc]), pv, extra=(pimp, vc[:, nch, g, 65:129]))
                get_rden(pv)
                for hh in range(4):
                    if hh == 0:
                        kb.ts('dve', imp[:], pimp[:, 0:64], rden[:, 0:1], ALU.mult, [pimp, rden], [imp])
                    else:
                        kb.stt('dve', imp[:], pimp[:, hh * 64:(hh + 1) * 64], rden[:, hh:hh + 1], imp[:], ALU.mult, ALU.add, [pimp, rden, imp], [imp])
                kb.tt('dve', coef[:, 0:4], rden[:, 0:4], gates[:, g, :, 0], ALU.mult, [rden, tm], [coef])
                for hh in range(4):
                    kb.ts('dve', oacc[:, hh, :], pv[:, hh * 65:hh * 65 + 64], coef[:, hh:hh + 1], ALU.mult, [pv, coef], [oacc])
                kb.tt('dve', scs[:], imp[:], fb[:], ALU.add, [imp, fb], [scs])
                kb.P.op('dve', lambda e, a=m8b[:], b=scs[:]: e.max(out=a, in_=b), _bufs([scs]), _bufs([m8b]))
                kb.P.op('dve', lambda e, a=scw[:], b=m8b[:], c=scs[:]: e.match_replace(out=a, in_to_replace=b, in_values=c, imm_value=-3e9),
                        _bufs([scs, m8b]), _bufs([scw]))
                kb.P.op('dve', lambda e, a=m8b[:], b=scw[:]: e.max(out=a, in_=b), _bufs([scw]), _bufs([m8b]))
                kb.ts('dve', thr[:, 0:1], m8b[:, 7:8], -1e8, ALU.max, [m8b], [thr])
                kb.ts('dve', selb[:], scs[:], thr[:, 0:1], ALU.is_lt, [scs, thr], [selb], s2=NEG, op1=ALU.mult)
                ms = msel[g]
                nb = N // 64
                kb.cp('pool', ms[:, 0:N].rearrange("p (m s) -> p m s", s=64), selb[:, 0:nb].unsqueeze(2).broadcast_to([128, nb, 64]), [selb], [ms])
                kb.tt('pool', ms[:, cols], ms[:, cols], tri[:], ALU.add, [ms, tri], [ms])
                ks = kres['ks']
                pv = new_pv()
                for j in range(i + 1):
                    attn_block(ms[:, j * 128:(j + 1) * 128], [ms], ks[rows, j * 128:(j + 1) * 128], [ks],
                               qg, [q_], (vt[:, j, 1 + g, 0:65], [vt]), pv)
                get_rden(pv)
                kb.tt('dve', coef[:, 0:4], rden[:, 0:4], gates[:, g, :, 1], ALU.mult, [rden, tm], [coef])
                for hh in range(4):
                    kb.stt('dve', oacc[:, hh, :], pv[:, hh * 65:hh * 65 + 64], coef[:, hh:hh + 1], oacc[:, hh, :], ALU.mult, ALU.add, [pv, coef, oacc], [oacc])
                kw_ = kres['kw']
                pv = new_pv()
                for j in range(max(0, i - 4), i + 1):
                    if j == i:
                        ml, mr = tri[:], [tri]
                    elif j == i - 4:
                        ml, mr = wfar[:], [wfar]
                    else:
                        ml, mr = None, []
                    attn_block(ml, mr, kw_[rows, j * 128:(j + 1) * 128], [kw_], qg, [q_], (vt[:, j, 3 + g, 0:65], [vt]), pv)
                get_rden(pv)
                kb.tt('dve', coef[:, 0:4], rden[:, 0:4], gates[:, g, :, 2], ALU.mult, [rden, tm], [coef])
                for hh in range(4):
                    kb.stt('dve', oacc[:, hh, :], pv[:, hh * 65:hh * 65 + 64], coef[:, hh:hh + 1], oacc[:, hh, :], ALU.mult, ALU.add, [pv, coef, oacc], [oacc])
                kb.cp('pool', o_[:, 512 + g * 256:512 + (g + 1) * 256], oacc[:].rearrange("p h d -> p (h d)"), [oacc], [o_])
            kb.dma(dram['os'][rows_t, :], o_[:], [o_], [kb.db('os', i // 4, hh_) for hh_ in range(8)])
        P.barrier()
        P.emit()


def phase_c(kb, dram, l, hsrc, ident, dst, final_norm):
    P = kb.P
    with ExitStack() as pes:
        wu = kb.sb(pes, [128, 8, DFF], BF16, 'wu')
        wd = kb.sb(pes, [128, 32, D], BF16, 'wd')
        wo = kb.sb(pes, [128, 8, D], BF16, 'wo')
        g_sb = kb.sb(pes, [128, 8], F32, 'g')
        kb.dma(g_sb[:], dram['mlp_g'][l], [], [g_sb])
        for c in range(8):
            kb.dma(wo[:, c, :], dram['w_out%d' % l][c * 128:(c + 1) * 128, :], [], [wo], q='pool')
        for c in range(8):
            kb.dma(wu[:, c, :], dram['w_up%d' % l][c * 128:(c + 1) * 128, :], [], [wu], q='pool')
        wdv = dram['w_down%d' % l].rearrange("(c p) n -> p c n", p=128)
        for c in range(8):
            kb.dma(wd[:, 4 * c:4 * c + 4, :], wdv[:, 4 * c:4 * c + 4, :], [], [wd], q='pool')
        if final_norm:
            fg = kb.sb(pes, [128, D], F32, 'fg')
            kb.dma(fg[:], dram['final_g'][0:1, :].partition_broadcast(128), [], [fg])
        ht = [kb.sb(pes, [128, D], F32, 'ht') for _ in range(2)]
        ot = [kb.sb(pes, [128, D], BF16, 'ot') for _ in range(2)]
        oT = kb.sb(pes, [128, 8, 128], BF16, 'oT')
        junk = kb.sb(pes, [128, D], BF16, 'junk')
        ss = kb.sb(pes, [128, 1], F32, 'ss')
        rstd = kb.sb(pes, [128, 1], F32, 'rstd')
        xn = kb.sb(pes, [128, D], BF16, 'xn')
        uT = kb.sb(pes, [128, 8, 128], BF16, 'uT')
        rl = [kb.sb(pes, [128, 512], F32, 'rl') for _ in range(2)]
        aT = kb.sb(pes, [128, 32, 128], BF16, 'aT')
        pst = [kb.ps(pes, [128, 1024], BF16, 'pst') for _ in range(2)]
        pu = [kb.ps(pes, [128, 512], F32, 'pu') for _ in range(2)]
        pd = [kb.ps(pes, [128, 512], F32, 'pd') for _ in range(2)]
        for i in range(NT):
            h_ = ht[i % 2]
            o_ = ot[i % 2]
            rows = slice(i * 128, (i + 1) * 128)
            kb.dma(h_[:], hsrc[rows, :], [kb.db('h', i)], [h_])
            kb.dma(o_[:], dram['os'][rows, :], [kb.db('os', i // 4, hh) for hh in range(8)], [o_])
            for c in range(8):
                kb.tr(pst[0][:, c * 128:(c + 1) * 128], o_[:, c * 128:(c + 1) * 128], ident[:], [o_, ident], [pst[0]])
            kb.cp('act', oT[:].rearrange("p c t -> p (c t)"), pst[0][:, :], [pst[0]], [oT])
            for half in range(2):
                for c in range(8):
                    kb.mm(pd[half][:, :], oT[:, c, :], wo[:, c, half * 512:(half + 1) * 512], c == 0, c == 7, [oT, wo], [pd[half]])
                kb.tt('dve', h_[:, half * 512:(half + 1) * 512], pd[half][:, :], h_[:, half * 512:(half + 1) * 512],
                      ALU.add, [pd[half], h_], [h_])
            kb.rms_rstd(h_, junk, ss, rstd, D)
            kb.ts('dve', xn[:], h_[:], rstd[:, 0:1], ALU.mult, [h_, rstd], [xn])
            for c in range(8):
                kb.tr(pst[1][:, c * 128:(c + 1) * 128], xn[:, c * 128:(c + 1) * 128], ident[:], [xn, ident], [pst[1]])
            kb.tt('dve', uT[:], pst[1][:].rearrange("p (c t) -> p c t", c=8),
                  g_sb[:].unsqueeze(2).broadcast_to([128, 8, 128]), ALU.mult, [pst[1], g_sb], [uT])
            for fg4 in range(8):
                put = pu[fg4 % 2]
                for f_ in range(4):
                    fc = fg4 * 4 + f_
                    for c in range(8):
                        kb.mm(put[:, f_ * 128:(f_ + 1) * 128], wu[:, c, fc * 128:(fc + 1) * 128], uT[:, c, :],
                              c == 0, c == 7, [wu, uT], [put])
                r_ = rl[fg4 % 2]
                kb.act(r_[:], put[:, :], AF.Relu, [put], [r_])
                kb.tt('pool', aT[:, fg4 * 4:fg4 * 4 + 4, :].rearrange("p c t -> p (c t)"), r_[:], r_[:], ALU.mult, [r_], [aT])
            for half in range(2):
                for fc in range(32):
                    kb.mm(pd[half][:, :], aT[:, fc, :], wd[:, fc, half * 512:(half + 1) * 512], fc == 0, fc == 31, [aT, wd], [pd[half]])
                kb.tt('dve', h_[:, half * 512:(half + 1) * 512], pd[half][:, :], h_[:, half * 512:(half + 1) * 512],
                      ALU.add, [pd[half], h_], [h_])
            if final_norm:
                kb.rms_rstd(h_, junk, ss, rstd, D)
                kb.stt('dve', h_[:], h_[:], rstd[:, 0:1], fg[:], ALU.mult, ALU.mult, [h_, rstd, fg], [h_])
            kb.dma(dst[rows, :], h_[:], [h_], [kb.db('h', i) if not final_norm else kb.db('y', i)])
        P.barrier()
        P.emit()


def _bf(a):
    return np.asarray(a, dtype=np.float32).astype(ml_dtypes.bfloat16)


def host_consts():
    c = {}
    c['ident'] = _bf(np.eye(128))
    c['i4'] = _bf(np.tile(np.eye(128), (1, 4)))
    t = np.arange(128)[:, None]
    s = np.arange(128)[None, :]
    c['tri'] = _bf(np.where(s <= t, 0.0, NEG))
    inv = 1.0 / (10000.0 ** (np.arange(0, 64, 2, dtype=np.float32) / 64))
    ang = np.arange(T, dtype=np.float32)[None, :] * inv[:, None].astype(np.float32)
    cos = np.cos(ang).astype(np.float32)
    sin = np.sin(ang).astype(np.float32)
    c['winfar'] = _bf(np.where(s > t, 0.0, NEG))
    c['negm'] = np.where(s <= t, 0.0, -1e30).astype(np.float32)
    tq = np.arange(T)[:, None]
    nn = np.arange(256)[None, :]
    c['cmask'] = _bf(np.where((16 * nn + 31 <= tq) & (nn < 255), 0.0, NEG))
    mm_ = np.arange(64)[None, :]
    cur = tq // 64
    fbv = np.where(mm_ * 64 <= tq, 0.0, -1e9)
    fbv = np.where(mm_ == cur - 1, 1e9, fbv)
    fbv = np.where(mm_ == cur, 2e9, fbv)
    fbv = np.where(mm_ == 0, 3e9, fbv)
    c['fbias'] = fbv.astype(np.float32)
    n_ = np.arange(256)[:, None]
    c0_ = n_ * 16
    ov = ((c0_ < (mm_ + 1) * 64) & (c0_ + 32 > mm_ * 64) & (n_ < 255))
    c['ovl'] = _bf(ov.astype(np.float32))
    c['ropeC'] = np.ascontiguousarray(np.concatenate([cos, cos, cos, cos], 0))
    c['ropeS'] = np.ascontiguousarray(np.concatenate([-sin, sin, -sin, sin], 0))
    return c


def swap_cols(w):
    d, n = w.shape
    return np.ascontiguousarray(w.reshape(d, n // 64, 2, 32)[:, :, ::-1, :].reshape(d, n))


def even_cols():
    qa0, ka0, va0, qi0, ki0, wi0, qb0, kv0, gb0 = 0, 512, 576, 640, 896, 960, 964, 1476, 2244

    def hd(base, h):
        return list(range(base + h * 64, base + (h + 1) * 64))

    def kvh(s6, g):
        return hd(kv0, s6 * 2 + g)
    blocks = []
    for r in range(4):
        blocks.append(hd(qa0, r) + hd(qa0, 4 + r))
    blocks.append(hd(ka0, 0) * 2)
    for r in range(2):
        blocks.append(hd(qi0, r) + hd(qi0, 2 + r))
    blocks.append(hd(ki0, 0) * 2)
    for r in range(4):
        blocks.append(hd(qb0, r) + hd(qb0, 4 + r))
    blocks.append(kvh(0, 0) + kvh(0, 1))
    blocks.append(kvh(2, 0) + kvh(2, 1))
    blocks.append(kvh(4, 0) + kvh(4, 1))
    blocks.append(kvh(1, 0) + kvh(1, 1))
    fm = [c for b in blocks for c in b]
    tmc = hd(va0, 0) + kvh(3, 0) + kvh(3, 1) + kvh(5, 0) + kvh(5, 1) + list(range(wi0, wi0 + 4)) + list(range(gb0, gb0 + 24))
    assert len(fm) == 2048 and len(tmc) == 348
    return fm, tmc


def gain_layout(g):
    return np.ascontiguousarray(g.reshape(g.shape[0], 8, 128).transpose(0, 2, 1))


def host_inputs(inputs, layers):
    m = dict(host_consts())
    m['mix_g'] = gain_layout(np.asarray(inputs['mix_norm_g'], np.float32))
    m['mlp_g'] = gain_layout(np.asarray(inputs['mlp_norm_g'], np.float32))
    m['final_g'] = np.asarray(inputs['final_norm_g'], np.float32).reshape(1, D)
    for l in layers:
        m['w_up%d' % l] = np.ascontiguousarray(inputs['mlp_w_up'][l])
        m['w_down%d' % l] = np.ascontiguousarray(inputs['mlp_w_down'][l])
        if l % 2 == 0:
            e = l // 2
            w = np.asarray(inputs['even_w_in'][e], np.float32)
            fm, tmc = even_cols()
            wqk = np.ascontiguousarray(w[:, fm])
            m['wqk%d' % l] = wqk
            m['wqks%d' % l] = swap_cols(np.ascontiguousarray(wqk[:, 0:1920]))
            m['wv%d' % l] = np.ascontiguousarray(w[:, tmc])
            m['w1_%d' % l] = np.ascontiguousarray(inputs['even_cmp_w1'][e], dtype=np.float32)
            m['w2_%d' % l] = np.ascontiguousarray(inputs['even_cmp_w2'][e], dtype=np.float32)
            pe = np.asarray(inputs['even_cmp_pe'][e], np.float32)
            peT = pe.transpose(0, 2, 1)
            m['peT%d' % l] = np.ascontiguousarray(np.concatenate([peT, peT], 1))
            m['w_out%d' % l] = np.ascontiguousarray(inputs['even_w_out'][e])
        if l % 2 == 1:
            o = l // 2
            w = np.asarray(inputs['odd_w_in'][o], np.float32)
            wqk = np.ascontiguousarray(w[:, 0:2048])
            m['wqk%d' % l] = wqk
            m['wqks%d' % l] = swap_cols(wqk)
            m['wv%d' % l] = np.ascontiguousarray(w[:, 2048:3072])
            m['lam%d' % l] = np.asarray(inputs['odd_lambda'][o], np.float32).reshape(1, 256)
            m['subg%d' % l] = np.asarray(inputs['odd_subln_g'][o], np.float32).reshape(1, 128)
            m['w_out%d' % l] = np.ascontiguousarray(inputs['odd_w_out'][o])
    return m


def kernel(**inputs):
    layers = [0, 1, 2, 3]
    inputs = {k: np.asarray(v) for k, v in inputs.items()}
    nc = build_program(dict(layers=layers, final_norm=True))
    shared = host_inputs(inputs, layers)
    in_maps = []
    for c in range(N_CORES):
        m = dict(shared)
        m['x'] = np.ascontiguousarray(inputs['x'][c % 4])
        in_maps.append(m)
    res = run_bass_kernel_spmd(nc, in_maps, core_ids=list(range(N_CORES)))
    return np.stack([res.results[b]['y'] for b in range(4)], 0).astype(np.float32)
```

```python
import math
import numpy as np
from contextlib import ExitStack
import ml_dtypes
import concourse.bass as bass
import concourse.mybir as mybir
from concourse.bass_utils import run_bass_kernel_spmd

F32 = mybir.dt.float32
BF16 = mybir.dt.bfloat16
AF = mybir.ActivationFunctionType
ALU = mybir.AluOpType
AX = mybir.AxisListType

T = 4096
D = 1024
NT = 32
DFF = 4096
EPS = 1e-6
NEG = -30000.0
N_CORES = 8


class Buf:
    __slots__ = ('name', 'lw', 'rd')

    def __init__(self, name=''):
        self.name = name
        self.lw = None
        self.rd = {}


class Tl:
    def __init__(self, t, name=''):
        self.t = t
        self.b = Buf(name)

    def __getitem__(self, k):
        return self.t[k]


def _bufs(lst):
    out = []
    for x in lst:
        if x is None:
            continue
        out.append(x.b if isinstance(x, Tl) else x)
    return out


class Prog:
    CE = ('pe', 'act', 'dve', 'pool')

    def __init__(self, nc, es, n_dma_sems=12):
        self.nc = nc
        self.ops = {e: [] for e in ('pe', 'act', 'dve', 'pool', 'sp')}
        self.sems = {}
        for e in self.CE:
            self.sems[e] = es.enter_context(nc.semaphore('s_' + e))
        self.cnt = {e: 0 for e in self.CE}
        self.dq = {}
        for q in ('sp', 'pool', 'act'):
            n = n_dma_sems if q == 'sp' else 6
            ss = []
            for i in range(n):
                k = 'd_%s_%d' % (q, i)
                self.sems[k] = es.enter_context(nc.semaphore(k))
                self.cnt[k] = 0
                ss.append(k)
            self.dq[q] = [ss, 0]
        self.waited = {e: {} for e in self.ops}
        self.same_engine_sync = {'pe': False, 'act': True, 'dve': True, 'pool': True, 'sp': True}
        self.total = 0

    def _deps(self, reads, writes):
        deps = {}
        for b in reads:
            if b.lw is not None:
                k, v = b.lw
                if deps.get(k, 0) < v:
                    deps[k] = v
        for b in writes:
            if b.lw is not None:
                k, v = b.lw
                if deps.get(k, 0) < v:
                    deps[k] = v
            for k, v in b.rd.items():
                if deps.get(k, 0) < v:
                    deps[k] = v
        return deps

    def _mark(self, tok, reads, writes):
        k, v = tok
        for b in reads:
            if b.rd.get(k, 0) < v:
                b.rd[k] = v
        for b in writes:
            b.lw = tok
            b.rd = {}

    def op(self, eng, fn, reads=(), writes=()):
        reads = _bufs(reads)
        writes = _bufs(writes)
        deps = self._deps(reads, writes)
        waits = []
        wd = self.waited[eng]
        for k, v in deps.items():
            if k == eng and not self.same_engine_sync[eng]:
                continue
            if wd.get(k, 0) >= v:
                continue
            wd[k] = v
            waits.append((k, v))
        self.cnt[eng] += 1
        tok = (eng, self.cnt[eng])
        self.ops[eng].append((waits, fn, eng, 1))
        self._mark(tok, reads, writes)
        self.total += 1

    def dma(self, fn, reads=(), writes=(), q='sp', inc=16):
        reads = _bufs(reads)
        writes = _bufs(writes)
        ss, i = self.dq[q]
        k = ss[i % len(ss)]
        self.dq[q][1] = i + 1
        deps = self._deps(reads, writes)
        if self.cnt[k] > 0 and deps.get(k, 0) < self.cnt[k]:
            deps[k] = self.cnt[k]
        waits = []
        wd = self.waited[q]
        for kk, v in deps.items():
            if wd.get(kk, 0) >= v:
                continue
            wd[kk] = v
            waits.append((kk, v))
        self.cnt[k] += inc
        tok = (k, self.cnt[k])
        self.ops[q].append((waits, fn, k, inc))
        self._mark(tok, reads, writes)
        self.total += 1

    def barrier(self):
        for e in self.ops:
            waits = []
            wd = self.waited[e]
            for k, v in self.cnt.items():
                if v > 0 and wd.get(k, 0) < v:
                    wd[k] = v
                    waits.append((k, v))
            if waits:
                self.ops[e].append((waits, None, None, 0))

    def emit(self):
        nc = self.nc
        sems = self.sems

        def mk(name):
            lst = self.ops[name]

            def body(e):
                for waits, fn, sk, inc in lst:
                    for k, v in waits:
                        e.wait_ge(sems[k], v)
                    if fn is not None:
                        fn(e).then_inc(sems[sk], inc)
            return body

        with nc.Block() as block:
            block.tensor(mk('pe'))
            block.scalar(mk('act'))
            block.vector(mk('dve'))
            block.gpsimd(mk('pool'))
            block.sync(mk('sp'))
        self.ops = {e: [] for e in self.ops}


class KB:
    def __init__(self, nc, es):
        self.nc = nc
        self.P = Prog(nc, es)
        self.dbufs = {}
        self.uid = 0

    def sb(self, es, shape, dt, name=None):
        self.uid += 1
        name = (name or 't') + '_%d' % self.uid
        return Tl(es.enter_context(self.nc.sbuf_tensor(name, list(shape), dt)), name)

    def ps(self, es, shape, dt, name=None):
        self.uid += 1
        name = (name or 'p') + '_%d' % self.uid
        return Tl(es.enter_context(self.nc.psum_tensor(name, list(shape), dt)), name)

    def db(self, *key):
        b = self.dbufs.get(key)
        if b is None:
            b = Buf(str(key))
            self.dbufs[key] = b
        return b

    def mm(self, out, lhsT, rhs, start, stop, r, w):
        self.P.op('pe', lambda e: e.matmul(out, lhsT=lhsT, rhs=rhs, start=start, stop=stop,
                                           skip_group_check=True), r, w)

    def tr(self, out, in_, ident, r, w):
        self.P.op('pe', lambda e: e.transpose(out=out, in_=in_, identity=ident), r, w)

    def act(self, out, in_, func, r, w, scale=None, bias=None, accum_out=None, eng='act'):
        kw = {}
        if scale is not None:
            kw['scale'] = scale
        if bias is not None:
            kw['bias'] = bias
        if accum_out is not None:
            kw['accum_out'] = accum_out
        self.P.op('act', lambda e: e.activation(out=out, in_=in_, func=func, **kw), r, w)

    def ts(self, eng, out, in0, s1, op0, r, w, s2=None, op1=None, accum_out=None):
        kw = {}
        if op1 is not None:
            kw['op1'] = op1
        if accum_out is not None:
            kw['accum_out'] = accum_out
        self.P.op(eng, lambda e: e.tensor_scalar(out=out, in0=in0, scalar1=s1, scalar2=s2, op0=op0, **kw), r, w)

    def tt(self, eng, out, in0, in1, op, r, w):
        self.P.op(eng, lambda e: e.tensor_tensor(out=out, in0=in0, in1=in1, op=op), r, w)

    def stt(self, eng, out, in0, scalar, in1, op0, op1, r, w):
        self.P.op(eng, lambda e: e.scalar_tensor_tensor(out=out, in0=in0, scalar=scalar, in1=in1, op0=op0, op1=op1), r, w)

    def cp(self, eng, out, in_, r, w):
        if eng == 'act':
            self.P.op('act', lambda e: e.copy(out=out, in_=in_), r, w)
        else:
            self.P.op(eng, lambda e: e.tensor_copy(out=out, in_=in_), r, w)

    def memset(self, eng, ap, val, w):
        self.P.op(eng, lambda e: e.memset(ap, val), (), w)

    def recip(self, out, in_, r, w):
        self.P.op('dve', lambda e: e.reciprocal(out=out, in_=in_), r, w)

    def dma(self, out, in_, r, w, q='sp'):
        self.P.dma(lambda e: e.dma_start(out=out, in_=in_), r, w, q=q)

    def rms_rstd(self, h, junk, ss, rstd, width):
        self.act(junk[:, 0:width], h[:, 0:width], AF.Square, [h], [junk, ss], accum_out=ss[:, 0:1])
        self.ts('dve', ss[:, 0:1], ss[:, 0:1], 1.0 / width, ALU.mult, [ss], [ss], s2=EPS, op1=ALU.add)
        self.act(ss[:, 0:1], ss[:, 0:1], AF.Sqrt, [ss], [ss])
        self.recip(rstd[:, 0:1], ss[:, 0:1], [ss], [rstd])


def build_program(cfg):
    nc = bass.Bass("TRN2", target_bir_lowering=False)
    layers = cfg['layers']
    dram = {}

    def din(name, shape, dt=F32):
        dram[name] = nc.dram_tensor(name, list(shape), dt, kind="ExternalInput").ap()
        return dram[name]

    def dscr(name, shape, dt):
        dram[name] = nc.dram_tensor(name, list(shape), dt, kind="Internal").ap()
        return dram[name]

    x = din('x', [T, D])
    y = nc.dram_tensor('y', [T, D], F32, kind="ExternalOutput").ap()
    din('mix_g', [4, 128, 8])
    din('mlp_g', [4, 128, 8])
    din('final_g', [1, D])
    din('ident', [128, 128], BF16)
    din('i4', [128, 512], BF16)
    din('tri', [128, 128], BF16)
    din('ropeC', [128, T])
    din('ropeS', [128, T])
    din('winfar', [128, 128], BF16)
    din('negm', [128, 128])
    din('cmask', [T, 256], BF16)
    din('fbias', [T, 64])
    din('ovl', [256, 64], BF16)
    din('pw', [128, 32])
    for l in layers:
        din('w_up%d' % l, [D, DFF])
        din('w_down%d' % l, [DFF, D])
        din('w_out%d' % l, [D, D])
        if l % 2 == 0:
            din('wqk%d' % l, [D, 2048])
            din('wqks%d' % l, [D, 1920])
            din('wv%d' % l, [D, 348])
            din('w1_%d' % l, [2, 2048, 256])
            din('w2_%d' % l, [2, 256, 64])
            din('peT%d' % l, [2, 128, 32])
        if l % 2 == 1:
            din('wqk%d' % l, [D, 2048])
            din('wqks%d' % l, [D, 2048])
            din('wv%d' % l, [D, 1024])
            din('lam%d' % l, [1, 256])
            din('subg%d' % l, [1, 128])
    hs = dscr('hs', [T, D], F32)
    qk = dscr('qk', [20, 128, T], BF16)
    vs = dscr('vs', [T, 1024], BF16)
    os_ = dscr('os', [T, D], BF16)
    dscr('tm32', [T, 32], F32)
    dscr('kcs', [128, 256], BF16)
    dscr('vcs', [128, 256], BF16)

    with ExitStack() as es:
        kb = KB(nc, es)
        P = kb.P
        ident = kb.sb(es, [128, 128], BF16, 'ident')
        kb.dma(ident[:], dram['ident'][:, :], [], [ident])
        hsrc = x
        for li, l in enumerate(layers):
            if l % 2 == 1:
                phase_a(kb, dram, l, hsrc, ident)
                phase_b_odd(kb, dram, l, ident)
            else:
                phase_a(kb, dram, l, hsrc, ident)
                phase_b0_even(kb, dram, l)
                phase_b_even(kb, dram, l)
            last = (li == len(layers) - 1)
            phase_c(kb, dram, l, hsrc, ident, y if last else hs, last and cfg.get('final_norm', True))
            hsrc = hs
    return nc


def load_norm_transpose(kb, pes, h_ap, hb, g_sb, ident, ht, junk, ss, rstd, xn, pst, uT_ap, uT_tl):
    kb.dma(ht[:], h_ap, [hb], [ht])
    kb.rms_rstd(ht, junk, ss, rstd, D)
    kb.ts('dve', xn[:], ht[:], rstd[:, 0:1], ALU.mult, [ht, rstd], [xn])
    for c in range(8):
        kb.tr(pst[:, c * 128:(c + 1) * 128], xn[:, c * 128:(c + 1) * 128], ident[:], [xn, ident], [pst])
    kb.tt('dve', uT_ap, pst[:].rearrange("p (c t) -> p c t", c=8),
          g_sb[:].unsqueeze(2).broadcast_to([128, 8, 128]), ALU.mult, [pst, g_sb], [uT_tl])


def phase_a(kb, dram, l, hsrc, ident):
    P = kb.P
    even = (l % 2 == 0)
    nfm = 16
    nsw = 15 if even else 16
    ntm = 348 if even else 1024
    with ExitStack() as pes:
        wqk = kb.sb(pes, [128, 8, nfm * 128], BF16, 'wqk')
        wqks = kb.sb(pes, [128, 8, nsw * 128], BF16, 'wqks')
        wv = kb.sb(pes, [128, 8, ntm], BF16, 'wv')
        g_sb = kb.sb(pes, [128, 8], F32, 'g')
        kb.dma(g_sb[:], dram['mix_g'][l], [], [g_sb])
        for c in range(8):
            kb.dma(wqk[:, c, :], dram['wqk%d' % l][c * 128:(c + 1) * 128, :], [], [wqk], q='pool')
            kb.dma(wqks[:, c, :], dram['wqks%d' % l][c * 128:(c + 1) * 128, :], [], [wqks], q='pool')
            kb.dma(wv[:, c, :], dram['wv%d' % l][c * 128:(c + 1) * 128, :], [], [wv], q='pool')
        ht = [kb.sb(pes, [128, D], F32, 'ht') for _ in range(2)]
        junk = kb.sb(pes, [128, D], BF16, 'junk')
        ss = [kb.sb(pes, [128, 1], F32, 'ss') for _ in range(2)]
        rstd = [kb.sb(pes, [128, 1], F32, 'rstd') for _ in range(2)]
        xn = [kb.sb(pes, [128, D], BF16, 'xn') for _ in range(2)]
        uT = [kb.sb(pes, [128, 8, 512], BF16, 'uT') for _ in range(2)]
        cc = [kb.sb(pes, [128, 512], F32, 'cc') for _ in range(2)]
        sn = [kb.sb(pes, [128, 512], F32, 'sn') for _ in range(2)]
        t1 = [kb.sb(pes, [128, 512], F32, 't1') for _ in range(2)]
        t2 = [kb.sb(pes, [128, 512], F32, 't2') for _ in range(2)]
        stg = [kb.sb(pes, [128, 512], BF16, 'stg') for _ in range(3)]
        vst = [kb.sb(pes, [128, 1024], BF16, 'vst') for _ in range(2)]
        tmf = [kb.sb(pes, [128, 32], F32, 'tmf') for _ in range(2)]
        for t_ in tmf:
            kb.memset('pool', t_[:], 0.0, [t_])
        pst = [kb.ps(pes, [128, 1024], BF16, 'pst') for _ in range(2)]
        pa = [kb.ps(pes, [128, 512], F32, 'pa') for _ in range(2)]
        pb = [kb.ps(pes, [128, 512], F32, 'pb') for _ in range(2)]
        pv = [kb.ps(pes, [128, 512], F32, 'pv') for _ in range(2)]
        n = 0
        for G in range(8):
            u = uT[G % 2]
            kb.dma(cc[G % 2][:], dram['ropeC'][:, G * 512:(G + 1) * 512], [], [cc[G % 2]])
            kb.dma(sn[G % 2][:], dram['ropeS'][:, G * 512:(G + 1) * 512], [], [sn[G % 2]])
            for tt_ in range(4):
                i = G * 4 + tt_
                load_norm_transpose(kb, pes, hsrc[i * 128:(i + 1) * 128, :], kb.db('h', i), g_sb, ident,
                                    ht[i % 2], junk, ss[i % 2], rstd[i % 2], xn[i % 2], pst[i % 2],
                                    u[:, :, tt_ * 128:(tt_ + 1) * 128], u)
                us = u[:, :, tt_ * 128:(tt_ + 1) * 128]
                if not even:
                    for half in range(2):
                        pvt = pv[half]
                        for c in range(8):
                            kb.mm(pvt[:, :], u[:, c, tt_ * 128:(tt_ + 1) * 128], wv[:, c, half * 512:(half + 1) * 512],
                                  c == 0, c == 7, [u, wv], [pvt])
                        kb.cp('act', vst[i % 2][:, half * 512:(half + 1) * 512], pvt[:, :], [pvt], [vst[i % 2]])
                    kb.dma(dram['vs'][i * 128:(i + 1) * 128, :], vst[i % 2][:], [vst[i % 2]], [kb.db('vs', i)])
                else:
                    pvt = pv[i % 2]
                    for c in range(8):
                        kb.mm(pvt[:, 0:ntm], u[:, c, tt_ * 128:(tt_ + 1) * 128], wv[:, c, :], c == 0, c == 7, [u, wv], [pvt])
                    kb.cp('act', vst[i % 2][:, 0:320], pvt[:, 0:320], [pvt], [vst[i % 2]])
                    kb.cp('act', tmf[i % 2][:, 0:4], pvt[:, 320:324], [pvt], [tmf[i % 2]])
                    kb.act(tmf[i % 2][:, 4:28], pvt[:, 324:348], AF.Sigmoid, [pvt], [tmf[i % 2]])
                    kb.dma(dram['vs'][i * 128:(i + 1) * 128, 0:320], vst[i % 2][:, 0:320], [vst[i % 2]], [kb.db('vs', i)])
                    kb.dma(dram['tm32'][i * 128:(i + 1) * 128, :], tmf[i % 2][:, :], [tmf[i % 2]], [kb.db('tm32', i)])
            for rb in range(nfm):
                pat = pa[rb % 2]
                pbt = pb[rb % 2]
                for c in range(8):
                    kb.mm(pat[:, :], wqk[:, c, rb * 128:(rb + 1) * 128], u[:, c, :], c == 0, c == 7, [u, wqk], [pat])
                st = stg[n % 3]
                n += 1
                if rb < nsw:
                    for c in range(8):
                        kb.mm(pbt[:, :], wqks[:, c, rb * 128:(rb + 1) * 128], u[:, c, :], c == 0, c == 7, [u, wqks], [pbt])
                    a1 = t1[rb % 2]
                    a2 = t2[rb % 2]
                    kb.tt('dve', a1[:], pat[:, :], cc[G % 2][:], ALU.mult, [pat, cc[G % 2]], [a1])
                    kb.tt('dve', a2[:], pbt[:, :], sn[G % 2][:], ALU.mult, [pbt, sn[G % 2]], [a2])
                    kb.tt('pool', st[:], a1[:], a2[:], ALU.add, [a1, a2], [st])
                else:
                    kb.cp('act', st[:], pat[:, :], [pat], [st])
                kb.dma(dram['qk'][rb, :, G * 512:(G + 1) * 512], st[:], [st], [kb.db('qk', rb, G)])
        P.barrier()
        P.emit()


def phase_b_odd(kb, dram, l, ident):
    P = kb.P
    lambda_init = 0.8 - 0.6 * math.exp(-0.3 * l)
    with ExitStack() as pes:
        i4 = kb.sb(pes, [128, 512], BF16, 'i4')
        tri = kb.sb(pes, [128, 128], BF16, 'tri')
        zer = kb.sb(pes, [128, 512], BF16, 'zer')
        kb.dma(i4[:], dram['i4'][:, :], [], [i4])
        kb.dma(tri[:], dram['tri'][:, :], [], [tri])
        kb.memset('pool', zer[:], 0.0, [zer])
        lam = kb.sb(pes, [128, 256], F32, 'lam')
        lw = kb.sb(pes, [128, 128], F32, 'lw')
        lv = kb.sb(pes, [128, 4], F32, 'lv')
        subg = kb.sb(pes, [128, 128], F32, 'subg')
        kb.dma(lam[:], dram['lam%d' % l][0:1, :].partition_broadcast(128), [], [lam])
        kb.dma(subg[:], dram['subg%d' % l][0:1, :].partition_broadcast(128), [], [subg])
        kb.tt('dve', lw[:, 0:64], lam[:, 0:64], lam[:, 64:128], ALU.mult, [lam], [lw])
        kb.tt('dve', lw[:, 64:128], lam[:, 128:192], lam[:, 192:256], ALU.mult, [lam, lw], [lw])
        kb.P.op('dve', lambda e: e.reduce_sum(out=lv[:, 0:2], in_=lw[:].rearrange("p (a b) -> p a b", a=2), axis=AX.X), _bufs([lw]), _bufs([lv]))
        kb.act(lv[:, 0:2], lv[:, 0:2], AF.Exp, [lv], [lv])
        kb.tt('dve', lv[:, 2:3], lv[:, 1:2], lv[:, 0:1], ALU.subtract, [lv], [lv])
        kb.ts('dve', lv[:, 2:3], lv[:, 2:3], -lambda_init, ALU.add, [lv], [lv])
        kb.ts('dve', subg[:], subg[:], 1.0 - lambda_init, ALU.mult, [subg], [subg])

        kT = [kb.sb(pes, [128, T], BF16, 'kT') for _ in range(2)]
        vh = [kb.sb(pes, [128, NT, 130], BF16, 'vh') for _ in range(2)]
        for v_ in vh:
            kb.memset('pool', v_[:, :, 128:129], 1.0, [v_])
        qT = [kb.sb(pes, [128, 512], BF16, 'qT') for _ in range(2)]
        pT = [kb.sb(pes, [128, 512], BF16, 'pT') for _ in range(6)]
        oc = [kb.sb(pes, [128, 4, 128], F32, 'oc') for _ in range(2)]
        rd = [kb.sb(pes, [128, 4], F32, 'rd') for _ in range(2)]
        ao = kb.sb(pes, [128, 4, 128], F32, 'ao')
        junk = kb.sb(pes, [128, 128], F32, 'junk')
        s4 = kb.sb(pes, [128, 4], F32, 's4')
        ob = [kb.sb(pes, [128, 4, 128], BF16, 'ob') for _ in range(2)]
        psS = [kb.ps(pes, [128, 512], F32, 'psS') for _ in range(4)]
        pvb = [[kb.ps(pes, [128, 512], F32, 'pvb') for _ in range(2)] for _ in range(2)]
        ns = 0
        nq = 0
        for h in range(8):
            kt = kT[h % 2]
            vt = vh[h % 2]
            kb.dma(kt[:], dram['qk'][8 + h, :, :], [kb.db('qk', 8 + h, G) for G in range(8)], [kt])
            kb.dma(vt[:, :, 0:128], dram['vs'][:, h * 128:(h + 1) * 128].rearrange("(n p) d -> p n d", p=128),
                   [kb.db('vs', i) for i in range(NT)], [vt])
            for Q in range(8):
                qt = qT[nq % 2]
                nq += 1
                kb.dma(qt[:], dram['qk'][h, :, Q * 512:(Q + 1) * 512], [kb.db('qk', h, Q)], [qt])
                for c in range(2):
                    for bnk in pvb[c]:
                        kb.mm(bnk[:, 0:258], zer[:, 0:128], zer[:, 0:258], True, False, [zer], [bnk])
                pend = []
                for j in range(4 * Q + 4):
                    t0 = max(0, j - 4 * Q)
                    cur = []
                    for c in range(2):
                        lo = c * 64
                        hi = lo + 64
                        pS = psS[ns % 4]
                        pt = pT[ns % 6]
                        ns += 1
                        if j >= 4 * Q:
                            kb.mm(pS[:, t0 * 128:(t0 + 1) * 128], tri[:], i4[:, 0:128], True, False, [tri, i4], [pS])
                            kb.mm(pS[:, t0 * 128:512], kt[lo:hi, j * 128:(j + 1) * 128], qt[lo:hi, t0 * 128:512],
                                  False, True, [kt, qt], [pS])
                        else:
                            kb.mm(pS[:, :], kt[lo:hi, j * 128:(j + 1) * 128], qt[lo:hi, :], True, True, [kt, qt], [pS])
                        cur.append((c, pS, pt))
                    for c, pS, pt in cur:
                        kb.act(pt[:, t0 * 128:512], pS[:, t0 * 128:512], AF.Exp, [pS], [pt], scale=0.125)
                    for f in pend:
                        f()
                    pend = []
                    for c, pS, pt in cur:
                        def pvpart(t0=t0, pt=pt, j=j, pv2=pvb[c], vt=vt):
                            for t_ in range(t0, 4):
                                bnk = pv2[t_ // 2]
                                col = (t_ % 2) * 129
                                kb.mm(bnk[:, col:col + 129], pt[:, t_ * 128:(t_ + 1) * 128], vt[:, j, 0:129],
                                      False, False, [pt, vt], [bnk])
                        pend.append(pvpart)
                for f in pend:
                    f()
                pend = []
                for c in range(2):
                    pv2 = pvb[c]
                    for t_ in range(4):
                        bnk = pv2[t_ // 2]
                        col = (t_ % 2) * 129
                        kb.recip(rd[c][:, t_:t_ + 1], bnk[:, col + 128:col + 129], [bnk], [rd[c]])
                    if c == 1:
                        kb.ts('dve', rd[c][:], rd[c][:], lv[:, 2:3], ALU.mult, [rd[c], lv], [rd[c]])
                    for t_ in range(4):
                        bnk = pv2[t_ // 2]
                        col = (t_ % 2) * 129
                        kb.act(oc[c][:, t_, :], bnk[:, col:col + 128], AF.Copy, [bnk, rd[c]], [oc[c]], scale=rd[c][:, t_:t_ + 1])
                kb.tt('dve', ao[:], oc[0][:], oc[1][:], ALU.add, [oc[0], oc[1]], [ao])
                for t_ in range(4):
                    kb.act(junk[:], ao[:, t_, :], AF.Square, [ao], [junk, s4], accum_out=s4[:, t_:t_ + 1])
                kb.ts('dve', s4[:], s4[:], 1.0 / 128, ALU.mult, [s4], [s4], s2=EPS, op1=ALU.add)
                kb.act(s4[:], s4[:], AF.Sqrt, [s4], [s4])
                kb.recip(s4[:], s4[:], [s4], [s4])
                kb.tt('dve', ao[:], ao[:], s4[:].unsqueeze(2).broadcast_to([128, 4, 128]), ALU.mult, [ao, s4], [ao])
                o_ = ob[Q % 2]
                kb.tt('pool', o_[:], ao[:], subg[:].unsqueeze(1).broadcast_to([128, 4, 128]), ALU.mult, [ao, subg], [o_])
                kb.dma(dram['os'][Q * 512:(Q + 1) * 512, h * 128:(h + 1) * 128].rearrange("(n p) d -> p n d", p=128),
                       o_[:], [o_], [kb.db('os', Q, h)])
        P.barrier()
        P.emit()


def phase_b0_even(kb, dram, l):
    P = kb.P
    with ExitStack() as pes:
        w1 = [kb.sb(pes, [128, 32, 256], BF16, 'w1') for _ in range(2)]
        w2k = kb.sb(pes, [128, 2, 128], BF16, 'w2k')
        w2v = kb.sb(pes, [128, 2, 64], BF16, 'w2v')
        peT = kb.sb(pes, [128, 2, 32], F32, 'peT')
        srcs = [kb.sb(pes, [128, T], BF16, 'src') for _ in range(2)]
        xlo = [kb.sb(pes, [128, T], BF16, 'xlo') for _ in range(2)]
        xhi = [kb.sb(pes, [128, T], BF16, 'xhi') for _ in range(2)]
        hT = [kb.sb(pes, [128, 2, 2, 256], BF16, 'hT') for _ in range(2)]
        g0 = [kb.sb(pes, [128, 256], F32, 'g0') for _ in range(2)]
        g1 = [kb.sb(pes, [128, 256], F32, 'g1') for _ in range(2)]
        kcs = kb.sb(pes, [128, 256], BF16, 'kcs')
        vcs = kb.sb(pes, [128, 2, 2, 64], BF16, 'vcs')
        ph = [kb.ps(pes, [128, 512], F32, 'ph') for _ in range(2)]
        pk = [kb.ps(pes, [128, 512], F32, 'pk') for _ in range(2)]
        for kv in range(2):
            wv_ = dram['w1_%d' % l][kv].rearrange("(l d) f -> d l f", d=64)
            kb.dma(w1[kv][0:64, :, :], wv_, [], [w1[kv]], q='pool')
            kb.dma(w1[kv][64:128, :, :], wv_, [], [w1[kv]], q='pool')
            kb.dma(srcs[kv][:], dram['qk'][12 if kv == 0 else 15, :, :], [kb.db('qk', 12 if kv == 0 else 15, G) for G in range(8)], [srcs[kv]])
            kb.memset('pool', hT[kv][:, :, :, 255:256], 0.0, [hT[kv]])
        w2kv = dram['w2_%d' % l][0].rearrange("(c p) d -> p c d", p=128)
        kb.dma(w2k[:, :, 0:64], w2kv, [], [w2k], q='pool')
        kb.dma(w2k[:, :, 64:128], w2kv, [], [w2k], q='pool')
        kb.dma(w2v[:, :, :], dram['w2_%d' % l][1].rearrange("(c p) d -> p c d", p=128), [], [w2v], q='pool')
        kb.dma(peT[:, :, :], dram['peT%d' % l].rearrange("k p l -> p k l"), [], [peT])
        n = 0
        for kv in range(2):
            sv = srcs[kv][:].rearrange("p (n r) -> p n r", r=16)
            kb.tt('dve', xlo[kv][:].rearrange("p (n r) -> p n r", r=16), sv,
                  peT[:, kv, 0:16].unsqueeze(1).broadcast_to([128, 256, 16]), ALU.add, [srcs[kv], peT], [xlo[kv]])
            kb.tt('dve', xhi[kv][:].rearrange("p (n r) -> p n r", r=16), sv,
                  peT[:, kv, 16:32].unsqueeze(1).broadcast_to([128, 256, 16]), ALU.add, [srcs[kv], peT], [xhi[kv]])
            lov = xlo[kv][:].rearrange("p (n r) -> p n r", r=16)
            hiv = xhi[kv][:].rearrange("p (n r) -> p n r", r=16)
            for g in range(2):
                rows = slice(g * 64, g * 64 + 64)
                for fc in range(2):
                    p_ = ph[n % 2]
                    a0 = g0[n % 2]
                    a1 = g1[n % 2]
                    n += 1
                    for l_ in range(32):
                        rhs = lov[rows, 0:255, l_] if l_ < 16 else hiv[rows, 1:256, l_ - 16]
                        kb.mm(p_[:, 0:255], w1[kv][rows, l_, fc * 128:(fc + 1) * 128], rhs, l_ == 0, l_ == 31,
                              [w1[kv], xlo[kv], xhi[kv]], [p_])
                    kb.cp('act', a0[:, 0:255], p_[:, 0:255], [p_], [a0])
                    kb.tt('dve', a1[:, 0:255], a0[:, 0:255], a0[:, 0:255], ALU.mult, [a0], [a1])
                    kb.ts('dve', a1[:, 0:255], a1[:, 0:255], 0.044715, ALU.mult, [a1], [a1], s2=1.0, op1=ALU.add)
                    kb.tt('dve', a1[:, 0:255], a1[:, 0:255], a0[:, 0:255], ALU.mult, [a1, a0], [a1])
                    kb.act(a1[:, 0:255], a1[:, 0:255], AF.Tanh, [a1], [a1], scale=0.7978845608028654)
                    kb.ts('dve', a1[:, 0:255], a1[:, 0:255], 0.5, ALU.mult, [a1], [a1], s2=0.5, op1=ALU.add)
                    kb.tt('dve', hT[kv][:, g, fc, 0:255], a1[:, 0:255], a0[:, 0:255], ALU.mult, [a1, a0], [hT[kv]])
                if kv == 0:
                    p2 = pk[g]
                    for fc in range(2):
                        kb.mm(p2[:, 0:256], w2k[:, fc, :], hT[0][:, g, fc, :], fc == 0, fc == 1, [w2k, hT[0]], [p2])
                    kb.cp('act', kcs[rows, :], p2[rows, 0:256], [p2], [kcs])
                else:
                    for nch in range(2):
                        p2 = pk[nch]
                        for fc in range(2):
                            kb.mm(p2[:, 0:64], hT[1][:, g, fc, nch * 128:(nch + 1) * 128], w2v[:, fc, :], fc == 0, fc == 1,
                                  [w2v, hT[1]], [p2])
                        kb.cp('act', vcs[:, nch, g, :], p2[:, 0:64], [p2], [vcs])
        kb.dma(dram['kcs'][:, :], kcs[:], [kcs], [kb.db('kcs')])
        kb.dma(dram['vcs'][:, :], vcs[:].rearrange("p a g d -> p (a g d)"), [vcs], [kb.db('vcs')])
        P.barrier()
        P.emit()


def phase_b_even(kb, dram, l):
    P = kb.P
    with ExitStack() as pes:
        i4 = kb.sb(pes, [128, 512], BF16, 'i4')
        tri = kb.sb(pes, [128, 128], BF16, 'tri')
        wfar = kb.sb(pes, [128, 128], BF16, 'wfar')
        negm = kb.sb(pes, [128, 128], F32, 'negm')
        zer = kb.sb(pes, [128, 512], BF16, 'zer')
        kb.dma(i4[:], dram['i4'][:, :], [], [i4])
        kb.dma(tri[:], dram['tri'][:, :], [], [tri])
        kb.dma(wfar[:], dram['winfar'][:, :], [], [wfar])
        kb.dma(negm[:], dram['negm'][:, :], [], [negm])
        kb.memset('pool', zer[:], 0.0, [zer])
        kres = {}
        for nm, blk in (('ka', 4), ('ki', 7), ('ks', 13), ('kw', 14)):
            kres[nm] = kb.sb(pes, [128, T], BF16, nm)
            kb.dma(kres[nm][:], dram['qk'][blk, :, :], [kb.db('qk', blk, G) for G in range(8)], [kres[nm]])
        kcT = kb.sb(pes, [128, 256], BF16, 'kcT')
        kb.dma(kcT[:], dram['kcs'][:, :], [kb.db('kcs')], [kcT])
        vc = kb.sb(pes, [128, 2, 2, 130], BF16, 'vc')
        kb.dma(vc[:, :, :, 0:64], dram['vcs'].rearrange("p (a g d) -> p a g d", a=2, g=2), [kb.db('vcs')], [vc])
        kb.memset('pool', vc[:, :, :, 64:65], 1.0, [vc])
        for g in range(2):
            kb.dma(vc[:, :, g, 65:129], dram['ovl'].rearrange("(a p) m -> p a m", p=128), [], [vc])
        vt = kb.sb(pes, [128, NT, 5, 66], BF16, 'vt')
        kb.memset('pool', vt[:, :, :, 64:65], 1.0, [vt])
        for h5 in range(5):
            vsv = dram['vs'][:, h5 * 64:(h5 + 1) * 64].rearrange("(n p) d -> p n d", p=128)
            for q2 in range(2):
                kb.dma(vt[:, q2 * 16:(q2 + 1) * 16, h5, 0:64], vsv[:, q2 * 16:(q2 + 1) * 16, :], [kb.db('vs', i) for i in range(NT)], [vt])

        qs = [kb.sb(pes, [128, 10, 128], BF16, 'qs') for _ in range(2)]
        tmt = [kb.sb(pes, [128, 32], F32, 'tm') for _ in range(2)]
        cmt = [kb.sb(pes, [128, 256], BF16, 'cm') for _ in range(2)]
        fbt = [kb.sb(pes, [128, 64], F32, 'fb') for _ in range(2)]
        sc2 = [kb.sb(pes, [128, T], F32, 'sc') for _ in range(2)]
        rtmp = [kb.sb(pes, [128, 512], F32, 'rt') for _ in range(2)]
        m8 = kb.sb(pes, [128, 8], F32, 'm8')
        mb2 = [kb.sb(pes, [128, T], BF16, 'mb') for _ in range(2)]
        msel = [kb.sb(pes, [128, T], BF16, 'msel') for _ in range(2)]
        pT = [kb.sb(pes, [128, 512], BF16, 'pT') for _ in range(6)]
        rden = kb.sb(pes, [128, 4], F32, 'rden')
        coef = kb.sb(pes, [128, 4], F32, 'coef')
        imp = kb.sb(pes, [128, 64], F32, 'imp')
        scs = kb.sb(pes, [128, 64], F32, 'scs')
        scw = kb.sb(pes, [128, 64], F32, 'scw')
        selb = kb.sb(pes, [128, 64], BF16, 'selb')
        m8b = kb.sb(pes, [128, 8], F32, 'm8b')
        thr = kb.sb(pes, [128, 1], F32, 'thr')
        oacc2 = [kb.sb(pes, [128, 4, 64], F32, 'oacc') for _ in range(2)]
        ot = [kb.sb(pes, [128, D], BF16, 'ot') for _ in range(2)]
        psS = [kb.ps(pes, [128, 512], F32, 'psS') for _ in range(4)]
        pvb = [kb.ps(pes, [128, 512], F32, 'pvb') for _ in range(2)]
        pimp = kb.ps(pes, [128, 512], F32, 'pimp')
        pidx = [kb.ps(pes, [128, 512], F32, 'pidx') for _ in range(1)]
        cnt = {'s': 0, 'v': 0, 'x': 0}

        def attn_block(mask_lhsT, mask_r, kT_ap, k_r, q_ap, q_r, v_aps, pv, extra=None):
            pS = psS[cnt['s'] % 2]
            pt = pT[cnt['s'] % 3]
            cnt['s'] += 1
            if mask_lhsT is not None:
                kb.mm(pS[:, :], mask_lhsT, i4[:], True, False, [i4] + mask_r, [pS])
                kb.mm(pS[:, :], kT_ap, q_ap, False, True, k_r + q_r, [pS])
            else:
                kb.mm(pS[:, :], kT_ap, q_ap, True, True, k_r + q_r, [pS])
            kb.act(pt[:], pS[:, :], AF.Exp, [pS], [pt], scale=0.125)

            def pvpart():
                for hh in range(4):
                    kb.mm(pv[:, hh * 65:hh * 65 + 65], pt[:, hh * 128:(hh + 1) * 128], v_aps[0], False, False, [pt] + v_aps[1], [pv])
                    if extra is not None:
                        kb.mm(extra[0][:, hh * 64:hh * 64 + 64], pt[:, hh * 128:(hh + 1) * 128], extra[1], False, False,
                              [pt] + v_aps[1], [extra[0]])
            prev = pend[0]
            pend[0] = pvpart
            if prev is not None:
                prev()

        pend = [None]

        def flush_pv():
            if pend[0] is not None:
                f = pend[0]
                pend[0] = None
                f()

        def new_pv():
            pv = pvb[cnt['v'] % 2]
            cnt['v'] += 1
            kb.mm(pv[:, 0:260], zer[:, 0:128], zer[:, 0:260], True, False, [zer], [pv])
            return pv

        def get_rden(pv):
            flush_pv()
            dv = pv[:, 0:260].rearrange("p (h c) -> p h c", c=65)[:, :, 64]
            kb.ts('dve', rden[:, 0:4], dv, 1e-30, ALU.max, [pv], [rden])
            kb.recip(rden[:, 0:4], rden[:, 0:4], [rden], [rden])

        pw = kb.sb(pes, [128, 32], F32, 'pw')
        kb.dma(pw[:], dram['pw'][:, :], [], [pw])
        bmx = kb.sb(pes, [128, 1], F32, 'bmx')
        bmn = kb.sb(pes, [128, 1], F32, 'bmn')
        brng = kb.sb(pes, [128, 1], F32, 'brng')
        bthr = kb.sb(pes, [128, 1], F32, 'bthr')
        bcnt = kb.sb(pes, [128, 1], F32, 'bcnt')
        bw = kb.sb(pes, [128, 1], F32, 'bw')
        brk = kb.sb(pes, [128, 32], F32, 'brk')
        bjunk = kb.sb(pes, [128, T], BF16, 'bjunk')
        NSTEP = 17

        def stage1(i):
            N = (i + 1) * 128
            cols = slice(i * 128, (i + 1) * 128)
            rows_t = slice(i * 128, (i + 1) * 128)
            q_ = qs[i % 2]
            tm = tmt[i % 2]
            cm = cmt[i % 2]
            fb = fbt[i % 2]
            sc = sc2[i % 2]
            mb = mb2[i % 2]
            Gq = i // 4
            kb.dma(q_[:, 0:4, :], dram['qk'][0:4, :, cols].rearrange("b p t -> p b t"), [kb.db('qk', b_, Gq) for b_ in range(4)], [q_])
            kb.dma(q_[:, 4:6, :], dram['qk'][5:7, :, cols].rearrange("b p t -> p b t"), [kb.db('qk', b_, Gq) for b_ in (5, 6)], [q_])
            kb.dma(q_[:, 6:10, :], dram['qk'][8:12, :, cols].rearrange("b p t -> p b t"), [kb.db('qk', b_, Gq) for b_ in range(8, 12)], [q_])
            kb.dma(tm[:], dram['tm32'][rows_t, :], [kb.db('tm32', i)], [tm])
            kb.dma(cm[:], dram['cmask'][rows_t, :], [], [cm])
            kb.dma(fb[:], dram['fbias'][rows_t, :], [], [fb])
            ki = kres['ki']
            for ck in range((N + 511) // 512):
                c0 = ck * 512
                c1 = min(N, c0 + 512)
                w_ = c1 - c0
                for hi in range(4):
                    rows = slice((hi // 2) * 64, (hi // 2) * 64 + 64)
                    blk = 4 + (hi % 2)
                    pI = pidx[0]
                    rt = rtmp[cnt['x'] % 2]
                    cnt['x'] += 1
                    kb.mm(pI[:, 0:w_], q_[rows, blk, :], ki[rows, c0:c1], True, True, [q_, ki], [pI])
                    kb.act(rt[:, 0:w_], pI[:, 0:w_], AF.Relu, [pI], [rt])
                    if hi == 0:
                        kb.ts('pool', sc[:, c0:c1], rt[:, 0:w_], tm[:, 0:1], ALU.mult, [rt, tm], [sc], s2=0.0, op1=ALU.add)
                    else:
                        kb.ts('pool', rt[:, 0:w_], rt[:, 0:w_], tm[:, hi:hi + 1], ALU.mult, [rt, tm], [rt], s2=0.0, op1=ALU.add)
                        kb.tt('pool', sc[:, c0:c1], sc[:, c0:c1], rt[:, 0:w_], ALU.add, [rt, sc], [sc])
            kb.tt('pool', sc[:, cols], sc[:, cols], negm[:], ALU.add, [sc, negm], [sc])
            items = []
            if i < 2:
                items.append(lambda: kb.ts('dve', mb[:, 0:N], sc[:, 0:N], -1e29, ALU.is_lt, [sc], [mb], s2=NEG, op1=ALU.mult))
                return items
            def it_max():
                kb.P.op('dve', lambda e: e.tensor_reduce(out=bmx[:, 0:1], in_=sc[:, 0:N], axis=AX.X, op=ALU.max), _bufs([sc]), _bufs([bmx]))
            def it_min():
                kb.P.op('dve', lambda e: e.tensor_reduce(out=bmn[:, 0:1], in_=sc[:, 0:i * 128], axis=AX.X, op=ALU.min), _bufs([sc]), _bufs([bmn]))
                kb.tt('dve', brng[:, 0:1], bmx[:, 0:1], bmn[:, 0:1], ALU.subtract, [bmx, bmn], [brng])
                kb.stt('dve', bthr[:, 0:1], brng[:, 0:1], 0.5, bmn[:, 0:1], ALU.mult, ALU.add, [brng, bmn], [bthr])
                kb.ts('dve', brk[:, :], pw[:, :], brng[:, 0:1], ALU.mult, [pw, brng], [brk])
            items.append(it_max)
            items.append(it_min)
            for k in range(NSTEP):
                def it_step(k=k):
                    kb.ts('dve', bjunk[:, 0:N], sc[:, 0:N], bthr[:, 0:1], ALU.is_ge, [sc, bthr], [bjunk, bcnt], op1=ALU.add, accum_out=bcnt[:, 0:1])
                    kb.ts('dve', bw[:, 0:1], bcnt[:, 0:1], 255.5, ALU.is_gt, [bcnt], [bw], s2=0.5, op1=ALU.subtract)
                    kb.stt('dve', bthr[:, 0:1], bw[:, 0:1], brk[:, k:k + 1], bthr[:, 0:1], ALU.mult, ALU.add, [bw, brk, bthr], [bthr])
                items.append(it_step)
            def it_fin():
                kb.stt('dve', bthr[:, 0:1], brk[:, NSTEP:NSTEP + 1], -1.0, bthr[:, 0:1], ALU.mult, ALU.add, [brk, bthr], [bthr])
                kb.ts('dve', mb[:, 0:N], sc[:, 0:N], bthr[:, 0:1], ALU.is_lt, [sc, bthr], [mb], s2=NEG, op1=ALU.mult)
            items.append(it_fin)
            return items

        def pop(items, n):
            for _ in range(n):
                if items:
                    items.pop(0)()

        def qk_part(mask_lhsT, mask_r, kT_ap, k_r, q_ap, q_r):
            pS = psS[cnt['s'] % 4]
            pt = pT[cnt['s'] % 6]
            cnt['s'] += 1
            if mask_lhsT is not None:
                kb.mm(pS[:, :], mask_lhsT, i4[:], True, False, [i4] + mask_r, [pS])
                kb.mm(pS[:, :], kT_ap, q_ap, False, True, k_r + q_r, [pS])
            else:
                kb.mm(pS[:, :], kT_ap, q_ap, True, True, k_r + q_r, [pS])
            return pS, pt

        def pair_step(specs):
            cur = []
            for sp in specs:
                pS = psS[cnt['s'] % 4]
                pt = pT[cnt['s'] % 6]
                cnt['s'] += 1
                cur.append((pS, pt, sp))
            for pS, pt, sp in cur:
                if sp[0] is not None:
                    kb.mm(pS[:, :], sp[0], i4[:], True, False, [i4] + sp[1], [pS])
            for pS, pt, sp in cur:
                if sp[0] is not None:
                    kb.mm(pS[:, :], sp[2], sp[4], False, True, sp[3] + sp[5], [pS])
                else:
                    kb.mm(pS[:, :], sp[2], sp[4], True, True, sp[3] + sp[5], [pS])
            for pS, pt, sp in cur:
                kb.act(pt[:], pS[:, :], AF.Exp, [pS], [pt], scale=0.125)
            old = pend2[:]
            del pend2[:]
            for f in old:
                f()
            for pS, pt, sp in cur:
                def pvpart(pt=pt, sp=sp):
                    v_ap, v_r, pv, extra = sp[6], sp[7], sp[8], sp[9]
                    for hh in range(4):
                        kb.mm(pv[:, hh * 65:hh * 65 + 65], pt[:, hh * 128:(hh + 1) * 128], v_ap, False, False, [pt] + v_r, [pv])
                        if extra is not None:
                            kb.mm(extra[0][:, extra[2] + hh * 64:extra[2] + hh * 64 + 64], pt[:, hh * 128:(hh + 1) * 128], extra[1],
                                  False, False, [pt] + v_r, [extra[0]])
                pend2.append(pvpart)

        pend2 = []

        def flush2():
            old = pend2[:]
            del pend2[:]
            for f in old:
                f()

        def new_pair():
            for pv in pvb:
                kb.mm(pv[:, 0:260], zer[:, 0:128], zer[:, 0:260], True, False, [zer], [pv])
            return pvb

        def rden_of(pv):
            dv = pv[:, 0:260].rearrange("p (h c) -> p h c", c=65)[:, :, 64]
            kb.ts('dve', rden[:, 0:4], dv, 1e-30, ALU.max, [pv], [rden])
            kb.recip(rden[:, 0:4], rden[:, 0:4], [rden], [rden])

        def stage2(i, nxt):
            N = (i + 1) * 128
            cols = slice(i * 128, (i + 1) * 128)
            rows_t = slice(i * 128, (i + 1) * 128)
            q_ = qs[i % 2]
            tm = tmt[i % 2]
            cm = cmt[i % 2]
            fb = fbt[i % 2]
            mb = mb2[i % 2]
            o_ = ot[i % 2]
            per = (len(nxt) + 3) // 4
            R = [slice(0, 64), slice(64, 128)]
            ka = kres['ka']
            pvp = new_pair()
            for j in range(i + 1):
                pair_step([(mb[:, j * 128:(j + 1) * 128], [mb], ka[R[g], j * 128:(j + 1) * 128], [ka], q_[R[g], 0:4, :], [q_],
                            vt[:, j, 0, 0:65], [vt], pvp[g], None) for g in range(2)])
            flush2()
            for grp in range(2):
                rden_of(pvp[grp])
                for hh in range(4):
                    hd = grp * 4 + hh
                    kb.act(o_[:, hd * 64:(hd + 1) * 64], pvp[grp][:, hh * 65:hh * 65 + 64], AF.Copy, [pvp[grp], rden], [o_], scale=rden[:, hh:hh + 1])
            pop(nxt, per)
            gates = tm[:, 4:28].rearrange("p (g h b) -> p g h b", g=2, h=4)
            nchk = 1 if i < 16 else 2
            qg = [q_[R[g], 6:10, :] for g in range(2)]
            pvp = new_pair()
            kb.mm(pimp[:, 0:512], zer[:, 0:128], zer[:, 0:512], True, False, [zer], [pimp])
            for nch in range(nchk):
                pair_step([(cm[:, nch * 128:(nch + 1) * 128], [cm], kcT[R[g], nch * 128:(nch + 1) * 128], [kcT], qg[g], [q_],
                            vc[:, nch, g, 0:65], [vc], pvp[g], (pimp, vc[:, nch, g, 65:129], g * 256)) for g in range(2)])
            flush2()
            for g in range(2):
                pv = pvp[g]
                rden_of(pv)
                for hh in range(4):
                    pc = g * 256 + hh * 64
                    if hh == 0:
                        kb.ts('dve', imp[:], pimp[:, pc:pc + 64], rden[:, 0:1], ALU.mult, [pimp, rden], [imp])
                    else:
                        kb.stt('dve', imp[:], pimp[:, pc:pc + 64], rden[:, hh:hh + 1], imp[:], ALU.mult, ALU.add, [pimp, rden, imp], [imp])
                kb.tt('dve', coef[:, 0:4], rden[:, 0:4], gates[:, g, :, 0], ALU.mult, [rden, tm], [coef])
                for hh in range(4):
                    kb.ts('dve', oacc2[g][:, hh, :], pv[:, hh * 65:hh * 65 + 64], coef[:, hh:hh + 1], ALU.mult, [pv, coef], [oacc2[g]])
                kb.tt('dve', scs[:], imp[:], fb[:], ALU.add, [imp, fb], [scs])
                kb.P.op('dve', lambda e, a=m8b[:], b=scs[:]: e.max(out=a, in_=b), _bufs([scs]), _bufs([m8b]))
                kb.P.op('dve', lambda e, a=scw[:], b=m8b[:], c=scs[:]: e.match_replace(out=a, in_to_replace=b, in_values=c, imm_value=-3e9),
                        _bufs([scs, m8b]), _bufs([scw]))
                kb.P.op('dve', lambda e, a=m8b[:], b=scw[:]: e.max(out=a, in_=b), _bufs([scw]), _bufs([m8b]))
                kb.ts('dve', thr[:, 0:1], m8b[:, 7:8], -1e8, ALU.max, [m8b], [thr])
                kb.ts('dve', selb[:], scs[:], thr[:, 0:1], ALU.is_lt, [scs, thr], [selb], s2=NEG, op1=ALU.mult)
                ms = msel[g]
                nb = N // 64
                kb.cp('pool', ms[:, 0:N].rearrange("p (m s) -> p m s", s=64), selb[:, 0:nb].unsqueeze(2).broadcast_to([128, nb, 64]), [selb], [ms])
                kb.tt('pool', ms[:, cols], ms[:, cols], tri[:], ALU.add, [ms, tri], [ms])
            kw_ = kres['kw']
            pvp = new_pair()
            for j in range(max(0, i - 4), i + 1):
                if j == i:
                    ml, mr = tri[:], [tri]
                elif j == i - 4:
                    ml, mr = wfar[:], [wfar]
                else:
                    ml, mr = None, []
                pair_step([(ml, mr, kw_[R[g], j * 128:(j + 1) * 128], [kw_], qg[g], [q_], vt[:, j, 3 + g, 0:65], [vt], pvp[g], None)
                           for g in range(2)])
            flush2()
            for g in range(2):
                pv = pvp[g]
                rden_of(pv)
                kb.tt('dve', coef[:, 0:4], rden[:, 0:4], gates[:, g, :, 2], ALU.mult, [rden, tm], [coef])
                for hh in range(4):
                    kb.stt('dve', oacc2[g][:, hh, :], pv[:, hh * 65:hh * 65 + 64], coef[:, hh:hh + 1], oacc2[g][:, hh, :], ALU.mult, ALU.add,
                           [pv, coef, oacc2[g]], [oacc2[g]])
            pop(nxt, per)
            ks = kres['ks']
            pvp = new_pair()
            for j in range(i + 1):
                pair_step([(msel[g][:, j * 128:(j + 1) * 128], [msel[g]], ks[R[g], j * 128:(j + 1) * 128], [ks], qg[g], [q_],
                            vt[:, j, 1 + g, 0:65], [vt], pvp[g], None) for g in range(2)])
            flush2()
            for g in range(2):
                pv = pvp[g]
                rden_of(pv)
                kb.tt('dve', coef[:, 0:4], rden[:, 0:4], gates[:, g, :, 1], ALU.mult, [rden, tm], [coef])
                for hh in range(4):
                    kb.stt('dve', oacc2[g][:, hh, :], pv[:, hh * 65:hh * 65 + 64], coef[:, hh:hh + 1], oacc2[g][:, hh, :], ALU.mult, ALU.add,
                           [pv, coef, oacc2[g]], [oacc2[g]])
                kb.cp('pool', o_[:, 512 + g * 256:512 + (g + 1) * 256], oacc2[g][:].rearrange("p h d -> p (h d)"), [oacc2[g]], [o_])
            pop(nxt, 2 * per)
            kb.dma(dram['os'][rows_t, :], o_[:], [o_], [kb.db('os', i // 4, hh_) for hh_ in range(8)])

        items0 = stage1(0)
        pop(items0, len(items0))
        for i in range(NT):
            nxt = stage1(i + 1) if i + 1 < NT else []
            stage2(i, nxt)
            pop(nxt, len(nxt))
        P.barrier()
        P.emit()


def phase_c(kb, dram, l, hsrc, ident, dst, final_norm):
    P = kb.P
    with ExitStack() as pes:
        wu = kb.sb(pes, [128, 8, DFF], BF16, 'wu')
        wd = kb.sb(pes, [128, 32, D], BF16, 'wd')
        wo = kb.sb(pes, [128, 8, D], BF16, 'wo')
        g_sb = kb.sb(pes, [128, 8], F32, 'g')
        kb.dma(g_sb[:], dram['mlp_g'][l], [], [g_sb])
        for c in range(8):
            kb.dma(wo[:, c, :], dram['w_out%d' % l][c * 128:(c + 1) * 128, :], [], [wo], q='pool')
        for c in range(8):
            kb.dma(wu[:, c, :], dram['w_up%d' % l][c * 128:(c + 1) * 128, :], [], [wu], q='pool')
        wdv = dram['w_down%d' % l].rearrange("(c p) n -> p c n", p=128)
        for c in range(8):
            kb.dma(wd[:, 4 * c:4 * c + 4, :], wdv[:, 4 * c:4 * c + 4, :], [], [wd], q='pool')
        if final_norm:
            fg = kb.sb(pes, [128, D], F32, 'fg')
            kb.dma(fg[:], dram['final_g'][0:1, :].partition_broadcast(128), [], [fg])
        ht = [kb.sb(pes, [128, D], F32, 'ht') for _ in range(2)]
        ot = [kb.sb(pes, [128, D], BF16, 'ot') for _ in range(2)]
        oT = kb.sb(pes, [128, 8, 128], BF16, 'oT')
        junk = kb.sb(pes, [128, D], BF16, 'junk')
        ss = kb.sb(pes, [128, 1], F32, 'ss')
        rstd = kb.sb(pes, [128, 1], F32, 'rstd')
        xn = kb.sb(pes, [128, D], BF16, 'xn')
        uT = kb.sb(pes, [128, 8, 128], BF16, 'uT')
        rl = [kb.sb(pes, [128, 512], F32, 'rl') for _ in range(2)]
        aT = kb.sb(pes, [128, 32, 128], BF16, 'aT')
        pst = [kb.ps(pes, [128, 1024], BF16, 'pst') for _ in range(2)]
        pu = [kb.ps(pes, [128, 512], F32, 'pu') for _ in range(2)]
        pd = [kb.ps(pes, [128, 512], F32, 'pd') for _ in range(2)]
        ss2 = kb.sb(pes, [128, 1], F32, 'ss2')
        rstd2 = kb.sb(pes, [128, 1], F32, 'rstd2')

        def front_a(i):
            h_ = ht[i % 2]
            o_ = ot[i % 2]
            rows = slice(i * 128, (i + 1) * 128)
            kb.dma(h_[:], hsrc[rows, :], [kb.db('h', i)], [h_])
            kb.dma(o_[:], dram['os'][rows, :], [kb.db('os', i // 4, hh) for hh in range(8)], [o_])
            for c in range(8):
                kb.tr(pst[0][:, c * 128:(c + 1) * 128], o_[:, c * 128:(c + 1) * 128], ident[:], [o_, ident], [pst[0]])
            kb.cp('act', oT[:].rearrange("p c t -> p (c t)"), pst[0][:, :], [pst[0]], [oT])

        def front_b(i):
            h_ = ht[i % 2]
            for half in range(2):
                for c in range(8):
                    kb.mm(pd[half][:, :], oT[:, c, :], wo[:, c, half * 512:(half + 1) * 512], c == 0, c == 7, [oT, wo], [pd[half]])
                kb.tt('dve', h_[:, half * 512:(half + 1) * 512], pd[half][:, :], h_[:, half * 512:(half + 1) * 512],
                      ALU.add, [pd[half], h_], [h_])
            kb.rms_rstd(h_, junk, ss, rstd, D)
            kb.ts('dve', xn[:], h_[:], rstd[:, 0:1], ALU.mult, [h_, rstd], [xn])

        def front_c(i):
            for c in range(8):
                kb.tr(pst[1][:, c * 128:(c + 1) * 128], xn[:, c * 128:(c + 1) * 128], ident[:], [xn, ident], [pst[1]])
            kb.tt('dve', uT[:], pst[1][:].rearrange("p (c t) -> p c t", c=8),
                  g_sb[:].unsqueeze(2).broadcast_to([128, 8, 128]), ALU.mult, [pst[1], g_sb], [uT])

        def up(i, g0, g1):
            for fg4 in range(g0, g1):
                put = pu[fg4 % 2]
                for f_ in range(4):
                    fc = fg4 * 4 + f_
                    for c in range(8):
                        kb.mm(put[:, f_ * 128:(f_ + 1) * 128], wu[:, c, fc * 128:(fc + 1) * 128], uT[:, c, :],
                              c == 0, c == 7, [wu, uT], [put])
                r_ = rl[fg4 % 2]
                kb.act(r_[:], put[:, :], AF.Relu, [put], [r_])
                kb.tt('pool', aT[:, fg4 * 4:fg4 * 4 + 4, :].rearrange("p c t -> p (c t)"), r_[:], r_[:], ALU.mult, [r_], [aT])

        def down(i):
            h_ = ht[i % 2]
            rows = slice(i * 128, (i + 1) * 128)
            for half in range(2):
                for fc in range(32):
                    kb.mm(pd[half][:, :], aT[:, fc, :], wd[:, fc, half * 512:(half + 1) * 512], fc == 0, fc == 31, [aT, wd], [pd[half]])
                kb.tt('dve', h_[:, half * 512:(half + 1) * 512], pd[half][:, :], h_[:, half * 512:(half + 1) * 512],
                      ALU.add, [pd[half], h_], [h_])
            if final_norm:
                kb.rms_rstd(h_, junk, ss2, rstd2, D)
                kb.stt('dve', h_[:], h_[:], rstd2[:, 0:1], fg[:], ALU.mult, ALU.mult, [h_, rstd2, fg], [h_])
            kb.dma(dst[rows, :], h_[:], [h_], [kb.db('h', i) if not final_norm else kb.db('y', i)])

        front_a(0)
        front_b(0)
        front_c(0)
        for i in range(NT):
            nx = i + 1 < NT
            if nx:
                front_a(i + 1)
            up(i, 0, 4)
            if nx:
                front_b(i + 1)
            up(i, 4, 8)
            down(i)
            if nx:
                front_c(i + 1)
        P.barrier()
        P.emit()


def _bf(a):
    return np.asarray(a, dtype=np.float32).astype(ml_dtypes.bfloat16)


def host_consts():
    c = {}
    c['ident'] = _bf(np.eye(128))
    c['i4'] = _bf(np.tile(np.eye(128), (1, 4)))
    t = np.arange(128)[:, None]
    s = np.arange(128)[None, :]
    c['tri'] = _bf(np.where(s <= t, 0.0, NEG))
    inv = 1.0 / (10000.0 ** (np.arange(0, 64, 2, dtype=np.float32) / 64))
    ang = np.arange(T, dtype=np.float32)[None, :] * inv[:, None].astype(np.float32)
    cos = np.cos(ang).astype(np.float32)
    sin = np.sin(ang).astype(np.float32)
    c['winfar'] = _bf(np.where(s > t, 0.0, NEG))
    c['negm'] = np.where(s <= t, 0.0, -1e30).astype(np.float32)
    tq = np.arange(T)[:, None]
    nn = np.arange(256)[None, :]
    c['cmask'] = _bf(np.where((16 * nn + 31 <= tq) & (nn < 255), 0.0, NEG))
    mm_ = np.arange(64)[None, :]
    cur = tq // 64
    fbv = np.where(mm_ * 64 <= tq, 0.0, -1e9)
    fbv = np.where(mm_ == cur - 1, 1e9, fbv)
    fbv = np.where(mm_ == cur, 2e9, fbv)
    fbv = np.where(mm_ == 0, 3e9, fbv)
    c['fbias'] = fbv.astype(np.float32)
    n_ = np.arange(256)[:, None]
    c0_ = n_ * 16
    ov = ((c0_ < (mm_ + 1) * 64) & (c0_ + 32 > mm_ * 64) & (n_ < 255))
    c['ovl'] = _bf(ov.astype(np.float32))
    c['pw'] = np.ascontiguousarray(np.tile((2.0 ** -(np.arange(32) + 1.0))[None, :], (128, 1)).astype(np.float32))
    c['ropeC'] = np.ascontiguousarray(np.concatenate([cos, cos, cos, cos], 0))
    c['ropeS'] = np.ascontiguousarray(np.concatenate([-sin, sin, -sin, sin], 0))
    return c


def swap_cols(w):
    d, n = w.shape
    return np.ascontiguousarray(w.reshape(d, n // 64, 2, 32)[:, :, ::-1, :].reshape(d, n))


def even_cols():
    qa0, ka0, va0, qi0, ki0, wi0, qb0, kv0, gb0 = 0, 512, 576, 640, 896, 960, 964, 1476, 2244

    def hd(base, h):
        return list(range(base + h * 64, base + (h + 1) * 64))

    def kvh(s6, g):
        return hd(kv0, s6 * 2 + g)
    blocks = []
    for r in range(4):
        blocks.append(hd(qa0, r) + hd(qa0, 4 + r))
    blocks.append(hd(ka0, 0) * 2)
    for r in range(2):
        blocks.append(hd(qi0, r) + hd(qi0, 2 + r))
    blocks.append(hd(ki0, 0) * 2)
    for r in range(4):
        blocks.append(hd(qb0, r) + hd(qb0, 4 + r))
    blocks.append(kvh(0, 0) + kvh(0, 1))
    blocks.append(kvh(2, 0) + kvh(2, 1))
    blocks.append(kvh(4, 0) + kvh(4, 1))
    blocks.append(kvh(1, 0) + kvh(1, 1))
    fm = [c for b in blocks for c in b]
    tmc = hd(va0, 0) + kvh(3, 0) + kvh(3, 1) + kvh(5, 0) + kvh(5, 1) + list(range(wi0, wi0 + 4)) + list(range(gb0, gb0 + 24))
    assert len(fm) == 2048 and len(tmc) == 348
    return fm, tmc


def gain_layout(g):
    return np.ascontiguousarray(g.reshape(g.shape[0], 8, 128).transpose(0, 2, 1))


def host_inputs(inputs, layers):
    m = dict(host_consts())
    m['mix_g'] = gain_layout(np.asarray(inputs['mix_norm_g'], np.float32))
    m['mlp_g'] = gain_layout(np.asarray(inputs['mlp_norm_g'], np.float32))
    m['final_g'] = np.asarray(inputs['final_norm_g'], np.float32).reshape(1, D)
    for l in layers:
        m['w_up%d' % l] = np.ascontiguousarray(inputs['mlp_w_up'][l])
        m['w_down%d' % l] = np.ascontiguousarray(inputs['mlp_w_down'][l])
        if l % 2 == 0:
            e = l // 2
            w = np.asarray(inputs['even_w_in'][e], np.float32)
            fm, tmc = even_cols()
            wqk = np.ascontiguousarray(w[:, fm])
            m['wqk%d' % l] = wqk
            m['wqks%d' % l] = swap_cols(np.ascontiguousarray(wqk[:, 0:1920]))
            m['wv%d' % l] = np.ascontiguousarray(w[:, tmc])
            m['w1_%d' % l] = np.ascontiguousarray(inputs['even_cmp_w1'][e], dtype=np.float32)
            m['w2_%d' % l] = np.ascontiguousarray(inputs['even_cmp_w2'][e], dtype=np.float32)
            pe = np.asarray(inputs['even_cmp_pe'][e], np.float32)
            peT = pe.transpose(0, 2, 1)
            m['peT%d' % l] = np.ascontiguousarray(np.concatenate([peT, peT], 1))
            m['w_out%d' % l] = np.ascontiguousarray(inputs['even_w_out'][e])
        if l % 2 == 1:
            o = l // 2
            w = np.asarray(inputs['odd_w_in'][o], np.float32)
            wqk = np.ascontiguousarray(w[:, 0:2048])
            m['wqk%d' % l] = wqk
            m['wqks%d' % l] = swap_cols(wqk)
            m['wv%d' % l] = np.ascontiguousarray(w[:, 2048:3072])
            m['lam%d' % l] = np.asarray(inputs['odd_lambda'][o], np.float32).reshape(1, 256)
            m['subg%d' % l] = np.asarray(inputs['odd_subln_g'][o], np.float32).reshape(1, 128)
            m['w_out%d' % l] = np.ascontiguousarray(inputs['odd_w_out'][o])
    return m


def kernel(**inputs):
    layers = [0, 1, 2, 3]
    inputs = {k: np.asarray(v) for k, v in inputs.items()}
    nc = build_program(dict(layers=layers, final_norm=True))
    shared = host_inputs(inputs, layers)
    in_maps = []
    for c in range(N_CORES):
        m = dict(shared)
        m['x'] = np.ascontiguousarray(inputs['x'][c % 4])
        in_maps.append(m)
    res = run_bass_kernel_spmd(nc, in_maps, core_ids=list(range(N_CORES)))
    return np.stack([res.results[b]['y'] for b in range(4)], 0).astype(np.float32)
```

```python
import math
import numpy as np
from contextlib import ExitStack
import ml_dtypes
import concourse.bass as bass
import concourse.mybir as mybir
from concourse.bass_utils import run_bass_kernel_spmd

F32 = mybir.dt.float32
BF16 = mybir.dt.bfloat16
AF = mybir.ActivationFunctionType
ALU = mybir.AluOpType
AX = mybir.AxisListType

T = 4096
D = 1024
NT = 32
DFF = 4096
EPS = 1e-6
NEG = -30000.0
N_CORES = 8


class Buf:
    __slots__ = ('name', 'lw', 'rd')

    def __init__(self, name=''):
        self.name = name
        self.lw = None
        self.rd = {}


class Tl:
    def __init__(self, t, name=''):
        self.t = t
        self.b = Buf(name)

    def __getitem__(self, k):
        return self.t[k]


def _bufs(lst):
    out = []
    for x in lst:
        if x is None:
            continue
        out.append(x.b if isinstance(x, Tl) else x)
    return out


class Prog:
    CE = ('pe', 'act', 'dve', 'pool')

    def __init__(self, nc, es, n_dma_sems=12):
        self.nc = nc
        self.ops = {e: [] for e in ('pe', 'act', 'dve', 'pool', 'sp')}
        self.sems = {}
        for e in self.CE:
            self.sems[e] = es.enter_context(nc.semaphore('s_' + e))
        self.cnt = {e: 0 for e in self.CE}
        self.dq = {}
        for q in ('sp', 'pool', 'act'):
            n = n_dma_sems if q == 'sp' else 6
            ss = []
            for i in range(n):
                k = 'd_%s_%d' % (q, i)
                self.sems[k] = es.enter_context(nc.semaphore(k))
                self.cnt[k] = 0
                ss.append(k)
            self.dq[q] = [ss, 0]
        self.waited = {e: {} for e in self.ops}
        self.same_engine_sync = {'pe': False, 'act': True, 'dve': True, 'pool': True, 'sp': True}
        self.total = 0

    def _deps(self, reads, writes):
        deps = {}
        for b in reads:
            if b.lw is not None:
                k, v = b.lw
                if deps.get(k, 0) < v:
                    deps[k] = v
        for b in writes:
            if b.lw is not None:
                k, v = b.lw
                if deps.get(k, 0) < v:
                    deps[k] = v
            for k, v in b.rd.items():
                if deps.get(k, 0) < v:
                    deps[k] = v
        return deps

    def _mark(self, tok, reads, writes):
        k, v = tok
        for b in reads:
            if b.rd.get(k, 0) < v:
                b.rd[k] = v
        for b in writes:
            b.lw = tok
            b.rd = {}

    def op(self, eng, fn, reads=(), writes=()):
        reads = _bufs(reads)
        writes = _bufs(writes)
        deps = self._deps(reads, writes)
        waits = []
        wd = self.waited[eng]
        for k, v in deps.items():
            if k == eng and not self.same_engine_sync[eng]:
                continue
            if wd.get(k, 0) >= v:
                continue
            wd[k] = v
            waits.append((k, v))
        self.cnt[eng] += 1
        tok = (eng, self.cnt[eng])
        self.ops[eng].append((waits, fn, eng, 1))
        self._mark(tok, reads, writes)
        self.total += 1

    def dma(self, fn, reads=(), writes=(), q='sp', inc=16):
        reads = _bufs(reads)
        writes = _bufs(writes)
        ss, i = self.dq[q]
        k = ss[i % len(ss)]
        self.dq[q][1] = i + 1
        deps = self._deps(reads, writes)
        if self.cnt[k] > 0 and deps.get(k, 0) < self.cnt[k]:
            deps[k] = self.cnt[k]
        waits = []
        wd = self.waited[q]
        for kk, v in deps.items():
            if wd.get(kk, 0) >= v:
                continue
            wd[kk] = v
            waits.append((kk, v))
        self.cnt[k] += inc
        tok = (k, self.cnt[k])
        self.ops[q].append((waits, fn, k, inc))
        self._mark(tok, reads, writes)
        self.total += 1

    def barrier(self):
        for e in self.ops:
            waits = []
            wd = self.waited[e]
            for k, v in self.cnt.items():
                if v > 0 and wd.get(k, 0) < v:
                    wd[k] = v
                    waits.append((k, v))
            if waits:
                self.ops[e].append((waits, None, None, 0))

    def emit(self):
        nc = self.nc
        sems = self.sems

        def mk(name):
            lst = self.ops[name]

            def body(e):
                for waits, fn, sk, inc in lst:
                    for k, v in waits:
                        e.wait_ge(sems[k], v)
                    if fn is not None:
                        fn(e).then_inc(sems[sk], inc)
            return body

        with nc.Block() as block:
            block.tensor(mk('pe'))
            block.scalar(mk('act'))
            block.vector(mk('dve'))
            block.gpsimd(mk('pool'))
            block.sync(mk('sp'))
        self.ops = {e: [] for e in self.ops}


class KB:
    def __init__(self, nc, es):
        self.nc = nc
        self.P = Prog(nc, es)
        self.dbufs = {}
        self.uid = 0

    def sb(self, es, shape, dt, name=None):
        self.uid += 1
        name = (name or 't') + '_%d' % self.uid
        return Tl(es.enter_context(self.nc.sbuf_tensor(name, list(shape), dt)), name)

    def ps(self, es, shape, dt, name=None):
        self.uid += 1
        name = (name or 'p') + '_%d' % self.uid
        return Tl(es.enter_context(self.nc.psum_tensor(name, list(shape), dt)), name)

    def db(self, *key):
        b = self.dbufs.get(key)
        if b is None:
            b = Buf(str(key))
            self.dbufs[key] = b
        return b

    def mm(self, out, lhsT, rhs, start, stop, r, w):
        self.P.op('pe', lambda e: e.matmul(out, lhsT=lhsT, rhs=rhs, start=start, stop=stop,
                                           skip_group_check=True), r, w)

    def tr(self, out, in_, ident, r, w):
        self.P.op('pe', lambda e: e.transpose(out=out, in_=in_, identity=ident), r, w)

    def act(self, out, in_, func, r, w, scale=None, bias=None, accum_out=None, eng='act'):
        kw = {}
        if scale is not None:
            kw['scale'] = scale
        if bias is not None:
            kw['bias'] = bias
        if accum_out is not None:
            kw['accum_out'] = accum_out
        self.P.op('act', lambda e: e.activation(out=out, in_=in_, func=func, **kw), r, w)

    def ts(self, eng, out, in0, s1, op0, r, w, s2=None, op1=None, accum_out=None):
        kw = {}
        if op1 is not None:
            kw['op1'] = op1
        if accum_out is not None:
            kw['accum_out'] = accum_out
        self.P.op(eng, lambda e: e.tensor_scalar(out=out, in0=in0, scalar1=s1, scalar2=s2, op0=op0, **kw), r, w)

    def tt(self, eng, out, in0, in1, op, r, w):
        self.P.op(eng, lambda e: e.tensor_tensor(out=out, in0=in0, in1=in1, op=op), r, w)

    def stt(self, eng, out, in0, scalar, in1, op0, op1, r, w):
        self.P.op(eng, lambda e: e.scalar_tensor_tensor(out=out, in0=in0, scalar=scalar, in1=in1, op0=op0, op1=op1), r, w)

    def cp(self, eng, out, in_, r, w):
        if eng == 'act':
            self.P.op('act', lambda e: e.copy(out=out, in_=in_), r, w)
        else:
            self.P.op(eng, lambda e: e.tensor_copy(out=out, in_=in_), r, w)

    def memset(self, eng, ap, val, w):
        self.P.op(eng, lambda e: e.memset(ap, val), (), w)

    def recip(self, out, in_, r, w):
        self.P.op('dve', lambda e: e.reciprocal(out=out, in_=in_), r, w)

    def dma(self, out, in_, r, w, q='sp'):
        self.P.dma(lambda e: e.dma_start(out=out, in_=in_), r, w, q=q)

    def rms_rstd(self, h, junk, ss, rstd, width):
        self.act(junk[:, 0:width], h[:, 0:width], AF.Square, [h], [junk, ss], accum_out=ss[:, 0:1])
        self.ts('dve', ss[:, 0:1], ss[:, 0:1], 1.0 / width, ALU.mult, [ss], [ss], s2=EPS, op1=ALU.add)
        self.act(ss[:, 0:1], ss[:, 0:1], AF.Sqrt, [ss], [ss])
        self.recip(rstd[:, 0:1], ss[:, 0:1], [ss], [rstd])


def build_program(cfg):
    nc = bass.Bass("TRN2", target_bir_lowering=False)
    layers = cfg['layers']
    dram = {}

    def din(name, shape, dt=F32):
        dram[name] = nc.dram_tensor(name, list(shape), dt, kind="ExternalInput").ap()
        return dram[name]

    def dscr(name, shape, dt):
        dram[name] = nc.dram_tensor(name, list(shape), dt, kind="Internal").ap()
        return dram[name]

    x = din('x', [T, D])
    y = nc.dram_tensor('y', [T, D], F32, kind="ExternalOutput").ap()
    din('mix_g', [4, 128, 8])
    din('mlp_g', [4, 128, 8])
    din('final_g', [1, D])
    din('ident', [128, 128], BF16)
    din('i4', [128, 512], BF16)
    din('tri', [128, 128], BF16)
    din('ropeC', [128, T])
    din('ropeS', [128, T])
    din('winfar', [128, 128], BF16)
    din('negm', [128, 128])
    din('cmask', [T, 256], BF16)
    din('fbias', [T, 64])
    din('ovl', [256, 64], BF16)
    din('pw', [128, 32])
    din('pswap', [128, 128], BF16)
    for l in layers:
        din('w_up%d' % l, [D, DFF])
        din('w_down%d' % l, [DFF, D])
        din('w_out%d' % l, [D, D])
        if l % 2 == 0:
            din('wqk%d' % l, [D, 2048])
            din('wv%d' % l, [D, 348])
            din('w1_%d' % l, [2, 2048, 256])
            din('w2_%d' % l, [2, 256, 64])
            din('peT%d' % l, [2, 128, 32])
        if l % 2 == 1:
            din('wqk%d' % l, [D, 2048])
            din('wv%d' % l, [D, 1024])
            din('lam%d' % l, [1, 256])
            din('subg%d' % l, [1, 128])
    hs = dscr('hs', [T, D], F32)
    qk = dscr('qk', [20, 128, T], BF16)
    vs = dscr('vs', [T, 1024], BF16)
    os_ = dscr('os', [T, D], BF16)
    dscr('tm32', [T, 32], F32)
    dscr('kcs', [128, 256], BF16)
    dscr('vcs', [128, 256], BF16)

    with ExitStack() as es:
        kb = KB(nc, es)
        P = kb.P
        ident = kb.sb(es, [128, 128], BF16, 'ident')
        kb.dma(ident[:], dram['ident'][:, :], [], [ident])
        hsrc = x
        for li, l in enumerate(layers):
            if l % 2 == 1:
                phase_a(kb, dram, l, hsrc, ident)
                phase_b_odd(kb, dram, l, ident)
            else:
                phase_a(kb, dram, l, hsrc, ident)
                phase_b0_even(kb, dram, l)
                phase_b_even(kb, dram, l)
            last = (li == len(layers) - 1)
            phase_c(kb, dram, l, hsrc, ident, y if last else hs, last and cfg.get('final_norm', True))
            hsrc = hs
    return nc


def load_norm_transpose(kb, pes, h_ap, hb, g_sb, ident, ht, junk, ss, rstd, xn, pst, uT_ap, uT_tl):
    kb.dma(ht[:], h_ap, [hb], [ht])
    kb.rms_rstd(ht, junk, ss, rstd, D)
    kb.ts('dve', xn[:], ht[:], rstd[:, 0:1], ALU.mult, [ht, rstd], [xn])
    for c in range(8):
        kb.tr(pst[:, c * 128:(c + 1) * 128], xn[:, c * 128:(c + 1) * 128], ident[:], [xn, ident], [pst])
    kb.tt('dve', uT_ap, pst[:].rearrange("p (c t) -> p c t", c=8),
          g_sb[:].unsqueeze(2).broadcast_to([128, 8, 128]), ALU.mult, [pst, g_sb], [uT_tl])


def phase_a(kb, dram, l, hsrc, ident):
    P = kb.P
    even = (l % 2 == 0)
    nfm = 16
    nsw = 15 if even else 16
    ntm = 348 if even else 1024
    with ExitStack() as pes:
        wqk = kb.sb(pes, [128, 8, nfm * 128], BF16, 'wqk')
        pswap = kb.sb(pes, [128, 128], BF16, 'pswap')
        kb.dma(pswap[:], dram['pswap'][:, :], [], [pswap])
        xraw = [kb.sb(pes, [128, 512], BF16, 'xraw') for _ in range(2)]
        wv = kb.sb(pes, [128, 8, ntm], BF16, 'wv')
        g_sb = kb.sb(pes, [128, 8], F32, 'g')
        kb.dma(g_sb[:], dram['mix_g'][l], [], [g_sb])
        for c in range(8):
            kb.dma(wqk[:, c, :], dram['wqk%d' % l][c * 128:(c + 1) * 128, :], [], [wqk], q='pool')
            kb.dma(wv[:, c, :], dram['wv%d' % l][c * 128:(c + 1) * 128, :], [], [wv], q='pool')
        ht = [kb.sb(pes, [128, D], F32, 'ht') for _ in range(2)]
        junk = kb.sb(pes, [128, D], BF16, 'junk')
        ss = [kb.sb(pes, [128, 1], F32, 'ss') for _ in range(2)]
        rstd = [kb.sb(pes, [128, 1], F32, 'rstd') for _ in range(2)]
        xn = [kb.sb(pes, [128, D], BF16, 'xn') for _ in range(2)]
        uT = [kb.sb(pes, [128, 8, 512], BF16, 'uT') for _ in range(2)]
        cc = [kb.sb(pes, [128, 512], F32, 'cc') for _ in range(2)]
        sn = [kb.sb(pes, [128, 512], F32, 'sn') for _ in range(2)]
        t1 = [kb.sb(pes, [128, 512], F32, 't1') for _ in range(2)]
        t2 = [kb.sb(pes, [128, 512], F32, 't2') for _ in range(2)]
        stg = [kb.sb(pes, [128, 512], BF16, 'stg') for _ in range(3)]
        vst = [kb.sb(pes, [128, 1024], BF16, 'vst') for _ in range(2)]
        tmf = [kb.sb(pes, [128, 32], F32, 'tmf') for _ in range(2)]
        for t_ in tmf:
            kb.memset('pool', t_[:], 0.0, [t_])
        pst = [kb.ps(pes, [128, 1024], BF16, 'pst') for _ in range(2)]
        pa = [kb.ps(pes, [128, 512], F32, 'pa') for _ in range(2)]
        pb = [kb.ps(pes, [128, 512], F32, 'pb') for _ in range(2)]
        pv = [kb.ps(pes, [128, 512], F32, 'pv') for _ in range(2)]
        n = 0

        def front(G, tt_):
            u = uT[G % 2]
            i = G * 4 + tt_
            if tt_ == 0:
                kb.dma(cc[G % 2][:], dram['ropeC'][:, G * 512:(G + 1) * 512], [], [cc[G % 2]])
                kb.dma(sn[G % 2][:], dram['ropeS'][:, G * 512:(G + 1) * 512], [], [sn[G % 2]])
            load_norm_transpose(kb, pes, hsrc[i * 128:(i + 1) * 128, :], kb.db('h', i), g_sb, ident,
                                ht[i % 2], junk, ss[i % 2], rstd[i % 2], xn[i % 2], pst[i % 2],
                                u[:, :, tt_ * 128:(tt_ + 1) * 128], u)
            if not even:
                for half in range(2):
                    pvt = pv[half]
                    for c in range(8):
                        kb.mm(pvt[:, :], u[:, c, tt_ * 128:(tt_ + 1) * 128], wv[:, c, half * 512:(half + 1) * 512],
                              c == 0, c == 7, [u, wv], [pvt])
                    kb.cp('act', vst[i % 2][:, half * 512:(half + 1) * 512], pvt[:, :], [pvt], [vst[i % 2]])
                kb.dma(dram['vs'][i * 128:(i + 1) * 128, :], vst[i % 2][:], [vst[i % 2]], [kb.db('vs', i)])
            else:
                pvt = pv[i % 2]
                for c in range(8):
                    kb.mm(pvt[:, 0:ntm], u[:, c, tt_ * 128:(tt_ + 1) * 128], wv[:, c, :], c == 0, c == 7, [u, wv], [pvt])
                kb.cp('act', vst[i % 2][:, 0:320], pvt[:, 0:320], [pvt], [vst[i % 2]])
                kb.cp('act', tmf[i % 2][:, 0:4], pvt[:, 320:324], [pvt], [tmf[i % 2]])
                kb.act(tmf[i % 2][:, 4:28], pvt[:, 324:348], AF.Sigmoid, [pvt], [tmf[i % 2]])
                kb.dma(dram['vs'][i * 128:(i + 1) * 128, 0:320], vst[i % 2][:, 0:320], [vst[i % 2]], [kb.db('vs', i)])
                kb.dma(dram['tm32'][i * 128:(i + 1) * 128, :], tmf[i % 2][:, :], [tmf[i % 2]], [kb.db('tm32', i)])

        for tt_ in range(4):
            front(0, tt_)
        for G in range(8):
            u = uT[G % 2]
            for rb in range(nfm):
                pat = pa[rb % 2]
                pbt = pb[rb % 2]
                for c in range(8):
                    kb.mm(pat[:, :], wqk[:, c, rb * 128:(rb + 1) * 128], u[:, c, :], c == 0, c == 7, [u, wqk], [pat])
                st = stg[n % 3]
                n += 1
                if rb < nsw:
                    xr = xraw[rb % 2]
                    kb.cp('act', xr[:], pat[:, :], [pat], [xr])
                    kb.mm(pbt[:, :], pswap[:], xr[:], True, True, [pswap, xr], [pbt])
                    a1 = t1[rb % 2]
                    a2 = t2[rb % 2]
                    kb.tt('dve', a1[:], pat[:, :], cc[G % 2][:], ALU.mult, [pat, cc[G % 2], xr], [a1])
                    kb.tt('dve', a2[:], pbt[:, :], sn[G % 2][:], ALU.mult, [pbt, sn[G % 2]], [a2])
                    kb.tt('pool', st[:], a1[:], a2[:], ALU.add, [a1, a2], [st])
                else:
                    kb.cp('act', st[:], pat[:, :], [pat], [st])
                kb.dma(dram['qk'][rb, :, G * 512:(G + 1) * 512], st[:], [st], [kb.db('qk', rb, G)])
                if G + 1 < 8 and rb % 4 == 3:
                    front(G + 1, rb // 4)
        P.barrier()
        P.emit()


def phase_b_odd(kb, dram, l, ident):
    P = kb.P
    lambda_init = 0.8 - 0.6 * math.exp(-0.3 * l)
    with ExitStack() as pes:
        i4 = kb.sb(pes, [128, 512], BF16, 'i4')
        tri = kb.sb(pes, [128, 128], BF16, 'tri')
        zer = kb.sb(pes, [128, 512], BF16, 'zer')
        kb.dma(i4[:], dram['i4'][:, :], [], [i4])
        kb.dma(tri[:], dram['tri'][:, :], [], [tri])
        kb.memset('pool', zer[:], 0.0, [zer])
        lam = kb.sb(pes, [128, 256], F32, 'lam')
        lw = kb.sb(pes, [128, 128], F32, 'lw')
        lv = kb.sb(pes, [128, 4], F32, 'lv')
        subg = kb.sb(pes, [128, 128], F32, 'subg')
        kb.dma(lam[:], dram['lam%d' % l][0:1, :].partition_broadcast(128), [], [lam])
        kb.dma(subg[:], dram['subg%d' % l][0:1, :].partition_broadcast(128), [], [subg])
        kb.tt('dve', lw[:, 0:64], lam[:, 0:64], lam[:, 64:128], ALU.mult, [lam], [lw])
        kb.tt('dve', lw[:, 64:128], lam[:, 128:192], lam[:, 192:256], ALU.mult, [lam, lw], [lw])
        kb.P.op('dve', lambda e: e.reduce_sum(out=lv[:, 0:2], in_=lw[:].rearrange("p (a b) -> p a b", a=2), axis=AX.X), _bufs([lw]), _bufs([lv]))
        kb.act(lv[:, 0:2], lv[:, 0:2], AF.Exp, [lv], [lv])
        kb.tt('dve', lv[:, 2:3], lv[:, 1:2], lv[:, 0:1], ALU.subtract, [lv], [lv])
        kb.ts('dve', lv[:, 2:3], lv[:, 2:3], -lambda_init, ALU.add, [lv], [lv])
        kb.ts('dve', subg[:], subg[:], 1.0 - lambda_init, ALU.mult, [subg], [subg])

        kT = [kb.sb(pes, [128, T], BF16, 'kT') for _ in range(2)]
        vh = [kb.sb(pes, [128, NT, 130], BF16, 'vh') for _ in range(2)]
        for v_ in vh:
            kb.memset('pool', v_[:, :, 128:129], 1.0, [v_])
        qT = [kb.sb(pes, [128, 512], BF16, 'qT') for _ in range(2)]
        pT = [kb.sb(pes, [128, 512], BF16, 'pT') for _ in range(6)]
        oc = [kb.sb(pes, [128, 4, 128], F32, 'oc') for _ in range(2)]
        rd = [kb.sb(pes, [128, 4], F32, 'rd') for _ in range(2)]
        ao = kb.sb(pes, [128, 4, 128], F32, 'ao')
        junk = kb.sb(pes, [128, 128], F32, 'junk')
        s4 = kb.sb(pes, [128, 4], F32, 's4')
        ob = [kb.sb(pes, [128, 4, 128], BF16, 'ob') for _ in range(2)]
        psS = [kb.ps(pes, [128, 512], F32, 'psS') for _ in range(4)]
        pvb = [[kb.ps(pes, [128, 512], F32, 'pvb') for _ in range(2)] for _ in range(2)]
        ns = 0
        nq = 0
        for h in range(8):
            kt = kT[h % 2]
            vt = vh[h % 2]
            kb.dma(kt[:], dram['qk'][8 + h, :, :], [kb.db('qk', 8 + h, G) for G in range(8)], [kt])
            kb.dma(vt[:, :, 0:128], dram['vs'][:, h * 128:(h + 1) * 128].rearrange("(n p) d -> p n d", p=128),
                   [kb.db('vs', i) for i in range(NT)], [vt])
            for Q in range(8):
                qt = qT[nq % 2]
                nq += 1
                kb.dma(qt[:], dram['qk'][h, :, Q * 512:(Q + 1) * 512], [kb.db('qk', h, Q)], [qt])
                for c in range(2):
                    for bnk in pvb[c]:
                        kb.mm(bnk[:, 0:258], zer[:, 0:128], zer[:, 0:258], True, False, [zer], [bnk])
                pend = []
                for j in range(4 * Q + 4):
                    t0 = max(0, j - 4 * Q)
                    cur = []
                    for c in range(2):
                        lo = c * 64
                        hi = lo + 64
                        pS = psS[ns % 4]
                        pt = pT[ns % 6]
                        ns += 1
                        if j >= 4 * Q:
                            kb.mm(pS[:, t0 * 128:(t0 + 1) * 128], tri[:], i4[:, 0:128], True, False, [tri, i4], [pS])
                            kb.mm(pS[:, t0 * 128:512], kt[lo:hi, j * 128:(j + 1) * 128], qt[lo:hi, t0 * 128:512],
                                  False, True, [kt, qt], [pS])
                        else:
                            kb.mm(pS[:, :], kt[lo:hi, j * 128:(j + 1) * 128], qt[lo:hi, :], True, True, [kt, qt], [pS])
                        cur.append((c, pS, pt))
                    for c, pS, pt in cur:
                        kb.act(pt[:, t0 * 128:512], pS[:, t0 * 128:512], AF.Exp, [pS], [pt], scale=0.125)
                    for f in pend:
                        f()
                    pend = []
                    for c, pS, pt in cur:
                        def pvpart(t0=t0, pt=pt, j=j, pv2=pvb[c], vt=vt):
                            for t_ in range(t0, 4):
                                bnk = pv2[t_ // 2]
                                col = (t_ % 2) * 129
                                kb.mm(bnk[:, col:col + 129], pt[:, t_ * 128:(t_ + 1) * 128], vt[:, j, 0:129],
                                      False, False, [pt, vt], [bnk])
                        pend.append(pvpart)
                for f in pend:
                    f()
                pend = []
                for c in range(2):
                    pv2 = pvb[c]
                    for t_ in range(4):
                        bnk = pv2[t_ // 2]
                        col = (t_ % 2) * 129
                        kb.recip(rd[c][:, t_:t_ + 1], bnk[:, col + 128:col + 129], [bnk], [rd[c]])
                    if c == 1:
                        kb.ts('dve', rd[c][:], rd[c][:], lv[:, 2:3], ALU.mult, [rd[c], lv], [rd[c]])
                    for t_ in range(4):
                        bnk = pv2[t_ // 2]
                        col = (t_ % 2) * 129
                        kb.act(oc[c][:, t_, :], bnk[:, col:col + 128], AF.Copy, [bnk, rd[c]], [oc[c]], scale=rd[c][:, t_:t_ + 1])
                kb.tt('dve', ao[:], oc[0][:], oc[1][:], ALU.add, [oc[0], oc[1]], [ao])
                for t_ in range(4):
                    kb.act(junk[:], ao[:, t_, :], AF.Square, [ao], [junk, s4], accum_out=s4[:, t_:t_ + 1])
                kb.ts('dve', s4[:], s4[:], 1.0 / 128, ALU.mult, [s4], [s4], s2=EPS, op1=ALU.add)
                kb.act(s4[:], s4[:], AF.Sqrt, [s4], [s4])
                kb.recip(s4[:], s4[:], [s4], [s4])
                kb.tt('dve', ao[:], ao[:], s4[:].unsqueeze(2).broadcast_to([128, 4, 128]), ALU.mult, [ao, s4], [ao])
                o_ = ob[Q % 2]
                kb.tt('pool', o_[:], ao[:], subg[:].unsqueeze(1).broadcast_to([128, 4, 128]), ALU.mult, [ao, subg], [o_])
                kb.dma(dram['os'][Q * 512:(Q + 1) * 512, h * 128:(h + 1) * 128].rearrange("(n p) d -> p n d", p=128),
                       o_[:], [o_], [kb.db('os', Q, h)])
        P.barrier()
        P.emit()


def phase_b0_even(kb, dram, l):
    P = kb.P
    with ExitStack() as pes:
        w1 = [kb.sb(pes, [128, 32, 256], BF16, 'w1') for _ in range(2)]
        w2k = kb.sb(pes, [128, 2, 128], BF16, 'w2k')
        w2v = kb.sb(pes, [128, 2, 64], BF16, 'w2v')
        peT = kb.sb(pes, [128, 2, 32], F32, 'peT')
        srcs = [kb.sb(pes, [128, T], BF16, 'src') for _ in range(2)]
        xlo = [kb.sb(pes, [128, T], BF16, 'xlo') for _ in range(2)]
        xhi = [kb.sb(pes, [128, T], BF16, 'xhi') for _ in range(2)]
        hT = [kb.sb(pes, [128, 2, 2, 256], BF16, 'hT') for _ in range(2)]
        g0 = [kb.sb(pes, [128, 256], F32, 'g0') for _ in range(2)]
        g1 = [kb.sb(pes, [128, 256], F32, 'g1') for _ in range(2)]
        kcs = kb.sb(pes, [128, 256], BF16, 'kcs')
        vcs = kb.sb(pes, [128, 2, 2, 64], BF16, 'vcs')
        ph = [kb.ps(pes, [128, 512], F32, 'ph') for _ in range(2)]
        pk = [kb.ps(pes, [128, 512], F32, 'pk') for _ in range(2)]
        for kv in range(2):
            wv_ = dram['w1_%d' % l][kv].rearrange("(l d) f -> d l f", d=64)
            kb.dma(w1[kv][0:64, :, :], wv_, [], [w1[kv]], q='pool')
            kb.dma(w1[kv][64:128, :, :], wv_, [], [w1[kv]], q='pool')
            kb.dma(srcs[kv][:], dram['qk'][12 if kv == 0 else 15, :, :], [kb.db('qk', 12 if kv == 0 else 15, G) for G in range(8)], [srcs[kv]])
            kb.memset('pool', hT[kv][:, :, :, 255:256], 0.0, [hT[kv]])
        w2kv = dram['w2_%d' % l][0].rearrange("(c p) d -> p c d", p=128)
        kb.dma(w2k[:, :, 0:64], w2kv, [], [w2k], q='pool')
        kb.dma(w2k[:, :, 64:128], w2kv, [], [w2k], q='pool')
        kb.dma(w2v[:, :, :], dram['w2_%d' % l][1].rearrange("(c p) d -> p c d", p=128), [], [w2v], q='pool')
        kb.dma(peT[:, :, :], dram['peT%d' % l].rearrange("k p l -> p k l"), [], [peT])
        n = 0
        for kv in range(2):
            sv = srcs[kv][:].rearrange("p (n r) -> p n r", r=16)
            kb.tt('dve', xlo[kv][:].rearrange("p (n r) -> p n r", r=16), sv,
                  peT[:, kv, 0:16].unsqueeze(1).broadcast_to([128, 256, 16]), ALU.add, [srcs[kv], peT], [xlo[kv]])
            kb.tt('dve', xhi[kv][:].rearrange("p (n r) -> p n r", r=16), sv,
                  peT[:, kv, 16:32].unsqueeze(1).broadcast_to([128, 256, 16]), ALU.add, [srcs[kv], peT], [xhi[kv]])
            lov = xlo[kv][:].rearrange("p (n r) -> p n r", r=16)
            hiv = xhi[kv][:].rearrange("p (n r) -> p n r", r=16)
            for g in range(2):
                rows = slice(g * 64, g * 64 + 64)
                for fc in range(2):
                    p_ = ph[n % 2]
                    a0 = g0[n % 2]
                    a1 = g1[n % 2]
                    n += 1
                    for l_ in range(32):
                        rhs = lov[rows, 0:255, l_] if l_ < 16 else hiv[rows, 1:256, l_ - 16]
                        kb.mm(p_[:, 0:255], w1[kv][rows, l_, fc * 128:(fc + 1) * 128], rhs, l_ == 0, l_ == 31,
                              [w1[kv], xlo[kv], xhi[kv]], [p_])
                    kb.cp('act', a0[:, 0:255], p_[:, 0:255], [p_], [a0])
                    kb.tt('dve', a1[:, 0:255], a0[:, 0:255], a0[:, 0:255], ALU.mult, [a0], [a1])
                    kb.ts('dve', a1[:, 0:255], a1[:, 0:255], 0.044715, ALU.mult, [a1], [a1], s2=1.0, op1=ALU.add)
                    kb.tt('dve', a1[:, 0:255], a1[:, 0:255], a0[:, 0:255], ALU.mult, [a1, a0], [a1])
                    kb.act(a1[:, 0:255], a1[:, 0:255], AF.Tanh, [a1], [a1], scale=0.7978845608028654)
                    kb.ts('dve', a1[:, 0:255], a1[:, 0:255], 0.5, ALU.mult, [a1], [a1], s2=0.5, op1=ALU.add)
                    kb.tt('dve', hT[kv][:, g, fc, 0:255], a1[:, 0:255], a0[:, 0:255], ALU.mult, [a1, a0], [hT[kv]])
                if kv == 0:
                    p2 = pk[g]
                    for fc in range(2):
                        kb.mm(p2[:, 0:256], w2k[:, fc, :], hT[0][:, g, fc, :], fc == 0, fc == 1, [w2k, hT[0]], [p2])
                    kb.cp('act', kcs[rows, :], p2[rows, 0:256], [p2], [kcs])
                else:
                    for nch in range(2):
                        p2 = pk[nch]
                        for fc in range(2):
                            kb.mm(p2[:, 0:64], hT[1][:, g, fc, nch * 128:(nch + 1) * 128], w2v[:, fc, :], fc == 0, fc == 1,
                                  [w2v, hT[1]], [p2])
                        kb.cp('act', vcs[:, nch, g, :], p2[:, 0:64], [p2], [vcs])
        kb.dma(dram['kcs'][:, :], kcs[:], [kcs], [kb.db('kcs')])
        kb.dma(dram['vcs'][:, :], vcs[:].rearrange("p a g d -> p (a g d)"), [vcs], [kb.db('vcs')])
        P.barrier()
        P.emit()


def phase_b_even(kb, dram, l):
    P = kb.P
    with ExitStack() as pes:
        i4 = kb.sb(pes, [128, 512], BF16, 'i4')
        tri = kb.sb(pes, [128, 128], BF16, 'tri')
        wfar = kb.sb(pes, [128, 128], BF16, 'wfar')
        negm = kb.sb(pes, [128, 128], F32, 'negm')
        zer = kb.sb(pes, [128, 512], BF16, 'zer')
        kb.dma(i4[:], dram['i4'][:, :], [], [i4])
        kb.dma(tri[:], dram['tri'][:, :], [], [tri])
        kb.dma(wfar[:], dram['winfar'][:, :], [], [wfar])
        kb.dma(negm[:], dram['negm'][:, :], [], [negm])
        kb.memset('pool', zer[:], 0.0, [zer])
        kres = {}
        for nm, blk in (('ka', 4), ('ki', 7), ('ks', 13), ('kw', 14)):
            kres[nm] = kb.sb(pes, [128, T], BF16, nm)
            kb.dma(kres[nm][:], dram['qk'][blk, :, :], [kb.db('qk', blk, G) for G in range(8)], [kres[nm]])
        kcT = kb.sb(pes, [128, 256], BF16, 'kcT')
        kb.dma(kcT[:], dram['kcs'][:, :], [kb.db('kcs')], [kcT])
        vc = kb.sb(pes, [128, 2, 2, 130], BF16, 'vc')
        kb.dma(vc[:, :, :, 0:64], dram['vcs'].rearrange("p (a g d) -> p a g d", a=2, g=2), [kb.db('vcs')], [vc])
        kb.memset('pool', vc[:, :, :, 64:65], 1.0, [vc])
        for g in range(2):
            kb.dma(vc[:, :, g, 65:129], dram['ovl'].rearrange("(a p) m -> p a m", p=128), [], [vc])
        vt = kb.sb(pes, [128, NT, 5, 66], BF16, 'vt')
        kb.memset('pool', vt[:, :, :, 64:65], 1.0, [vt])
        for h5 in range(5):
            vsv = dram['vs'][:, h5 * 64:(h5 + 1) * 64].rearrange("(n p) d -> p n d", p=128)
            for q2 in range(2):
                kb.dma(vt[:, q2 * 16:(q2 + 1) * 16, h5, 0:64], vsv[:, q2 * 16:(q2 + 1) * 16, :], [kb.db('vs', i) for i in range(NT)], [vt])

        qs = [kb.sb(pes, [128, 10, 128], BF16, 'qs') for _ in range(2)]
        tmt = [kb.sb(pes, [128, 32], F32, 'tm') for _ in range(2)]
        cmt = [kb.sb(pes, [128, 256], BF16, 'cm') for _ in range(2)]
        fbt = [kb.sb(pes, [128, 64], F32, 'fb') for _ in range(2)]
        sc2 = [kb.sb(pes, [128, T], F32, 'sc') for _ in range(2)]
        rtmp = [kb.sb(pes, [128, 512], F32, 'rt') for _ in range(2)]
        m8 = kb.sb(pes, [128, 8], F32, 'm8')
        mb2 = [kb.sb(pes, [128, T], BF16, 'mb') for _ in range(2)]
        msel = [kb.sb(pes, [128, T], BF16, 'msel') for _ in range(2)]
        pT = [kb.sb(pes, [128, 512], BF16, 'pT') for _ in range(6)]
        rden = kb.sb(pes, [128, 4], F32, 'rden')
        coef = kb.sb(pes, [128, 4], F32, 'coef')
        imp = kb.sb(pes, [128, 64], F32, 'imp')
        scs = kb.sb(pes, [128, 64], F32, 'scs')
        scw = kb.sb(pes, [128, 64], F32, 'scw')
        selb = kb.sb(pes, [128, 64], BF16, 'selb')
        m8b = kb.sb(pes, [128, 8], F32, 'm8b')
        thr = kb.sb(pes, [128, 1], F32, 'thr')
        oacc2 = [kb.sb(pes, [128, 4, 64], F32, 'oacc') for _ in range(2)]
        ot = [kb.sb(pes, [128, D], BF16, 'ot') for _ in range(2)]
        psS = [kb.ps(pes, [128, 512], F32, 'psS') for _ in range(4)]
        pvb = [kb.ps(pes, [128, 512], F32, 'pvb') for _ in range(2)]
        pimp = kb.ps(pes, [128, 512], F32, 'pimp')
        pidx = [kb.ps(pes, [128, 512], F32, 'pidx') for _ in range(1)]
        cnt = {'s': 0, 'v': 0, 'x': 0}

        def attn_block(mask_lhsT, mask_r, kT_ap, k_r, q_ap, q_r, v_aps, pv, extra=None):
            pS = psS[cnt['s'] % 2]
            pt = pT[cnt['s'] % 3]
            cnt['s'] += 1
            if mask_lhsT is not None:
                kb.mm(pS[:, :], mask_lhsT, i4[:], True, False, [i4] + mask_r, [pS])
                kb.mm(pS[:, :], kT_ap, q_ap, False, True, k_r + q_r, [pS])
            else:
                kb.mm(pS[:, :], kT_ap, q_ap, True, True, k_r + q_r, [pS])
            kb.act(pt[:], pS[:, :], AF.Exp, [pS], [pt], scale=0.125)

            def pvpart():
                for hh in range(4):
                    kb.mm(pv[:, hh * 65:hh * 65 + 65], pt[:, hh * 128:(hh + 1) * 128], v_aps[0], False, False, [pt] + v_aps[1], [pv])
                    if extra is not None:
                        kb.mm(extra[0][:, hh * 64:hh * 64 + 64], pt[:, hh * 128:(hh + 1) * 128], extra[1], False, False,
                              [pt] + v_aps[1], [extra[0]])
            prev = pend[0]
            pend[0] = pvpart
            if prev is not None:
                prev()

        pend = [None]

        def flush_pv():
            if pend[0] is not None:
                f = pend[0]
                pend[0] = None
                f()

        def new_pv():
            pv = pvb[cnt['v'] % 2]
            cnt['v'] += 1
            kb.mm(pv[:, 0:260], zer[:, 0:128], zer[:, 0:260], True, False, [zer], [pv])
            return pv

        def get_rden(pv):
            flush_pv()
            dv = pv[:, 0:260].rearrange("p (h c) -> p h c", c=65)[:, :, 64]
            kb.ts('dve', rden[:, 0:4], dv, 1e-30, ALU.max, [pv], [rden])
            kb.recip(rden[:, 0:4], rden[:, 0:4], [rden], [rden])

        pw = kb.sb(pes, [128, 32], F32, 'pw')
        kb.dma(pw[:], dram['pw'][:, :], [], [pw])
        bmx = kb.sb(pes, [128, 1], F32, 'bmx')
        bmn = kb.sb(pes, [128, 1], F32, 'bmn')
        brng = kb.sb(pes, [128, 1], F32, 'brng')
        bthr = kb.sb(pes, [128, 1], F32, 'bthr')
        bcnt = kb.sb(pes, [128, 1], F32, 'bcnt')
        bw = kb.sb(pes, [128, 1], F32, 'bw')
        brk = kb.sb(pes, [128, 32], F32, 'brk')
        bjunk = kb.sb(pes, [128, T], BF16, 'bjunk')
        NSTEP = 17

        def stage1(i):
            N = (i + 1) * 128
            cols = slice(i * 128, (i + 1) * 128)
            rows_t = slice(i * 128, (i + 1) * 128)
            q_ = qs[i % 2]
            tm = tmt[i % 2]
            cm = cmt[i % 2]
            fb = fbt[i % 2]
            sc = sc2[i % 2]
            mb = mb2[i % 2]
            Gq = i // 4
            kb.dma(q_[:, 0:4, :], dram['qk'][0:4, :, cols].rearrange("b p t -> p b t"), [kb.db('qk', b_, Gq) for b_ in range(4)], [q_])
            kb.dma(q_[:, 4:6, :], dram['qk'][5:7, :, cols].rearrange("b p t -> p b t"), [kb.db('qk', b_, Gq) for b_ in (5, 6)], [q_])
            kb.dma(q_[:, 6:10, :], dram['qk'][8:12, :, cols].rearrange("b p t -> p b t"), [kb.db('qk', b_, Gq) for b_ in range(8, 12)], [q_])
            kb.dma(tm[:], dram['tm32'][rows_t, :], [kb.db('tm32', i)], [tm])
            kb.dma(cm[:], dram['cmask'][rows_t, :], [], [cm])
            kb.dma(fb[:], dram['fbias'][rows_t, :], [], [fb])
            ki = kres['ki']
            for ck in range((N + 511) // 512):
                c0 = ck * 512
                c1 = min(N, c0 + 512)
                w_ = c1 - c0
                for hi in range(4):
                    rows = slice((hi // 2) * 64, (hi // 2) * 64 + 64)
                    blk = 4 + (hi % 2)
                    pI = pidx[0]
                    rt = rtmp[cnt['x'] % 2]
                    cnt['x'] += 1
                    kb.mm(pI[:, 0:w_], q_[rows, blk, :], ki[rows, c0:c1], True, True, [q_, ki], [pI])
                    kb.act(rt[:, 0:w_], pI[:, 0:w_], AF.Relu, [pI], [rt])
                    if hi == 0:
                        kb.ts('pool', sc[:, c0:c1], rt[:, 0:w_], tm[:, 0:1], ALU.mult, [rt, tm], [sc], s2=0.0, op1=ALU.add)
                    else:
                        kb.ts('pool', rt[:, 0:w_], rt[:, 0:w_], tm[:, hi:hi + 1], ALU.mult, [rt, tm], [rt], s2=0.0, op1=ALU.add)
                        kb.tt('pool', sc[:, c0:c1], sc[:, c0:c1], rt[:, 0:w_], ALU.add, [rt, sc], [sc])
            kb.tt('pool', sc[:, cols], sc[:, cols], negm[:], ALU.add, [sc, negm], [sc])
            items = []
            if i < 2:
                items.append(lambda: kb.ts('dve', mb[:, 0:N], sc[:, 0:N], -1e29, ALU.is_lt, [sc], [mb], s2=NEG, op1=ALU.mult))
                return items
            def it_max():
                kb.P.op('dve', lambda e: e.tensor_reduce(out=bmx[:, 0:1], in_=sc[:, 0:N], axis=AX.X, op=ALU.max), _bufs([sc]), _bufs([bmx]))
            def it_min():
                kb.P.op('dve', lambda e: e.tensor_reduce(out=bmn[:, 0:1], in_=sc[:, 0:i * 128], axis=AX.X, op=ALU.min), _bufs([sc]), _bufs([bmn]))
                kb.tt('dve', brng[:, 0:1], bmx[:, 0:1], bmn[:, 0:1], ALU.subtract, [bmx, bmn], [brng])
                kb.stt('dve', bthr[:, 0:1], brng[:, 0:1], 0.5, bmn[:, 0:1], ALU.mult, ALU.add, [brng, bmn], [bthr])
                kb.ts('dve', brk[:, :], pw[:, :], brng[:, 0:1], ALU.mult, [pw, brng], [brk])
            items.append(it_max)
            items.append(it_min)
            for k in range(NSTEP):
                def it_step(k=k):
                    kb.ts('dve', bjunk[:, 0:N], sc[:, 0:N], bthr[:, 0:1], ALU.is_ge, [sc, bthr], [bjunk, bcnt], op1=ALU.add, accum_out=bcnt[:, 0:1])
                    kb.ts('dve', bw[:, 0:1], bcnt[:, 0:1], 255.5, ALU.is_gt, [bcnt], [bw], s2=0.5, op1=ALU.subtract)
                    kb.stt('dve', bthr[:, 0:1], bw[:, 0:1], brk[:, k:k + 1], bthr[:, 0:1], ALU.mult, ALU.add, [bw, brk, bthr], [bthr])
                items.append(it_step)
            def it_fin():
                kb.stt('dve', bthr[:, 0:1], brk[:, NSTEP:NSTEP + 1], -1.0, bthr[:, 0:1], ALU.mult, ALU.add, [brk, bthr], [bthr])
                kb.ts('dve', mb[:, 0:N], sc[:, 0:N], bthr[:, 0:1], ALU.is_lt, [sc, bthr], [mb], s2=NEG, op1=ALU.mult)
            items.append(it_fin)
            return items

        def pop(items, n):
            for _ in range(n):
                if items:
                    items.pop(0)()

        def qk_part(mask_lhsT, mask_r, kT_ap, k_r, q_ap, q_r):
            pS = psS[cnt['s'] % 4]
            pt = pT[cnt['s'] % 6]
            cnt['s'] += 1
            if mask_lhsT is not None:
                kb.mm(pS[:, :], mask_lhsT, i4[:], True, False, [i4] + mask_r, [pS])
                kb.mm(pS[:, :], kT_ap, q_ap, False, True, k_r + q_r, [pS])
            else:
                kb.mm(pS[:, :], kT_ap, q_ap, True, True, k_r + q_r, [pS])
            return pS, pt

        def pair_step(specs):
            cur = []
            for sp in specs:
                pS = psS[cnt['s'] % 4]
                pt = pT[cnt['s'] % 6]
                cnt['s'] += 1
                cur.append((pS, pt, sp))
            for pS, pt, sp in cur:
                if sp[0] is not None:
                    kb.mm(pS[:, :], sp[0], i4[:], True, False, [i4] + sp[1], [pS])
            for pS, pt, sp in cur:
                if sp[0] is not None:
                    kb.mm(pS[:, :], sp[2], sp[4], False, True, sp[3] + sp[5], [pS])
                else:
                    kb.mm(pS[:, :], sp[2], sp[4], True, True, sp[3] + sp[5], [pS])
            for pS, pt, sp in cur:
                kb.act(pt[:], pS[:, :], AF.Exp, [pS], [pt], scale=0.125)
            old = pend2[:]
            del pend2[:]
            for f in old:
                f()
            for pS, pt, sp in cur:
                def pvpart(pt=pt, sp=sp):
                    v_ap, v_r, pv, extra = sp[6], sp[7], sp[8], sp[9]
                    for hh in range(4):
                        kb.mm(pv[:, hh * 65:hh * 65 + 65], pt[:, hh * 128:(hh + 1) * 128], v_ap, False, False, [pt] + v_r, [pv])
                        if extra is not None:
                            kb.mm(extra[0][:, extra[2] + hh * 64:extra[2] + hh * 64 + 64], pt[:, hh * 128:(hh + 1) * 128], extra[1],
                                  False, False, [pt] + v_r, [extra[0]])
                pend2.append(pvpart)

        pend2 = []

        def flush2():
            old = pend2[:]
            del pend2[:]
            for f in old:
                f()

        def new_pair():
            for pv in pvb:
                kb.mm(pv[:, 0:260], zer[:, 0:128], zer[:, 0:260], True, False, [zer], [pv])
            return pvb

        def rden_of(pv):
            dv = pv[:, 0:260].rearrange("p (h c) -> p h c", c=65)[:, :, 64]
            kb.ts('dve', rden[:, 0:4], dv, 1e-30, ALU.max, [pv], [rden])
            kb.recip(rden[:, 0:4], rden[:, 0:4], [rden], [rden])

        def stage2(i, nxt):
            N = (i + 1) * 128
            cols = slice(i * 128, (i + 1) * 128)
            rows_t = slice(i * 128, (i + 1) * 128)
            q_ = qs[i % 2]
            tm = tmt[i % 2]
            cm = cmt[i % 2]
            fb = fbt[i % 2]
            mb = mb2[i % 2]
            o_ = ot[i % 2]
            per = (len(nxt) + 3) // 4
            R = [slice(0, 64), slice(64, 128)]
            ka = kres['ka']
            pvp = new_pair()
            for j in range(i + 1):
                pair_step([(mb[:, j * 128:(j + 1) * 128], [mb], ka[R[g], j * 128:(j + 1) * 128], [ka], q_[R[g], 0:4, :], [q_],
                            vt[:, j, 0, 0:65], [vt], pvp[g], None) for g in range(2)])
            flush2()
            for grp in range(2):
                rden_of(pvp[grp])
                for hh in range(4):
                    hd = grp * 4 + hh
                    kb.act(o_[:, hd * 64:(hd + 1) * 64], pvp[grp][:, hh * 65:hh * 65 + 64], AF.Copy, [pvp[grp], rden], [o_], scale=rden[:, hh:hh + 1])
            pop(nxt, per)
            gates = tm[:, 4:28].rearrange("p (g h b) -> p g h b", g=2, h=4)
            nchk = 1 if i < 16 else 2
            qg = [q_[R[g], 6:10, :] for g in range(2)]
            pvp = new_pair()
            kb.mm(pimp[:, 0:512], zer[:, 0:128], zer[:, 0:512], True, False, [zer], [pimp])
            for nch in range(nchk):
                pair_step([(cm[:, nch * 128:(nch + 1) * 128], [cm], kcT[R[g], nch * 128:(nch + 1) * 128], [kcT], qg[g], [q_],
                            vc[:, nch, g, 0:65], [vc], pvp[g], (pimp, vc[:, nch, g, 65:129], g * 256)) for g in range(2)])
            flush2()
            for g in range(2):
                pv = pvp[g]
                rden_of(pv)
                for hh in range(4):
                    pc = g * 256 + hh * 64
                    if hh == 0:
                        kb.ts('dve', imp[:], pimp[:, pc:pc + 64], rden[:, 0:1], ALU.mult, [pimp, rden], [imp])
                    else:
                        kb.stt('dve', imp[:], pimp[:, pc:pc + 64], rden[:, hh:hh + 1], imp[:], ALU.mult, ALU.add, [pimp, rden, imp], [imp])
                kb.tt('dve', coef[:, 0:4], rden[:, 0:4], gates[:, g, :, 0], ALU.mult, [rden, tm], [coef])
                for hh in range(4):
                    kb.ts('dve', oacc2[g][:, hh, :], pv[:, hh * 65:hh * 65 + 64], coef[:, hh:hh + 1], ALU.mult, [pv, coef], [oacc2[g]])
                kb.tt('dve', scs[:], imp[:], fb[:], ALU.add, [imp, fb], [scs])
                kb.P.op('dve', lambda e, a=m8b[:], b=scs[:]: e.max(out=a, in_=b), _bufs([scs]), _bufs([m8b]))
                kb.P.op('dve', lambda e, a=scw[:], b=m8b[:], c=scs[:]: e.match_replace(out=a, in_to_replace=b, in_values=c, imm_value=-3e9),
                        _bufs([scs, m8b]), _bufs([scw]))
                kb.P.op('dve', lambda e, a=m8b[:], b=scw[:]: e.max(out=a, in_=b), _bufs([scw]), _bufs([m8b]))
                kb.ts('dve', thr[:, 0:1], m8b[:, 7:8], -1e8, ALU.max, [m8b], [thr])
                kb.ts('dve', selb[:], scs[:], thr[:, 0:1], ALU.is_lt, [scs, thr], [selb], s2=NEG, op1=ALU.mult)
                ms = msel[g]
                nb = N // 64
                kb.cp('pool', ms[:, 0:N].rearrange("p (m s) -> p m s", s=64), selb[:, 0:nb].unsqueeze(2).broadcast_to([128, nb, 64]), [selb], [ms])
                kb.tt('pool', ms[:, cols], ms[:, cols], tri[:], ALU.add, [ms, tri], [ms])
            kw_ = kres['kw']
            pvp = new_pair()
            for j in range(max(0, i - 4), i + 1):
                if j == i:
                    ml, mr = tri[:], [tri]
                elif j == i - 4:
                    ml, mr = wfar[:], [wfar]
                else:
                    ml, mr = None, []
                pair_step([(ml, mr, kw_[R[g], j * 128:(j + 1) * 128], [kw_], qg[g], [q_], vt[:, j, 3 + g, 0:65], [vt], pvp[g], None)
                           for g in range(2)])
            flush2()
            for g in range(2):
                pv = pvp[g]
                rden_of(pv)
                kb.tt('dve', coef[:, 0:4], rden[:, 0:4], gates[:, g, :, 2], ALU.mult, [rden, tm], [coef])
                for hh in range(4):
                    kb.stt('dve', oacc2[g][:, hh, :], pv[:, hh * 65:hh * 65 + 64], coef[:, hh:hh + 1], oacc2[g][:, hh, :], ALU.mult, ALU.add,
                           [pv, coef, oacc2[g]], [oacc2[g]])
            pop(nxt, per)
            ks = kres['ks']
            pvp = new_pair()
            for j in range(i + 1):
                pair_step([(msel[g][:, j * 128:(j + 1) * 128], [msel[g]], ks[R[g], j * 128:(j + 1) * 128], [ks], qg[g], [q_],
                            vt[:, j, 1 + g, 0:65], [vt], pvp[g], None) for g in range(2)])
            flush2()
            for g in range(2):
                pv = pvp[g]
                rden_of(pv)
                kb.tt('dve', coef[:, 0:4], rden[:, 0:4], gates[:, g, :, 1], ALU.mult, [rden, tm], [coef])
                for hh in range(4):
                    kb.stt('dve', oacc2[g][:, hh, :], pv[:, hh * 65:hh * 65 + 64], coef[:, hh:hh + 1], oacc2[g][:, hh, :], ALU.mult, ALU.add,
                           [pv, coef, oacc2[g]], [oacc2[g]])
                kb.cp('pool', o_[:, 512 + g * 256:512 + (g + 1) * 256], oacc2[g][:].rearrange("p h d -> p (h d)"), [oacc2[g]], [o_])
            pop(nxt, 2 * per)
            kb.dma(dram['os'][rows_t, :], o_[:], [o_], [kb.db('os', i // 4, hh_) for hh_ in range(8)])

        items0 = stage1(0)
        pop(items0, len(items0))
        for i in range(NT):
            nxt = stage1(i + 1) if i + 1 < NT else []
            stage2(i, nxt)
            pop(nxt, len(nxt))
        P.barrier()
        P.emit()


def phase_c(kb, dram, l, hsrc, ident, dst, final_norm):
    P = kb.P
    with ExitStack() as pes:
        wu = kb.sb(pes, [128, 8, DFF], BF16, 'wu')
        wd = kb.sb(pes, [128, 32, D], BF16, 'wd')
        wo = kb.sb(pes, [128, 8, D], BF16, 'wo')
        g_sb = kb.sb(pes, [128, 8], F32, 'g')
        kb.dma(g_sb[:], dram['mlp_g'][l], [], [g_sb])
        for c in range(8):
            kb.dma(wo[:, c, :], dram['w_out%d' % l][c * 128:(c + 1) * 128, :], [], [wo], q='pool')
        for c in range(8):
            kb.dma(wu[:, c, :], dram['w_up%d' % l][c * 128:(c + 1) * 128, :], [], [wu], q='pool')
        wdv = dram['w_down%d' % l].rearrange("(c p) n -> p c n", p=128)
        for c in range(8):
            kb.dma(wd[:, 4 * c:4 * c + 4, :], wdv[:, 4 * c:4 * c + 4, :], [], [wd], q='pool')
        if final_norm:
            fg = kb.sb(pes, [128, D], F32, 'fg')
            kb.dma(fg[:], dram['final_g'][0:1, :].partition_broadcast(128), [], [fg])
        ht = [kb.sb(pes, [128, D], F32, 'ht') for _ in range(2)]
        ot = [kb.sb(pes, [128, D], BF16, 'ot') for _ in range(2)]
        oT = kb.sb(pes, [128, 8, 128], BF16, 'oT')
        junk = kb.sb(pes, [128, D], BF16, 'junk')
        ss = kb.sb(pes, [128, 1], F32, 'ss')
        rstd = kb.sb(pes, [128, 1], F32, 'rstd')
        xn = kb.sb(pes, [128, D], BF16, 'xn')
        uT = kb.sb(pes, [128, 8, 128], BF16, 'uT')
        rl = [kb.sb(pes, [128, 512], F32, 'rl') for _ in range(2)]
        aT = kb.sb(pes, [128, 32, 128], BF16, 'aT')
        pst = [kb.ps(pes, [128, 1024], BF16, 'pst') for _ in range(2)]
        pu = [kb.ps(pes, [128, 512], F32, 'pu') for _ in range(2)]
        pd = [kb.ps(pes, [128, 512], F32, 'pd') for _ in range(2)]
        ss2 = kb.sb(pes, [128, 1], F32, 'ss2')
        rstd2 = kb.sb(pes, [128, 1], F32, 'rstd2')

        def front_a(i):
            h_ = ht[i % 2]
            o_ = ot[i % 2]
            rows = slice(i * 128, (i + 1) * 128)
            kb.dma(h_[:], hsrc[rows, :], [kb.db('h', i)], [h_])
            kb.dma(o_[:], dram['os'][rows, :], [kb.db('os', i // 4, hh) for hh in range(8)], [o_])
            for c in range(8):
                kb.tr(pst[0][:, c * 128:(c + 1) * 128], o_[:, c * 128:(c + 1) * 128], ident[:], [o_, ident], [pst[0]])
            kb.cp('act', oT[:].rearrange("p c t -> p (c t)"), pst[0][:, :], [pst[0]], [oT])

        def front_b(i):
            h_ = ht[i % 2]
            for half in range(2):
                for c in range(8):
                    kb.mm(pd[half][:, :], oT[:, c, :], wo[:, c, half * 512:(half + 1) * 512], c == 0, c == 7, [oT, wo], [pd[half]])
                kb.tt('dve', h_[:, half * 512:(half + 1) * 512], pd[half][:, :], h_[:, half * 512:(half + 1) * 512],
                      ALU.add, [pd[half], h_], [h_])
            kb.rms_rstd(h_, junk, ss, rstd, D)
            kb.ts('dve', xn[:], h_[:], rstd[:, 0:1], ALU.mult, [h_, rstd], [xn])

        def front_c(i):
            for c in range(8):
                kb.tr(pst[1][:, c * 128:(c + 1) * 128], xn[:, c * 128:(c + 1) * 128], ident[:], [xn, ident], [pst[1]])
            kb.tt('dve', uT[:], pst[1][:].rearrange("p (c t) -> p c t", c=8),
                  g_sb[:].unsqueeze(2).broadcast_to([128, 8, 128]), ALU.mult, [pst[1], g_sb], [uT])

        def up(i, g0, g1):
            for fg4 in range(g0, g1):
                put = pu[fg4 % 2]
                for f_ in range(4):
                    fc = fg4 * 4 + f_
                    for c in range(8):
                        kb.mm(put[:, f_ * 128:(f_ + 1) * 128], wu[:, c, fc * 128:(fc + 1) * 128], uT[:, c, :],
                              c == 0, c == 7, [wu, uT], [put])
                r_ = rl[fg4 % 2]
                kb.act(r_[:], put[:, :], AF.Relu, [put], [r_])
                kb.tt('pool', aT[:, fg4 * 4:fg4 * 4 + 4, :].rearrange("p c t -> p (c t)"), r_[:], r_[:], ALU.mult, [r_], [aT])

        def down(i):
            h_ = ht[i % 2]
            rows = slice(i * 128, (i + 1) * 128)
            for half in range(2):
                for fc in range(32):
                    kb.mm(pd[half][:, :], aT[:, fc, :], wd[:, fc, half * 512:(half + 1) * 512], fc == 0, fc == 31, [aT, wd], [pd[half]])
                kb.tt('dve', h_[:, half * 512:(half + 1) * 512], pd[half][:, :], h_[:, half * 512:(half + 1) * 512],
                      ALU.add, [pd[half], h_], [h_])
            if final_norm:
                kb.rms_rstd(h_, junk, ss2, rstd2, D)
                kb.stt('dve', h_[:], h_[:], rstd2[:, 0:1], fg[:], ALU.mult, ALU.mult, [h_, rstd2, fg], [h_])
            kb.dma(dst[rows, :], h_[:], [h_], [kb.db('h', i) if not final_norm else kb.db('y', i)])

        front_a(0)
        front_b(0)
        front_c(0)
        for i in range(NT):
            nx = i + 1 < NT
            if nx:
                front_a(i + 1)
            up(i, 0, 4)
            if nx:
                front_b(i + 1)
            up(i, 4, 8)
            down(i)
            if nx:
                front_c(i + 1)
        P.barrier()
        P.emit()


def _bf(a):
    return np.asarray(a, dtype=np.float32).astype(ml_dtypes.bfloat16)


def host_consts():
    c = {}
    c['ident'] = _bf(np.eye(128))
    c['i4'] = _bf(np.tile(np.eye(128), (1, 4)))
    t = np.arange(128)[:, None]
    s = np.arange(128)[None, :]
    c['tri'] = _bf(np.where(s <= t, 0.0, NEG))
    inv = 1.0 / (10000.0 ** (np.arange(0, 64, 2, dtype=np.float32) / 64))
    ang = np.arange(T, dtype=np.float32)[None, :] * inv[:, None].astype(np.float32)
    cos = np.cos(ang).astype(np.float32)
    sin = np.sin(ang).astype(np.float32)
    c['winfar'] = _bf(np.where(s > t, 0.0, NEG))
    c['negm'] = np.where(s <= t, 0.0, -1e30).astype(np.float32)
    tq = np.arange(T)[:, None]
    nn = np.arange(256)[None, :]
    c['cmask'] = _bf(np.where((16 * nn + 31 <= tq) & (nn < 255), 0.0, NEG))
    mm_ = np.arange(64)[None, :]
    cur = tq // 64
    fbv = np.where(mm_ * 64 <= tq, 0.0, -1e9)
    fbv = np.where(mm_ == cur - 1, 1e9, fbv)
    fbv = np.where(mm_ == cur, 2e9, fbv)
    fbv = np.where(mm_ == 0, 3e9, fbv)
    c['fbias'] = fbv.astype(np.float32)
    n_ = np.arange(256)[:, None]
    c0_ = n_ * 16
    ov = ((c0_ < (mm_ + 1) * 64) & (c0_ + 32 > mm_ * 64) & (n_ < 255))
    c['ovl'] = _bf(ov.astype(np.float32))
    pp = np.arange(128)
    sw = np.where((pp % 64) < 32, pp + 32, pp - 32)
    pm = np.zeros((128, 128), np.float32)
    pm[sw, pp] = 1.0
    c['pswap'] = _bf(pm)
    c['pw'] = np.ascontiguousarray(np.tile((2.0 ** -(np.arange(32) + 1.0))[None, :], (128, 1)).astype(np.float32))
    c['ropeC'] = np.ascontiguousarray(np.concatenate([cos, cos, cos, cos], 0))
    c['ropeS'] = np.ascontiguousarray(np.concatenate([-sin, sin, -sin, sin], 0))
    return c


def swap_cols(w):
    d, n = w.shape
    return np.ascontiguousarray(w.reshape(d, n // 64, 2, 32)[:, :, ::-1, :].reshape(d, n))


def even_cols():
    qa0, ka0, va0, qi0, ki0, wi0, qb0, kv0, gb0 = 0, 512, 576, 640, 896, 960, 964, 1476, 2244

    def hd(base, h):
        return list(range(base + h * 64, base + (h + 1) * 64))

    def kvh(s6, g):
        return hd(kv0, s6 * 2 + g)
    blocks = []
    for r in range(4):
        blocks.append(hd(qa0, r) + hd(qa0, 4 + r))
    blocks.append(hd(ka0, 0) * 2)
    for r in range(2):
        blocks.append(hd(qi0, r) + hd(qi0, 2 + r))
    blocks.append(hd(ki0, 0) * 2)
    for r in range(4):
        blocks.append(hd(qb0, r) + hd(qb0, 4 + r))
    blocks.append(kvh(0, 0) + kvh(0, 1))
    blocks.append(kvh(2, 0) + kvh(2, 1))
    blocks.append(kvh(4, 0) + kvh(4, 1))
    blocks.append(kvh(1, 0) + kvh(1, 1))
    fm = [c for b in blocks for c in b]
    tmc = hd(va0, 0) + kvh(3, 0) + kvh(3, 1) + kvh(5, 0) + kvh(5, 1) + list(range(wi0, wi0 + 4)) + list(range(gb0, gb0 + 24))
    assert len(fm) == 2048 and len(tmc) == 348
    return fm, tmc


def gain_layout(g):
    return np.ascontiguousarray(g.reshape(g.shape[0], 8, 128).transpose(0, 2, 1))


def host_inputs(inputs, layers):
    m = dict(host_consts())
    m['mix_g'] = gain_layout(np.asarray(inputs['mix_norm_g'], np.float32))
    m['mlp_g'] = gain_layout(np.asarray(inputs['mlp_norm_g'], np.float32))
    m['final_g'] = np.asarray(inputs['final_norm_g'], np.float32).reshape(1, D)
    for l in layers:
        m['w_up%d' % l] = np.ascontiguousarray(inputs['mlp_w_up'][l])
        m['w_down%d' % l] = np.ascontiguousarray(inputs['mlp_w_down'][l])
        if l % 2 == 0:
            e = l // 2
            w = np.asarray(inputs['even_w_in'][e], np.float32)
            fm, tmc = even_cols()
            wqk = np.ascontiguousarray(w[:, fm])
            m['wqk%d' % l] = wqk
            m['wv%d' % l] = np.ascontiguousarray(w[:, tmc])
            m['w1_%d' % l] = np.ascontiguousarray(inputs['even_cmp_w1'][e], dtype=np.float32)
            m['w2_%d' % l] = np.ascontiguousarray(inputs['even_cmp_w2'][e], dtype=np.float32)
            pe = np.asarray(inputs['even_cmp_pe'][e], np.float32)
            peT = pe.transpose(0, 2, 1)
            m['peT%d' % l] = np.ascontiguousarray(np.concatenate([peT, peT], 1))
            m['w_out%d' % l] = np.ascontiguousarray(inputs['even_w_out'][e])
        if l % 2 == 1:
            o = l // 2
            w = np.asarray(inputs['odd_w_in'][o], np.float32)
            wqk = np.ascontiguousarray(w[:, 0:2048])
            m['wqk%d' % l] = wqk
            m['wv%d' % l] = np.ascontiguousarray(w[:, 2048:3072])
            m['lam%d' % l] = np.asarray(inputs['odd_lambda'][o], np.float32).reshape(1, 256)
            m['subg%d' % l] = np.asarray(inputs['odd_subln_g'][o], np.float32).reshape(1, 128)
            m['w_out%d' % l] = np.ascontiguousarray(inputs['odd_w_out'][o])
    return m


def kernel(**inputs):
    layers = [0, 1, 2, 3]
    inputs = {k: np.asarray(v) for k, v in inputs.items()}
    nc = build_program(dict(layers=layers, final_norm=True))
    shared = host_inputs(inputs, layers)
    in_maps = []
    for c in range(N_CORES):
        m = dict(shared)
        m['x'] = np.ascontiguousarray(inputs['x'][c % 4])
        in_maps.append(m)
    res = run_bass_kernel_spmd(nc, in_maps, core_ids=list(range(N_CORES)))
    return np.stack([res.results[b]['y'] for b in range(4)], 0).astype(np.float32)
```

```python
import math
import numpy as np
from contextlib import ExitStack
import ml_dtypes
import concourse.bass as bass
import concourse.mybir as mybir
from concourse.bass_utils import run_bass_kernel_spmd

F32 = mybir.dt.float32
BF16 = mybir.dt.bfloat16
AF = mybir.ActivationFunctionType
ALU = mybir.AluOpType
AX = mybir.AxisListType

T = 4096
D = 1024
NT = 32
DFF = 4096
EPS = 1e-6
NEG = -30000.0
N_CORES = 8


class Buf:
    __slots__ = ('name', 'lw', 'rd')

    def __init__(self, name=''):
        self.name = name
        self.lw = None
        self.rd = {}


class Tl:
    def __init__(self, t, name=''):
        self.t = t
        self.b = Buf(name)

    def __getitem__(self, k):
        return self.t[k]


def _bufs(lst):
    out = []
    for x in lst:
        if x is None:
            continue
        out.append(x.b if isinstance(x, Tl) else x)
    return out


class Prog:
    CE = ('pe', 'act', 'dve', 'pool')

    def __init__(self, nc, es, n_dma_sems=12):
        self.nc = nc
        self.ops = {e: [] for e in ('pe', 'act', 'dve', 'pool', 'sp')}
        self.sems = {}
        for e in self.CE:
            self.sems[e] = es.enter_context(nc.semaphore('s_' + e))
        self.cnt = {e: 0 for e in self.CE}
        self.dq = {}
        for q in ('sp', 'pool', 'act'):
            n = n_dma_sems if q == 'sp' else 6
            ss = []
            for i in range(n):
                k = 'd_%s_%d' % (q, i)
                self.sems[k] = es.enter_context(nc.semaphore(k))
                self.cnt[k] = 0
                ss.append(k)
            self.dq[q] = [ss, 0]
        self.waited = {e: {} for e in self.ops}
        self.same_engine_sync = {'pe': False, 'act': True, 'dve': True, 'pool': True, 'sp': True}
        self.total = 0

    def _deps(self, reads, writes):
        deps = {}
        for b in reads:
            if b.lw is not None:
                k, v = b.lw
                if deps.get(k, 0) < v:
                    deps[k] = v
        for b in writes:
            if b.lw is not None:
                k, v = b.lw
                if deps.get(k, 0) < v:
                    deps[k] = v
            for k, v in b.rd.items():
                if deps.get(k, 0) < v:
                    deps[k] = v
        return deps

    def _mark(self, tok, reads, writes):
        k, v = tok
        for b in reads:
            if b.rd.get(k, 0) < v:
                b.rd[k] = v
        for b in writes:
            b.lw = tok
            b.rd = {}

    def op(self, eng, fn, reads=(), writes=()):
        reads = _bufs(reads)
        writes = _bufs(writes)
        deps = self._deps(reads, writes)
        waits = []
        wd = self.waited[eng]
        for k, v in deps.items():
            if k == eng and not self.same_engine_sync[eng]:
                continue
            if wd.get(k, 0) >= v:
                continue
            wd[k] = v
            waits.append((k, v))
        self.cnt[eng] += 1
        tok = (eng, self.cnt[eng])
        self.ops[eng].append((waits, fn, eng, 1))
        self._mark(tok, reads, writes)
        self.total += 1

    def dma(self, fn, reads=(), writes=(), q='sp', inc=16):
        reads = _bufs(reads)
        writes = _bufs(writes)
        ss, i = self.dq[q]
        k = ss[i % len(ss)]
        self.dq[q][1] = i + 1
        deps = self._deps(reads, writes)
        if self.cnt[k] > 0 and deps.get(k, 0) < self.cnt[k]:
            deps[k] = self.cnt[k]
        waits = []
        wd = self.waited[q]
        for kk, v in deps.items():
            if wd.get(kk, 0) >= v:
                continue
            wd[kk] = v
            waits.append((kk, v))
        self.cnt[k] += inc
        tok = (k, self.cnt[k])
        self.ops[q].append((waits, fn, k, inc))
        self._mark(tok, reads, writes)
        self.total += 1

    def barrier(self):
        for e in self.ops:
            waits = []
            wd = self.waited[e]
            for k, v in self.cnt.items():
                if v > 0 and wd.get(k, 0) < v:
                    wd[k] = v
                    waits.append((k, v))
            if waits:
                self.ops[e].append((waits, None, None, 0))

    def emit(self):
        nc = self.nc
        sems = self.sems

        def mk(name):
            lst = self.ops[name]

            def body(e):
                for waits, fn, sk, inc in lst:
                    for k, v in waits:
                        e.wait_ge(sems[k], v)
                    if fn is not None:
                        fn(e).then_inc(sems[sk], inc)
            return body

        with nc.Block() as block:
            block.tensor(mk('pe'))
            block.scalar(mk('act'))
            block.vector(mk('dve'))
            block.gpsimd(mk('pool'))
            block.sync(mk('sp'))
        self.ops = {e: [] for e in self.ops}


class KB:
    def __init__(self, nc, es):
        self.nc = nc
        self.P = Prog(nc, es)
        self.dbufs = {}
        self.uid = 0

    def sb(self, es, shape, dt, name=None):
        self.uid += 1
        name = (name or 't') + '_%d' % self.uid
        return Tl(es.enter_context(self.nc.sbuf_tensor(name, list(shape), dt)), name)

    def ps(self, es, shape, dt, name=None):
        self.uid += 1
        name = (name or 'p') + '_%d' % self.uid
        return Tl(es.enter_context(self.nc.psum_tensor(name, list(shape), dt)), name)

    def db(self, *key):
        b = self.dbufs.get(key)
        if b is None:
            b = Buf(str(key))
            self.dbufs[key] = b
        return b

    def mm(self, out, lhsT, rhs, start, stop, r, w):
        self.P.op('pe', lambda e: e.matmul(out, lhsT=lhsT, rhs=rhs, start=start, stop=stop,
                                           skip_group_check=True), r, w)

    def tr(self, out, in_, ident, r, w):
        self.P.op('pe', lambda e: e.transpose(out=out, in_=in_, identity=ident), r, w)

    def act(self, out, in_, func, r, w, scale=None, bias=None, accum_out=None, eng='act'):
        kw = {}
        if scale is not None:
            kw['scale'] = scale
        if bias is not None:
            kw['bias'] = bias
        if accum_out is not None:
            kw['accum_out'] = accum_out
        self.P.op('act', lambda e: e.activation(out=out, in_=in_, func=func, **kw), r, w)

    def ts(self, eng, out, in0, s1, op0, r, w, s2=None, op1=None, accum_out=None):
        kw = {}
        if op1 is not None:
            kw['op1'] = op1
        if accum_out is not None:
            kw['accum_out'] = accum_out
        self.P.op(eng, lambda e: e.tensor_scalar(out=out, in0=in0, scalar1=s1, scalar2=s2, op0=op0, **kw), r, w)

    def tt(self, eng, out, in0, in1, op, r, w):
        self.P.op(eng, lambda e: e.tensor_tensor(out=out, in0=in0, in1=in1, op=op), r, w)

    def stt(self, eng, out, in0, scalar, in1, op0, op1, r, w):
        self.P.op(eng, lambda e: e.scalar_tensor_tensor(out=out, in0=in0, scalar=scalar, in1=in1, op0=op0, op1=op1), r, w)

    def cp(self, eng, out, in_, r, w):
        if eng == 'act':
            self.P.op('act', lambda e: e.copy(out=out, in_=in_), r, w)
        else:
            self.P.op(eng, lambda e: e.tensor_copy(out=out, in_=in_), r, w)

    def memset(self, eng, ap, val, w):
        self.P.op(eng, lambda e: e.memset(ap, val), (), w)

    def recip(self, out, in_, r, w):
        self.P.op('dve', lambda e: e.reciprocal(out=out, in_=in_), r, w)

    def dma(self, out, in_, r, w, q='sp'):
        self.P.dma(lambda e: e.dma_start(out=out, in_=in_), r, w, q=q)

    def rms_rstd(self, h, junk, ss, rstd, width):
        self.act(junk[:, 0:width], h[:, 0:width], AF.Square, [h], [junk, ss], accum_out=ss[:, 0:1])
        self.ts('dve', ss[:, 0:1], ss[:, 0:1], 1.0 / width, ALU.mult, [ss], [ss], s2=EPS, op1=ALU.add)
        self.act(ss[:, 0:1], ss[:, 0:1], AF.Sqrt, [ss], [ss])
        self.recip(rstd[:, 0:1], ss[:, 0:1], [ss], [rstd])


def build_program(cfg):
    nc = bass.Bass("TRN2", target_bir_lowering=False)
    layers = cfg['layers']
    dram = {}

    def din(name, shape, dt=F32):
        dram[name] = nc.dram_tensor(name, list(shape), dt, kind="ExternalInput").ap()
        return dram[name]

    def dscr(name, shape, dt):
        dram[name] = nc.dram_tensor(name, list(shape), dt, kind="Internal").ap()
        return dram[name]

    x = din('x', [T, D])
    y = nc.dram_tensor('y', [T, D], F32, kind="ExternalOutput").ap()
    din('mix_g', [4, 128, 8])
    din('mlp_g', [4, 128, 8])
    din('final_g', [1, D])
    din('ident', [128, 128], BF16)
    din('i4', [128, 512], BF16)
    din('tri', [128, 128], BF16)
    din('ropeC', [128, T])
    din('ropeS', [128, T])
    din('winfar', [128, 128], BF16)
    din('negm', [128, 128])
    din('cmask', [T, 256], BF16)
    din('fbias', [T, 64])
    din('ovl', [256, 64], BF16)
    din('pw', [128, 32])
    for l in layers:
        din('w_up%d' % l, [D, DFF])
        din('w_down%d' % l, [DFF, D])
        din('w_out%d' % l, [D, D])
        if l % 2 == 0:
            din('wqk%d' % l, [D, 2048])
            din('wqks%d' % l, [D, 1920])
            din('wv%d' % l, [D, 348])
            din('w1_%d' % l, [2, 2048, 256])
            din('w2_%d' % l, [2, 256, 64])
            din('peT%d' % l, [2, 128, 32])
        if l % 2 == 1:
            din('wqk%d' % l, [D, 2048])
            din('wqks%d' % l, [D, 2048])
            din('wv%d' % l, [D, 1024])
            din('lam%d' % l, [1, 256])
            din('subg%d' % l, [1, 128])
    hs = dscr('hs', [T, D], F32)
    qk = dscr('qk', [20, 128, T], BF16)
    vs = dscr('vs', [T, 1024], BF16)
    os_ = dscr('os', [T, D], BF16)
    dscr('tm32', [T, 32], F32)
    dscr('kcs', [128, 256], BF16)
    dscr('vcs', [128, 256], BF16)

    with ExitStack() as es:
        kb = KB(nc, es)
        P = kb.P
        ident = kb.sb(es, [128, 128], BF16, 'ident')
        kb.dma(ident[:], dram['ident'][:, :], [], [ident])
        hsrc = x
        for li, l in enumerate(layers):
            if l % 2 == 1:
                phase_a(kb, dram, l, hsrc, ident)
                phase_b_odd(kb, dram, l, ident)
            else:
                phase_a(kb, dram, l, hsrc, ident)
                phase_b0_even(kb, dram, l)
                phase_b_even(kb, dram, l)
            last = (li == len(layers) - 1)
            phase_c(kb, dram, l, hsrc, ident, y if last else hs, last and cfg.get('final_norm', True))
            hsrc = hs
    return nc


def load_norm_transpose(kb, pes, h_ap, hb, g_sb, ident, ht, junk, ss, rstd, xn, pst, uT_ap, uT_tl):
    kb.dma(ht[:], h_ap, [hb], [ht])
    kb.rms_rstd(ht, junk, ss, rstd, D)
    kb.ts('dve', xn[:], ht[:], rstd[:, 0:1], ALU.mult, [ht, rstd], [xn])
    for c in range(8):
        kb.tr(pst[:, c * 128:(c + 1) * 128], xn[:, c * 128:(c + 1) * 128], ident[:], [xn, ident], [pst])
    kb.tt('dve', uT_ap, pst[:].rearrange("p (c t) -> p c t", c=8),
          g_sb[:].unsqueeze(2).broadcast_to([128, 8, 128]), ALU.mult, [pst, g_sb], [uT_tl])


def phase_a(kb, dram, l, hsrc, ident):
    P = kb.P
    even = (l % 2 == 0)
    nfm = 16
    nsw = 15 if even else 16
    ntm = 348 if even else 1024
    with ExitStack() as pes:
        wqk = kb.sb(pes, [128, 8, nfm * 128], BF16, 'wqk')
        wqks = kb.sb(pes, [128, 8, nsw * 128], BF16, 'wqks')
        wv = kb.sb(pes, [128, 8, ntm], BF16, 'wv')
        g_sb = kb.sb(pes, [128, 8], F32, 'g')
        kb.dma(g_sb[:], dram['mix_g'][l], [], [g_sb])
        for c in range(8):
            kb.dma(wqk[:, c, :], dram['wqk%d' % l][c * 128:(c + 1) * 128, :], [], [wqk], q='pool')
            kb.dma(wqks[:, c, :], dram['wqks%d' % l][c * 128:(c + 1) * 128, :], [], [wqks], q='pool')
            kb.dma(wv[:, c, :], dram['wv%d' % l][c * 128:(c + 1) * 128, :], [], [wv], q='pool')
        ht = [kb.sb(pes, [128, D], F32, 'ht') for _ in range(2)]
        junk = kb.sb(pes, [128, D], BF16, 'junk')
        ss = [kb.sb(pes, [128, 1], F32, 'ss') for _ in range(2)]
        rstd = [kb.sb(pes, [128, 1], F32, 'rstd') for _ in range(2)]
        xn = [kb.sb(pes, [128, D], BF16, 'xn') for _ in range(2)]
        uT = [kb.sb(pes, [128, 8, 512], BF16, 'uT') for _ in range(2)]
        cc = [kb.sb(pes, [128, 512], F32, 'cc') for _ in range(2)]
        sn = [kb.sb(pes, [128, 512], F32, 'sn') for _ in range(2)]
        t1 = [kb.sb(pes, [128, 512], F32, 't1') for _ in range(2)]
        t2 = [kb.sb(pes, [128, 512], F32, 't2') for _ in range(2)]
        stg = [kb.sb(pes, [128, 512], BF16, 'stg') for _ in range(3)]
        vst = [kb.sb(pes, [128, 1024], BF16, 'vst') for _ in range(2)]
        tmf = [kb.sb(pes, [128, 32], F32, 'tmf') for _ in range(2)]
        for t_ in tmf:
            kb.memset('pool', t_[:], 0.0, [t_])
        pst = [kb.ps(pes, [128, 1024], BF16, 'pst') for _ in range(2)]
        pa = [kb.ps(pes, [128, 512], F32, 'pa') for _ in range(2)]
        pb = [kb.ps(pes, [128, 512], F32, 'pb') for _ in range(2)]
        pv = [kb.ps(pes, [128, 512], F32, 'pv') for _ in range(2)]
        n = 0
        for G in range(8):
            u = uT[G % 2]
            kb.dma(cc[G % 2][:], dram['ropeC'][:, G * 512:(G + 1) * 512], [], [cc[G % 2]])
            kb.dma(sn[G % 2][:], dram['ropeS'][:, G * 512:(G + 1) * 512], [], [sn[G % 2]])
            for tt_ in range(4):
                i = G * 4 + tt_
                load_norm_transpose(kb, pes, hsrc[i * 128:(i + 1) * 128, :], kb.db('h', i), g_sb, ident,
                                    ht[i % 2], junk, ss[i % 2], rstd[i % 2], xn[i % 2], pst[i % 2],
                                    u[:, :, tt_ * 128:(tt_ + 1) * 128], u)
                us = u[:, :, tt_ * 128:(tt_ + 1) * 128]
                if not even:
                    for half in range(2):
                        pvt = pv[half]
                        for c in range(8):
                            kb.mm(pvt[:, :], u[:, c, tt_ * 128:(tt_ + 1) * 128], wv[:, c, half * 512:(half + 1) * 512],
                                  c == 0, c == 7, [u, wv], [pvt])
                        kb.cp('act', vst[i % 2][:, half * 512:(half + 1) * 512], pvt[:, :], [pvt], [vst[i % 2]])
                    kb.dma(dram['vs'][i * 128:(i + 1) * 128, :], vst[i % 2][:], [vst[i % 2]], [kb.db('vs', i)])
                else:
                    pvt = pv[i % 2]
                    for c in range(8):
                        kb.mm(pvt[:, 0:ntm], u[:, c, tt_ * 128:(tt_ + 1) * 128], wv[:, c, :], c == 0, c == 7, [u, wv], [pvt])
                    kb.cp('act', vst[i % 2][:, 0:320], pvt[:, 0:320], [pvt], [vst[i % 2]])
                    kb.cp('act', tmf[i % 2][:, 0:4], pvt[:, 320:324], [pvt], [tmf[i % 2]])
                    kb.act(tmf[i % 2][:, 4:28], pvt[:, 324:348], AF.Sigmoid, [pvt], [tmf[i % 2]])
                    kb.dma(dram['vs'][i * 128:(i + 1) * 128, 0:320], vst[i % 2][:, 0:320], [vst[i % 2]], [kb.db('vs', i)])
                    kb.dma(dram['tm32'][i * 128:(i + 1) * 128, :], tmf[i % 2][:, :], [tmf[i % 2]], [kb.db('tm32', i)])
            for rb in range(nfm):
                pat = pa[rb % 2]
                pbt = pb[rb % 2]
                for c in range(8):
                    kb.mm(pat[:, :], wqk[:, c, rb * 128:(rb + 1) * 128], u[:, c, :], c == 0, c == 7, [u, wqk], [pat])
                st = stg[n % 3]
                n += 1
                if rb < nsw:
                    for c in range(8):
                        kb.mm(pbt[:, :], wqks[:, c, rb * 128:(rb + 1) * 128], u[:, c, :], c == 0, c == 7, [u, wqks], [pbt])
                    a1 = t1[rb % 2]
                    a2 = t2[rb % 2]
                    kb.tt('dve', a1[:], pat[:, :], cc[G % 2][:], ALU.mult, [pat, cc[G % 2]], [a1])
                    kb.tt('dve', a2[:], pbt[:, :], sn[G % 2][:], ALU.mult, [pbt, sn[G % 2]], [a2])
                    kb.tt('pool', st[:], a1[:], a2[:], ALU.add, [a1, a2], [st])
                else:
                    kb.cp('act', st[:], pat[:, :], [pat], [st])
                kb.dma(dram['qk'][rb, :, G * 512:(G + 1) * 512], st[:], [st], [kb.db('qk', rb, G)])
        P.barrier()
        P.emit()


def phase_b_odd(kb, dram, l, ident):
    P = kb.P
    lambda_init = 0.8 - 0.6 * math.exp(-0.3 * l)
    with ExitStack() as pes:
        i4 = kb.sb(pes, [128, 512], BF16, 'i4')
        tri = kb.sb(pes, [128, 128], BF16, 'tri')
        zer = kb.sb(pes, [128, 512], BF16, 'zer')
        kb.dma(i4[:], dram['i4'][:, :], [], [i4])
        kb.dma(tri[:], dram['tri'][:, :], [], [tri])
        kb.memset('pool', zer[:], 0.0, [zer])
        lam = kb.sb(pes, [128, 256], F32, 'lam')
        lw = kb.sb(pes, [128, 128], F32, 'lw')
        lv = kb.sb(pes, [128, 4], F32, 'lv')
        subg = kb.sb(pes, [128, 128], F32, 'subg')
        kb.dma(lam[:], dram['lam%d' % l][0:1, :].partition_broadcast(128), [], [lam])
        kb.dma(subg[:], dram['subg%d' % l][0:1, :].partition_broadcast(128), [], [subg])
        kb.tt('dve', lw[:, 0:64], lam[:, 0:64], lam[:, 64:128], ALU.mult, [lam], [lw])
        kb.tt('dve', lw[:, 64:128], lam[:, 128:192], lam[:, 192:256], ALU.mult, [lam, lw], [lw])
        kb.P.op('dve', lambda e: e.reduce_sum(out=lv[:, 0:2], in_=lw[:].rearrange("p (a b) -> p a b", a=2), axis=AX.X), _bufs([lw]), _bufs([lv]))
        kb.act(lv[:, 0:2], lv[:, 0:2], AF.Exp, [lv], [lv])
        kb.tt('dve', lv[:, 2:3], lv[:, 1:2], lv[:, 0:1], ALU.subtract, [lv], [lv])
        kb.ts('dve', lv[:, 2:3], lv[:, 2:3], -lambda_init, ALU.add, [lv], [lv])
        kb.ts('dve', subg[:], subg[:], 1.0 - lambda_init, ALU.mult, [subg], [subg])

        kT = [kb.sb(pes, [128, T], BF16, 'kT') for _ in range(2)]
        vh = [kb.sb(pes, [128, NT, 130], BF16, 'vh') for _ in range(2)]
        for v_ in vh:
            kb.memset('pool', v_[:, :, 128:129], 1.0, [v_])
        qT = [kb.sb(pes, [128, 512], BF16, 'qT') for _ in range(2)]
        pT = [kb.sb(pes, [128, 512], BF16, 'pT') for _ in range(6)]
        oc = [kb.sb(pes, [128, 4, 128], F32, 'oc') for _ in range(2)]
        rd = [kb.sb(pes, [128, 4], F32, 'rd') for _ in range(2)]
        ao = kb.sb(pes, [128, 4, 128], F32, 'ao')
        junk = kb.sb(pes, [128, 128], F32, 'junk')
        s4 = kb.sb(pes, [128, 4], F32, 's4')
        ob = [kb.sb(pes, [128, 4, 128], BF16, 'ob') for _ in range(2)]
        psS = [kb.ps(pes, [128, 512], F32, 'psS') for _ in range(4)]
        pvb = [[kb.ps(pes, [128, 512], F32, 'pvb') for _ in range(2)] for _ in range(2)]
        ns = 0
        nq = 0
        for h in range(8):
            kt = kT[h % 2]
            vt = vh[h % 2]
            kb.dma(kt[:], dram['qk'][8 + h, :, :], [kb.db('qk', 8 + h, G) for G in range(8)], [kt])
            kb.dma(vt[:, :, 0:128], dram['vs'][:, h * 128:(h + 1) * 128].rearrange("(n p) d -> p n d", p=128),
                   [kb.db('vs', i) for i in range(NT)], [vt])
            for Q in range(8):
                qt = qT[nq % 2]
                nq += 1
                kb.dma(qt[:], dram['qk'][h, :, Q * 512:(Q + 1) * 512], [kb.db('qk', h, Q)], [qt])
                for c in range(2):
                    for bnk in pvb[c]:
                        kb.mm(bnk[:, 0:258], zer[:, 0:128], zer[:, 0:258], True, False, [zer], [bnk])
                pend = []
                for j in range(4 * Q + 4):
                    t0 = max(0, j - 4 * Q)
                    cur = []
                    for c in range(2):
                        lo = c * 64
                        hi = lo + 64
                        pS = psS[ns % 4]
                        pt = pT[ns % 6]
                        ns += 1
                        if j >= 4 * Q:
                            kb.mm(pS[:, t0 * 128:(t0 + 1) * 128], tri[:], i4[:, 0:128], True, False, [tri, i4], [pS])
                            kb.mm(pS[:, t0 * 128:512], kt[lo:hi, j * 128:(j + 1) * 128], qt[lo:hi, t0 * 128:512],
                                  False, True, [kt, qt], [pS])
                        else:
                            kb.mm(pS[:, :], kt[lo:hi, j * 128:(j + 1) * 128], qt[lo:hi, :], True, True, [kt, qt], [pS])
                        cur.append((c, pS, pt))
                    for c, pS, pt in cur:
                        kb.act(pt[:, t0 * 128:512], pS[:, t0 * 128:512], AF.Exp, [pS], [pt], scale=0.125)
                    for f in pend:
                        f()
                    pend = []
                    for c, pS, pt in cur:
                        def pvpart(t0=t0, pt=pt, j=j, pv2=pvb[c], vt=vt):
                            for t_ in range(t0, 4):
                                bnk = pv2[t_ // 2]
                                col = (t_ % 2) * 129
                                kb.mm(bnk[:, col:col + 129], pt[:, t_ * 128:(t_ + 1) * 128], vt[:, j, 0:129],
                                      False, False, [pt, vt], [bnk])
                        pend.append(pvpart)
                for f in pend:
                    f()
                pend = []
                for c in range(2):
                    pv2 = pvb[c]
                    for t_ in range(4):
                        bnk = pv2[t_ // 2]
                        col = (t_ % 2) * 129
                        kb.recip(rd[c][:, t_:t_ + 1], bnk[:, col + 128:col + 129], [bnk], [rd[c]])
                    if c == 1:
                        kb.ts('dve', rd[c][:], rd[c][:], lv[:, 2:3], ALU.mult, [rd[c], lv], [rd[c]])
                    for t_ in range(4):
                        bnk = pv2[t_ // 2]
                        col = (t_ % 2) * 129
                        kb.act(oc[c][:, t_, :], bnk[:, col:col + 128], AF.Copy, [bnk, rd[c]], [oc[c]], scale=rd[c][:, t_:t_ + 1])
                kb.tt('dve', ao[:], oc[0][:], oc[1][:], ALU.add, [oc[0], oc[1]], [ao])
                for t_ in range(4):
                    kb.act(junk[:], ao[:, t_, :], AF.Square, [ao], [junk, s4], accum_out=s4[:, t_:t_ + 1])
                kb.ts('dve', s4[:], s4[:], 1.0 / 128, ALU.mult, [s4], [s4], s2=EPS, op1=ALU.add)
                kb.act(s4[:], s4[:], AF.Sqrt, [s4], [s4])
                kb.recip(s4[:], s4[:], [s4], [s4])
                kb.tt('dve', ao[:], ao[:], s4[:].unsqueeze(2).broadcast_to([128, 4, 128]), ALU.mult, [ao, s4], [ao])
                o_ = ob[Q % 2]
                kb.tt('pool', o_[:], ao[:], subg[:].unsqueeze(1).broadcast_to([128, 4, 128]), ALU.mult, [ao, subg], [o_])
                kb.dma(dram['os'][Q * 512:(Q + 1) * 512, h * 128:(h + 1) * 128].rearrange("(n p) d -> p n d", p=128),
                       o_[:], [o_], [kb.db('os', Q, h)])
        P.barrier()
        P.emit()


def phase_b0_even(kb, dram, l):
    P = kb.P
    with ExitStack() as pes:
        w1 = [kb.sb(pes, [128, 32, 256], BF16, 'w1') for _ in range(2)]
        w2k = kb.sb(pes, [128, 2, 128], BF16, 'w2k')
        w2v = kb.sb(pes, [128, 2, 64], BF16, 'w2v')
        peT = kb.sb(pes, [128, 2, 32], F32, 'peT')
        srcs = [kb.sb(pes, [128, T], BF16, 'src') for _ in range(2)]
        xlo = [kb.sb(pes, [128, T], BF16, 'xlo') for _ in range(2)]
        xhi = [kb.sb(pes, [128, T], BF16, 'xhi') for _ in range(2)]
        hT = [kb.sb(pes, [128, 2, 2, 256], BF16, 'hT') for _ in range(2)]
        g0 = [kb.sb(pes, [128, 256], F32, 'g0') for _ in range(2)]
        g1 = [kb.sb(pes, [128, 256], F32, 'g1') for _ in range(2)]
        kcs = kb.sb(pes, [128, 256], BF16, 'kcs')
        vcs = kb.sb(pes, [128, 2, 2, 64], BF16, 'vcs')
        ph = [kb.ps(pes, [128, 512], F32, 'ph') for _ in range(2)]
        pk = [kb.ps(pes, [128, 512], F32, 'pk') for _ in range(2)]
        for kv in range(2):
            wv_ = dram['w1_%d' % l][kv].rearrange("(l d) f -> d l f", d=64)
            kb.dma(w1[kv][0:64, :, :], wv_, [], [w1[kv]], q='pool')
            kb.dma(w1[kv][64:128, :, :], wv_, [], [w1[kv]], q='pool')
            kb.dma(srcs[kv][:], dram['qk'][12 if kv == 0 else 15, :, :], [kb.db('qk', 12 if kv == 0 else 15, G) for G in range(8)], [srcs[kv]])
            kb.memset('pool', hT[kv][:, :, :, 255:256], 0.0, [hT[kv]])
        w2kv = dram['w2_%d' % l][0].rearrange("(c p) d -> p c d", p=128)
        kb.dma(w2k[:, :, 0:64], w2kv, [], [w2k], q='pool')
        kb.dma(w2k[:, :, 64:128], w2kv, [], [w2k], q='pool')
        kb.dma(w2v[:, :, :], dram['w2_%d' % l][1].rearrange("(c p) d -> p c d", p=128), [], [w2v], q='pool')
        kb.dma(peT[:, :, :], dram['peT%d' % l].rearrange("k p l -> p k l"), [], [peT])
        n = 0
        for kv in range(2):
            sv = srcs[kv][:].rearrange("p (n r) -> p n r", r=16)
            kb.tt('dve', xlo[kv][:].rearrange("p (n r) -> p n r", r=16), sv,
                  peT[:, kv, 0:16].unsqueeze(1).broadcast_to([128, 256, 16]), ALU.add, [srcs[kv], peT], [xlo[kv]])
            kb.tt('dve', xhi[kv][:].rearrange("p (n r) -> p n r", r=16), sv,
                  peT[:, kv, 16:32].unsqueeze(1).broadcast_to([128, 256, 16]), ALU.add, [srcs[kv], peT], [xhi[kv]])
            lov = xlo[kv][:].rearrange("p (n r) -> p n r", r=16)
            hiv = xhi[kv][:].rearrange("p (n r) -> p n r", r=16)
            for g in range(2):
                rows = slice(g * 64, g * 64 + 64)
                for fc in range(2):
                    p_ = ph[n % 2]
                    a0 = g0[n % 2]
                    a1 = g1[n % 2]
                    n += 1
                    for l_ in range(32):
                        rhs = lov[rows, 0:255, l_] if l_ < 16 else hiv[rows, 1:256, l_ - 16]
                        kb.mm(p_[:, 0:255], w1[kv][rows, l_, fc * 128:(fc + 1) * 128], rhs, l_ == 0, l_ == 31,
                              [w1[kv], xlo[kv], xhi[kv]], [p_])
                    kb.cp('act', a0[:, 0:255], p_[:, 0:255], [p_], [a0])
                    kb.tt('dve', a1[:, 0:255], a0[:, 0:255], a0[:, 0:255], ALU.mult, [a0], [a1])
                    kb.ts('dve', a1[:, 0:255], a1[:, 0:255], 0.044715, ALU.mult, [a1], [a1], s2=1.0, op1=ALU.add)
                    kb.tt('dve', a1[:, 0:255], a1[:, 0:255], a0[:, 0:255], ALU.mult, [a1, a0], [a1])
                    kb.act(a1[:, 0:255], a1[:, 0:255], AF.Tanh, [a1], [a1], scale=0.7978845608028654)
                    kb.ts('dve', a1[:, 0:255], a1[:, 0:255], 0.5, ALU.mult, [a1], [a1], s2=0.5, op1=ALU.add)
                    kb.tt('dve', hT[kv][:, g, fc, 0:255], a1[:, 0:255], a0[:, 0:255], ALU.mult, [a1, a0], [hT[kv]])
                if kv == 0:
                    p2 = pk[g]
                    for fc in range(2):
                        kb.mm(p2[:, 0:256], w2k[:, fc, :], hT[0][:, g, fc, :], fc == 0, fc == 1, [w2k, hT[0]], [p2])
                    kb.cp('act', kcs[rows, :], p2[rows, 0:256], [p2], [kcs])
                else:
                    for nch in range(2):
                        p2 = pk[nch]
                        for fc in range(2):
                            kb.mm(p2[:, 0:64], hT[1][:, g, fc, nch * 128:(nch + 1) * 128], w2v[:, fc, :], fc == 0, fc == 1,
                                  [w2v, hT[1]], [p2])
                        kb.cp('act', vcs[:, nch, g, :], p2[:, 0:64], [p2], [vcs])
        kb.dma(dram['kcs'][:, :], kcs[:], [kcs], [kb.db('kcs')])
        kb.dma(dram['vcs'][:, :], vcs[:].rearrange("p a g d -> p (a g d)"), [vcs], [kb.db('vcs')])
        P.barrier()
        P.emit()


def phase_b_even(kb, dram, l):
    P = kb.P
    with ExitStack() as pes:
        i4 = kb.sb(pes, [128, 512], BF16, 'i4')
        tri = kb.sb(pes, [128, 128], BF16, 'tri')
        wfar = kb.sb(pes, [128, 128], BF16, 'wfar')
        negm = kb.sb(pes, [128, 128], F32, 'negm')
        zer = kb.sb(pes, [128, 512], BF16, 'zer')
        kb.dma(i4[:], dram['i4'][:, :], [], [i4])
        kb.dma(tri[:], dram['tri'][:, :], [], [tri])
        kb.dma(wfar[:], dram['winfar'][:, :], [], [wfar])
        kb.dma(negm[:], dram['negm'][:, :], [], [negm])
        kb.memset('pool', zer[:], 0.0, [zer])
        kres = {}
        for nm, blk in (('ka', 4), ('ki', 7), ('ks', 13), ('kw', 14)):
            kres[nm] = kb.sb(pes, [128, T], BF16, nm)
            kb.dma(kres[nm][:], dram['qk'][blk, :, :], [kb.db('qk', blk, G) for G in range(8)], [kres[nm]])
        kcT = kb.sb(pes, [128, 256], BF16, 'kcT')
        kb.dma(kcT[:], dram['kcs'][:, :], [kb.db('kcs')], [kcT])
        vc = kb.sb(pes, [128, 2, 2, 130], BF16, 'vc')
        kb.dma(vc[:, :, :, 0:64], dram['vcs'].rearrange("p (a g d) -> p a g d", a=2, g=2), [kb.db('vcs')], [vc])
        kb.memset('pool', vc[:, :, :, 64:65], 1.0, [vc])
        for g in range(2):
            kb.dma(vc[:, :, g, 65:129], dram['ovl'].rearrange("(a p) m -> p a m", p=128), [], [vc])
        vt = kb.sb(pes, [128, NT, 5, 66], BF16, 'vt')
        kb.memset('pool', vt[:, :, :, 64:65], 1.0, [vt])
        for h5 in range(5):
            vsv = dram['vs'][:, h5 * 64:(h5 + 1) * 64].rearrange("(n p) d -> p n d", p=128)
            for q2 in range(2):
                kb.dma(vt[:, q2 * 16:(q2 + 1) * 16, h5, 0:64], vsv[:, q2 * 16:(q2 + 1) * 16, :], [kb.db('vs', i) for i in range(NT)], [vt])

        qs = [kb.sb(pes, [128, 10, 128], BF16, 'qs') for _ in range(2)]
        tmt = [kb.sb(pes, [128, 32], F32, 'tm') for _ in range(2)]
        cmt = [kb.sb(pes, [128, 256], BF16, 'cm') for _ in range(2)]
        fbt = [kb.sb(pes, [128, 64], F32, 'fb') for _ in range(2)]
        sc2 = [kb.sb(pes, [128, T], F32, 'sc') for _ in range(2)]
        rtmp = [kb.sb(pes, [128, 512], F32, 'rt') for _ in range(2)]
        m8 = kb.sb(pes, [128, 8], F32, 'm8')
        mb2 = [kb.sb(pes, [128, T], BF16, 'mb') for _ in range(2)]
        msel = [kb.sb(pes, [128, T], BF16, 'msel') for _ in range(2)]
        pT = [kb.sb(pes, [128, 512], BF16, 'pT') for _ in range(6)]
        rden = kb.sb(pes, [128, 4], F32, 'rden')
        coef = kb.sb(pes, [128, 4], F32, 'coef')
        imp = kb.sb(pes, [128, 64], F32, 'imp')
        scs = kb.sb(pes, [128, 64], F32, 'scs')
        scw = kb.sb(pes, [128, 64], F32, 'scw')
        selb = kb.sb(pes, [128, 64], BF16, 'selb')
        m8b = kb.sb(pes, [128, 8], F32, 'm8b')
        thr = kb.sb(pes, [128, 1], F32, 'thr')
        oacc2 = [kb.sb(pes, [128, 4, 64], F32, 'oacc') for _ in range(2)]
        ot = [kb.sb(pes, [128, D], BF16, 'ot') for _ in range(2)]
        psS = [kb.ps(pes, [128, 512], F32, 'psS') for _ in range(4)]
        pvb = [kb.ps(pes, [128, 512], F32, 'pvb') for _ in range(2)]
        pimp = kb.ps(pes, [128, 512], F32, 'pimp')
        pidx = [kb.ps(pes, [128, 512], F32, 'pidx') for _ in range(1)]
        cnt = {'s': 0, 'v': 0, 'x': 0}

        def attn_block(mask_lhsT, mask_r, kT_ap, k_r, q_ap, q_r, v_aps, pv, extra=None):
            pS = psS[cnt['s'] % 2]
            pt = pT[cnt['s'] % 3]
            cnt['s'] += 1
            if mask_lhsT is not None:
                kb.mm(pS[:, :], mask_lhsT, i4[:], True, False, [i4] + mask_r, [pS])
                kb.mm(pS[:, :], kT_ap, q_ap, False, True, k_r + q_r, [pS])
            else:
                kb.mm(pS[:, :], kT_ap, q_ap, True, True, k_r + q_r, [pS])
            kb.act(pt[:], pS[:, :], AF.Exp, [pS], [pt], scale=0.125)

            def pvpart():
                for hh in range(4):
                    kb.mm(pv[:, hh * 65:hh * 65 + 65], pt[:, hh * 128:(hh + 1) * 128], v_aps[0], False, False, [pt] + v_aps[1], [pv])
                    if extra is not None:
                        kb.mm(extra[0][:, hh * 64:hh * 64 + 64], pt[:, hh * 128:(hh + 1) * 128], extra[1], False, False,
                              [pt] + v_aps[1], [extra[0]])
            prev = pend[0]
            pend[0] = pvpart
            if prev is not None:
                prev()

        pend = [None]

        def flush_pv():
            if pend[0] is not None:
                f = pend[0]
                pend[0] = None
                f()

        def new_pv():
            pv = pvb[cnt['v'] % 2]
            cnt['v'] += 1
            kb.mm(pv[:, 0:260], zer[:, 0:128], zer[:, 0:260], True, False, [zer], [pv])
            return pv

        def get_rden(pv):
            flush_pv()
            dv = pv[:, 0:260].rearrange("p (h c) -> p h c", c=65)[:, :, 64]
            kb.ts('dve', rden[:, 0:4], dv, 1e-30, ALU.max, [pv], [rden])
            kb.recip(rden[:, 0:4], rden[:, 0:4], [rden], [rden])

        pw = kb.sb(pes, [128, 32], F32, 'pw')
        kb.dma(pw[:], dram['pw'][:, :], [], [pw])
        bmx = kb.sb(pes, [128, 1], F32, 'bmx')
        bmn = kb.sb(pes, [128, 1], F32, 'bmn')
        brng = kb.sb(pes, [128, 1], F32, 'brng')
        bthr = kb.sb(pes, [128, 1], F32, 'bthr')
        bcnt = kb.sb(pes, [128, 1], F32, 'bcnt')
        bw = kb.sb(pes, [128, 1], F32, 'bw')
        brk = kb.sb(pes, [128, 32], F32, 'brk')
        bjunk = kb.sb(pes, [128, T], BF16, 'bjunk')
        NSTEP = 17

        def stage1(i):
            N = (i + 1) * 128
            cols = slice(i * 128, (i + 1) * 128)
            rows_t = slice(i * 128, (i + 1) * 128)
            q_ = qs[i % 2]
            tm = tmt[i % 2]
            cm = cmt[i % 2]
            fb = fbt[i % 2]
            sc = sc2[i % 2]
            mb = mb2[i % 2]
            Gq = i // 4
            kb.dma(q_[:, 0:4, :], dram['qk'][0:4, :, cols].rearrange("b p t -> p b t"), [kb.db('qk', b_, Gq) for b_ in range(4)], [q_])
            kb.dma(q_[:, 4:6, :], dram['qk'][5:7, :, cols].rearrange("b p t -> p b t"), [kb.db('qk', b_, Gq) for b_ in (5, 6)], [q_])
            kb.dma(q_[:, 6:10, :], dram['qk'][8:12, :, cols].rearrange("b p t -> p b t"), [kb.db('qk', b_, Gq) for b_ in range(8, 12)], [q_])
            kb.dma(tm[:], dram['tm32'][rows_t, :], [kb.db('tm32', i)], [tm])
            kb.dma(cm[:], dram['cmask'][rows_t, :], [], [cm])
            kb.dma(fb[:], dram['fbias'][rows_t, :], [], [fb])
            ki = kres['ki']
            for ck in range((N + 511) // 512):
                c0 = ck * 512
                c1 = min(N, c0 + 512)
                w_ = c1 - c0
                for hi in range(4):
                    rows = slice((hi // 2) * 64, (hi // 2) * 64 + 64)
                    blk = 4 + (hi % 2)
                    pI = pidx[0]
                    rt = rtmp[cnt['x'] % 2]
                    cnt['x'] += 1
                    kb.mm(pI[:, 0:w_], q_[rows, blk, :], ki[rows, c0:c1], True, True, [q_, ki], [pI])
                    kb.act(rt[:, 0:w_], pI[:, 0:w_], AF.Relu, [pI], [rt])
                    if hi == 0:
                        kb.ts('pool', sc[:, c0:c1], rt[:, 0:w_], tm[:, 0:1], ALU.mult, [rt, tm], [sc], s2=0.0, op1=ALU.add)
                    else:
                        kb.ts('pool', rt[:, 0:w_], rt[:, 0:w_], tm[:, hi:hi + 1], ALU.mult, [rt, tm], [rt], s2=0.0, op1=ALU.add)
                        kb.tt('pool', sc[:, c0:c1], sc[:, c0:c1], rt[:, 0:w_], ALU.add, [rt, sc], [sc])
            kb.tt('pool', sc[:, cols], sc[:, cols], negm[:], ALU.add, [sc, negm], [sc])
            items = []
            if i < 2:
                items.append(lambda: kb.ts('dve', mb[:, 0:N], sc[:, 0:N], -1e29, ALU.is_lt, [sc], [mb], s2=NEG, op1=ALU.mult))
                return items
            def it_max():
                kb.P.op('dve', lambda e: e.tensor_reduce(out=bmx[:, 0:1], in_=sc[:, 0:N], axis=AX.X, op=ALU.max), _bufs([sc]), _bufs([bmx]))
            def it_min():
                kb.P.op('dve', lambda e: e.tensor_reduce(out=bmn[:, 0:1], in_=sc[:, 0:i * 128], axis=AX.X, op=ALU.min), _bufs([sc]), _bufs([bmn]))
                kb.tt('dve', brng[:, 0:1], bmx[:, 0:1], bmn[:, 0:1], ALU.subtract, [bmx, bmn], [brng])
                kb.stt('dve', bthr[:, 0:1], brng[:, 0:1], 0.5, bmn[:, 0:1], ALU.mult, ALU.add, [brng, bmn], [bthr])
                kb.ts('dve', brk[:, :], pw[:, :], brng[:, 0:1], ALU.mult, [pw, brng], [brk])
            items.append(it_max)
            items.append(it_min)
            for k in range(NSTEP):
                def it_step(k=k):
                    kb.ts('dve', bjunk[:, 0:N], sc[:, 0:N], bthr[:, 0:1], ALU.is_ge, [sc, bthr], [bjunk, bcnt], op1=ALU.add, accum_out=bcnt[:, 0:1])
                    kb.ts('dve', bw[:, 0:1], bcnt[:, 0:1], 255.5, ALU.is_gt, [bcnt], [bw], s2=0.5, op1=ALU.subtract)
                    kb.stt('dve', bthr[:, 0:1], bw[:, 0:1], brk[:, k:k + 1], bthr[:, 0:1], ALU.mult, ALU.add, [bw, brk, bthr], [bthr])
                items.append(it_step)
            def it_fin():
                kb.stt('dve', bthr[:, 0:1], brk[:, NSTEP:NSTEP + 1], -1.0, bthr[:, 0:1], ALU.mult, ALU.add, [brk, bthr], [bthr])
                kb.ts('dve', mb[:, 0:N], sc[:, 0:N], bthr[:, 0:1], ALU.is_lt, [sc, bthr], [mb], s2=NEG, op1=ALU.mult)
            items.append(it_fin)
            return items

        def pop(items, n):
            for _ in range(n):
                if items:
                    items.pop(0)()

        def qk_part(mask_lhsT, mask_r, kT_ap, k_r, q_ap, q_r):
            pS = psS[cnt['s'] % 4]
            pt = pT[cnt['s'] % 6]
            cnt['s'] += 1
            if mask_lhsT is not None:
                kb.mm(pS[:, :], mask_lhsT, i4[:], True, False, [i4] + mask_r, [pS])
                kb.mm(pS[:, :], kT_ap, q_ap, False, True, k_r + q_r, [pS])
            else:
                kb.mm(pS[:, :], kT_ap, q_ap, True, True, k_r + q_r, [pS])
            return pS, pt

        def pair_step(specs):
            cur = []
            for sp in specs:
                pS = psS[cnt['s'] % 4]
                pt = pT[cnt['s'] % 6]
                cnt['s'] += 1
                cur.append((pS, pt, sp))
            for pS, pt, sp in cur:
                if sp[0] is not None:
                    kb.mm(pS[:, :], sp[0], i4[:], True, False, [i4] + sp[1], [pS])
            for pS, pt, sp in cur:
                if sp[0] is not None:
                    kb.mm(pS[:, :], sp[2], sp[4], False, True, sp[3] + sp[5], [pS])
                else:
                    kb.mm(pS[:, :], sp[2], sp[4], True, True, sp[3] + sp[5], [pS])
            for pS, pt, sp in cur:
                kb.act(pt[:], pS[:, :], AF.Exp, [pS], [pt], scale=0.125)
            old = pend2[:]
            del pend2[:]
            for f in old:
                f()
            for pS, pt, sp in cur:
                def pvpart(pt=pt, sp=sp):
                    v_ap, v_r, pv, extra = sp[6], sp[7], sp[8], sp[9]
                    for hh in range(4):
                        kb.mm(pv[:, hh * 65:hh * 65 + 65], pt[:, hh * 128:(hh + 1) * 128], v_ap, False, False, [pt] + v_r, [pv])
                        if extra is not None:
                            kb.mm(extra[0][:, extra[2] + hh * 64:extra[2] + hh * 64 + 64], pt[:, hh * 128:(hh + 1) * 128], extra[1],
                                  False, False, [pt] + v_r, [extra[0]])
                pend2.append(pvpart)

        pend2 = []

        def flush2():
            old = pend2[:]
            del pend2[:]
            for f in old:
                f()

        def new_pair():
            for pv in pvb:
                kb.mm(pv[:, 0:260], zer[:, 0:128], zer[:, 0:260], True, False, [zer], [pv])
            return pvb

        def rden_of(pv):
            dv = pv[:, 0:260].rearrange("p (h c) -> p h c", c=65)[:, :, 64]
            kb.ts('dve', rden[:, 0:4], dv, 1e-30, ALU.max, [pv], [rden])
            kb.recip(rden[:, 0:4], rden[:, 0:4], [rden], [rden])

        def stage2(i, nxt):
            N = (i + 1) * 128
            cols = slice(i * 128, (i + 1) * 128)
            rows_t = slice(i * 128, (i + 1) * 128)
            q_ = qs[i % 2]
            tm = tmt[i % 2]
            cm = cmt[i % 2]
            fb = fbt[i % 2]
            mb = mb2[i % 2]
            o_ = ot[i % 2]
            per = (len(nxt) + 3) // 4
            R = [slice(0, 64), slice(64, 128)]
            ka = kres['ka']
            pvp = new_pair()
            for j in range(i + 1):
                pair_step([(mb[:, j * 128:(j + 1) * 128], [mb], ka[R[g], j * 128:(j + 1) * 128], [ka], q_[R[g], 0:4, :], [q_],
                            vt[:, j, 0, 0:65], [vt], pvp[g], None) for g in range(2)])
            flush2()
            for grp in range(2):
                rden_of(pvp[grp])
                for hh in range(4):
                    hd = grp * 4 + hh
                    kb.act(o_[:, hd * 64:(hd + 1) * 64], pvp[grp][:, hh * 65:hh * 65 + 64], AF.Copy, [pvp[grp], rden], [o_], scale=rden[:, hh:hh + 1])
            pop(nxt, per)
            gates = tm[:, 4:28].rearrange("p (g h b) -> p g h b", g=2, h=4)
            nchk = 1 if i < 16 else 2
            qg = [q_[R[g], 6:10, :] for g in range(2)]
            pvp = new_pair()
            kb.mm(pimp[:, 0:512], zer[:, 0:128], zer[:, 0:512], True, False, [zer], [pimp])
            for nch in range(nchk):
                pair_step([(cm[:, nch * 128:(nch + 1) * 128], [cm], kcT[R[g], nch * 128:(nch + 1) * 128], [kcT], qg[g], [q_],
                            vc[:, nch, g, 0:65], [vc], pvp[g], (pimp, vc[:, nch, g, 65:129], g * 256)) for g in range(2)])
            flush2()
            for g in range(2):
                pv = pvp[g]
                rden_of(pv)
                for hh in range(4):
                    pc = g * 256 + hh * 64
                    if hh == 0:
                        kb.ts('dve', imp[:], pimp[:, pc:pc + 64], rden[:, 0:1], ALU.mult, [pimp, rden], [imp])
                    else:
                        kb.stt('dve', imp[:], pimp[:, pc:pc + 64], rden[:, hh:hh + 1], imp[:], ALU.mult, ALU.add, [pimp, rden, imp], [imp])
                kb.tt('dve', coef[:, 0:4], rden[:, 0:4], gates[:, g, :, 0], ALU.mult, [rden, tm], [coef])
                for hh in range(4):
                    kb.ts('dve', oacc2[g][:, hh, :], pv[:, hh * 65:hh * 65 + 64], coef[:, hh:hh + 1], ALU.mult, [pv, coef], [oacc2[g]])
                kb.tt('dve', scs[:], imp[:], fb[:], ALU.add, [imp, fb], [scs])
                kb.P.op('dve', lambda e, a=m8b[:], b=scs[:]: e.max(out=a, in_=b), _bufs([scs]), _bufs([m8b]))
                kb.P.op('dve', lambda e, a=scw[:], b=m8b[:], c=scs[:]: e.match_replace(out=a, in_to_replace=b, in_values=c, imm_value=-3e9),
                        _bufs([scs, m8b]), _bufs([scw]))
                kb.P.op('dve', lambda e, a=m8b[:], b=scw[:]: e.max(out=a, in_=b), _bufs([scw]), _bufs([m8b]))
                kb.ts('dve', thr[:, 0:1], m8b[:, 7:8], -1e8, ALU.max, [m8b], [thr])
                kb.ts('dve', selb[:], scs[:], thr[:, 0:1], ALU.is_lt, [scs, thr], [selb], s2=NEG, op1=ALU.mult)
                ms = msel[g]
                nb = N // 64
                kb.cp('pool', ms[:, 0:N].rearrange("p (m s) -> p m s", s=64), selb[:, 0:nb].unsqueeze(2).broadcast_to([128, nb, 64]), [selb], [ms])
                kb.tt('pool', ms[:, cols], ms[:, cols], tri[:], ALU.add, [ms, tri], [ms])
            kw_ = kres['kw']
            pvp = new_pair()
            for j in range(max(0, i - 4), i + 1):
                if j == i:
                    ml, mr = tri[:], [tri]
                elif j == i - 4:
                    ml, mr = wfar[:], [wfar]
                else:
                    ml, mr = None, []
                pair_step([(ml, mr, kw_[R[g], j * 128:(j + 1) * 128], [kw_], qg[g], [q_], vt[:, j, 3 + g, 0:65], [vt], pvp[g], None)
                           for g in range(2)])
            flush2()
            for g in range(2):
                pv = pvp[g]
                rden_of(pv)
                kb.tt('dve', coef[:, 0:4], rden[:, 0:4], gates[:, g, :, 2], ALU.mult, [rden, tm], [coef])
                for hh in range(4):
                    kb.stt('dve', oacc2[g][:, hh, :], pv[:, hh * 65:hh * 65 + 64], coef[:, hh:hh + 1], oacc2[g][:, hh, :], ALU.mult, ALU.add,
                           [pv, coef, oacc2[g]], [oacc2[g]])
            pop(nxt, per)
            ks = kres['ks']
            pvp = new_pair()
            for j in range(i + 1):
                pair_step([(msel[g][:, j * 128:(j + 1) * 128], [msel[g]], ks[R[g], j * 128:(j + 1) * 128], [ks], qg[g], [q_],
                            vt[:, j, 1 + g, 0:65], [vt], pvp[g], None) for g in range(2)])
            flush2()
            for g in range(2):
                pv = pvp[g]
                rden_of(pv)
                kb.tt('dve', coef[:, 0:4], rden[:, 0:4], gates[:, g, :, 1], ALU.mult, [rden, tm], [coef])
                for hh in range(4):
                    kb.stt('dve', oacc2[g][:, hh, :], pv[:, hh * 65:hh * 65 + 64], coef[:, hh:hh + 1], oacc2[g][:, hh, :], ALU.mult, ALU.add,
                           [pv, coef, oacc2[g]], [oacc2[g]])
                kb.cp('pool', o_[:, 512 + g * 256:512 + (g + 1) * 256], oacc2[g][:].rearrange("p h d -> p (h d)"), [oacc2[g]], [o_])
            pop(nxt, 2 * per)
            kb.dma(dram['os'][rows_t, :], o_[:], [o_], [kb.db('os', i // 4, hh_) for hh_ in range(8)])

        items0 = stage1(0)
        pop(items0, len(items0))
        for i in range(NT):
            nxt = stage1(i + 1) if i + 1 < NT else []
            stage2(i, nxt)
            pop(nxt, len(nxt))
        P.barrier()
        P.emit()


def phase_c(kb, dram, l, hsrc, ident, dst, final_norm):
    P = kb.P
    with ExitStack() as pes:
        wu = [kb.sb(pes, [128, 8, 512], BF16, 'wu') for _ in range(8)]
        wd = [kb.sb(pes, [128, 4, D], BF16, 'wd') for _ in range(8)]
        wo = kb.sb(pes, [128, 8, D], BF16, 'wo')
        g_sb = kb.sb(pes, [128, 8], F32, 'g')
        kb.dma(g_sb[:], dram['mlp_g'][l], [], [g_sb])
        for c in range(8):
            kb.dma(wo[:, c, :], dram['w_out%d' % l][c * 128:(c + 1) * 128, :], [], [wo], q='pool')
        wdv = dram['w_down%d' % l].rearrange("(c p) n -> p c n", p=128)
        for fg in range(8):
            kb.dma(wu[fg][:, :, :], dram['w_up%d' % l][:, fg * 512:(fg + 1) * 512].rearrange("(c p) f -> p c f", p=128),
                   [], [wu[fg]], q='pool')
        for fg in range(8):
            kb.dma(wd[fg][:, :, :], wdv[:, 4 * fg:4 * fg + 4, :], [], [wd[fg]], q='pool')
        if final_norm:
            fg = kb.sb(pes, [128, D], F32, 'fg')
            kb.dma(fg[:], dram['final_g'][0:1, :].partition_broadcast(128), [], [fg])
        ht = [kb.sb(pes, [128, D], F32, 'ht') for _ in range(2)]
        ot = [kb.sb(pes, [128, D], BF16, 'ot') for _ in range(2)]
        oT = kb.sb(pes, [128, 8, 128], BF16, 'oT')
        junk = kb.sb(pes, [128, D], BF16, 'junk')
        ss = kb.sb(pes, [128, 1], F32, 'ss')
        rstd = kb.sb(pes, [128, 1], F32, 'rstd')
        xn = kb.sb(pes, [128, D], BF16, 'xn')
        uT = kb.sb(pes, [128, 8, 128], BF16, 'uT')
        rl = [kb.sb(pes, [128, 512], F32, 'rl') for _ in range(2)]
        aT = kb.sb(pes, [128, 32, 128], BF16, 'aT')
        pst = [kb.ps(pes, [128, 1024], BF16, 'pst') for _ in range(2)]
        pu = [kb.ps(pes, [128, 512], F32, 'pu') for _ in range(2)]
        pd = [kb.ps(pes, [128, 512], F32, 'pd') for _ in range(2)]
        ss2 = kb.sb(pes, [128, 1], F32, 'ss2')
        rstd2 = kb.sb(pes, [128, 1], F32, 'rstd2')

        def front_a(i):
            h_ = ht[i % 2]
            o_ = ot[i % 2]
            rows = slice(i * 128, (i + 1) * 128)
            kb.dma(h_[:], hsrc[rows, :], [kb.db('h', i)], [h_])
            kb.dma(o_[:], dram['os'][rows, :], [kb.db('os', i // 4, hh) for hh in range(8)], [o_])
            for c in range(8):
                kb.tr(pst[0][:, c * 128:(c + 1) * 128], o_[:, c * 128:(c + 1) * 128], ident[:], [o_, ident], [pst[0]])
            kb.cp('act', oT[:].rearrange("p c t -> p (c t)"), pst[0][:, :], [pst[0]], [oT])

        def front_b(i):
            h_ = ht[i % 2]
            for half in range(2):
                for c in range(8):
                    kb.mm(pd[half][:, :], oT[:, c, :], wo[:, c, half * 512:(half + 1) * 512], c == 0, c == 7, [oT, wo], [pd[half]])
                kb.tt('dve', h_[:, half * 512:(half + 1) * 512], pd[half][:, :], h_[:, half * 512:(half + 1) * 512],
                      ALU.add, [pd[half], h_], [h_])
            kb.rms_rstd(h_, junk, ss, rstd, D)
            kb.ts('dve', xn[:], h_[:], rstd[:, 0:1], ALU.mult, [h_, rstd], [xn])

        def front_c(i):
            for c in range(8):
                kb.tr(pst[1][:, c * 128:(c + 1) * 128], xn[:, c * 128:(c + 1) * 128], ident[:], [xn, ident], [pst[1]])
            kb.tt('dve', uT[:], pst[1][:].rearrange("p (c t) -> p c t", c=8),
                  g_sb[:].unsqueeze(2).broadcast_to([128, 8, 128]), ALU.mult, [pst[1], g_sb], [uT])

        def up(i, g0, g1):
            for fg4 in range(g0, g1):
                put = pu[fg4 % 2]
                for f_ in range(4):
                    fc = fg4 * 4 + f_
                    for c in range(8):
                        kb.mm(put[:, f_ * 128:(f_ + 1) * 128], wu[fg4][:, c, f_ * 128:(f_ + 1) * 128], uT[:, c, :],
                              c == 0, c == 7, [wu[fg4], uT], [put])
                r_ = rl[fg4 % 2]
                kb.act(r_[:], put[:, :], AF.Relu, [put], [r_])
                kb.tt('pool', aT[:, fg4 * 4:fg4 * 4 + 4, :].rearrange("p c t -> p (c t)"), r_[:], r_[:], ALU.mult, [r_], [aT])

        def down(i):
            h_ = ht[i % 2]
            rows = slice(i * 128, (i + 1) * 128)
            for half in range(2):
                for fc in range(32):
                    kb.mm(pd[half][:, :], aT[:, fc, :], wd[fc // 4][:, fc % 4, half * 512:(half + 1) * 512], fc == 0, fc == 31,
                          [aT, wd[fc // 4]], [pd[half]])
                kb.tt('dve', h_[:, half * 512:(half + 1) * 512], pd[half][:, :], h_[:, half * 512:(half + 1) * 512],
                      ALU.add, [pd[half], h_], [h_])
            if final_norm:
                kb.rms_rstd(h_, junk, ss2, rstd2, D)
                kb.stt('dve', h_[:], h_[:], rstd2[:, 0:1], fg[:], ALU.mult, ALU.mult, [h_, rstd2, fg], [h_])
            kb.dma(dst[rows, :], h_[:], [h_], [kb.db('h', i) if not final_norm else kb.db('y', i)])

        front_a(0)
        front_b(0)
        front_c(0)
        for i in range(NT):
            nx = i + 1 < NT
            if nx:
                front_a(i + 1)
            up(i, 0, 4)
            if nx:
                front_b(i + 1)
            up(i, 4, 8)
            down(i)
            if nx:
                front_c(i + 1)
        P.barrier()
        P.emit()


def _bf(a):
    return np.asarray(a, dtype=np.float32).astype(ml_dtypes.bfloat16)


def host_consts():
    c = {}
    c['ident'] = _bf(np.eye(128))
    c['i4'] = _bf(np.tile(np.eye(128), (1, 4)))
    t = np.arange(128)[:, None]
    s = np.arange(128)[None, :]
    c['tri'] = _bf(np.where(s <= t, 0.0, NEG))
    inv = 1.0 / (10000.0 ** (np.arange(0, 64, 2, dtype=np.float32) / 64))
    ang = np.arange(T, dtype=np.float32)[None, :] * inv[:, None].astype(np.float32)
    cos = np.cos(ang).astype(np.float32)
    sin = np.sin(ang).astype(np.float32)
    c['winfar'] = _bf(np.where(s > t, 0.0, NEG))
    c['negm'] = np.where(s <= t, 0.0, -1e30).astype(np.float32)
    tq = np.arange(T)[:, None]
    nn = np.arange(256)[None, :]
    c['cmask'] = _bf(np.where((16 * nn + 31 <= tq) & (nn < 255), 0.0, NEG))
    mm_ = np.arange(64)[None, :]
    cur = tq // 64
    fbv = np.where(mm_ * 64 <= tq, 0.0, -1e9)
    fbv = np.where(mm_ == cur - 1, 1e9, fbv)
    fbv = np.where(mm_ == cur, 2e9, fbv)
    fbv = np.where(mm_ == 0, 3e9, fbv)
    c['fbias'] = fbv.astype(np.float32)
    n_ = np.arange(256)[:, None]
    c0_ = n_ * 16
    ov = ((c0_ < (mm_ + 1) * 64) & (c0_ + 32 > mm_ * 64) & (n_ < 255))
    c['ovl'] = _bf(ov.astype(np.float32))
    c['pw'] = np.ascontiguousarray(np.tile((2.0 ** -(np.arange(32) + 1.0))[None, :], (128, 1)).astype(np.float32))
    c['ropeC'] = np.ascontiguousarray(np.concatenate([cos, cos, cos, cos], 0))
    c['ropeS'] = np.ascontiguousarray(np.concatenate([-sin, sin, -sin, sin], 0))
    return c


def swap_cols(w):
    d, n = w.shape
    return np.ascontiguousarray(w.reshape(d, n // 64, 2, 32)[:, :, ::-1, :].reshape(d, n))


def even_cols():
    qa0, ka0, va0, qi0, ki0, wi0, qb0, kv0, gb0 = 0, 512, 576, 640, 896, 960, 964, 1476, 2244

    def hd(base, h):
        return list(range(base + h * 64, base + (h + 1) * 64))

    def kvh(s6, g):
        return hd(kv0, s6 * 2 + g)
    blocks = []
    for r in range(4):
        blocks.append(hd(qa0, r) + hd(qa0, 4 + r))
    blocks.append(hd(ka0, 0) * 2)
    for r in range(2):
        blocks.append(hd(qi0, r) + hd(qi0, 2 + r))
    blocks.append(hd(ki0, 0) * 2)
    for r in range(4):
        blocks.append(hd(qb0, r) + hd(qb0, 4 + r))
    blocks.append(kvh(0, 0) + kvh(0, 1))
    blocks.append(kvh(2, 0) + kvh(2, 1))
    blocks.append(kvh(4, 0) + kvh(4, 1))
    blocks.append(kvh(1, 0) + kvh(1, 1))
    fm = [c for b in blocks for c in b]
    tmc = hd(va0, 0) + kvh(3, 0) + kvh(3, 1) + kvh(5, 0) + kvh(5, 1) + list(range(wi0, wi0 + 4)) + list(range(gb0, gb0 + 24))
    assert len(fm) == 2048 and len(tmc) == 348
    return fm, tmc


def gain_layout(g):
    return np.ascontiguousarray(g.reshape(g.shape[0], 8, 128).transpose(0, 2, 1))


def host_inputs(inputs, layers):
    m = dict(host_consts())
    m['mix_g'] = gain_layout(np.asarray(inputs['mix_norm_g'], np.float32))
    m['mlp_g'] = gain_layout(np.asarray(inputs['mlp_norm_g'], np.float32))
    m['final_g'] = np.asarray(inputs['final_norm_g'], np.float32).reshape(1, D)
    for l in layers:
        m['w_up%d' % l] = np.ascontiguousarray(inputs['mlp_w_up'][l])
        m['w_down%d' % l] = np.ascontiguousarray(inputs['mlp_w_down'][l])
        if l % 2 == 0:
            e = l // 2
            w = np.asarray(inputs['even_w_in'][e], np.float32)
            fm, tmc = even_cols()
            wqk = np.ascontiguousarray(w[:, fm])
            m['wqk%d' % l] = wqk
            m['wqks%d' % l] = swap_cols(np.ascontiguousarray(wqk[:, 0:1920]))
            m['wv%d' % l] = np.ascontiguousarray(w[:, tmc])
            m['w1_%d' % l] = np.ascontiguousarray(inputs['even_cmp_w1'][e], dtype=np.float32)
            m['w2_%d' % l] = np.ascontiguousarray(inputs['even_cmp_w2'][e], dtype=np.float32)
            pe = np.asarray(inputs['even_cmp_pe'][e], np.float32)
            peT = pe.transpose(0, 2, 1)
            m['peT%d' % l] = np.ascontiguousarray(np.concatenate([peT, peT], 1))
            m['w_out%d' % l] = np.ascontiguousarray(inputs['even_w_out'][e])
        if l % 2 == 1:
            o = l // 2
            w = np.asarray(inputs['odd_w_in'][o], np.float32)
            wqk = np.ascontiguousarray(w[:, 0:2048])
            m['wqk%d' % l] = wqk
            m['wqks%d' % l] = swap_cols(wqk)
            m['wv%d' % l] = np.ascontiguousarray(w[:, 2048:3072])
            m['lam%d' % l] = np.asarray(inputs['odd_lambda'][o], np.float32).reshape(1, 256)
            m['subg%d' % l] = np.asarray(inputs['odd_subln_g'][o], np.float32).reshape(1, 128)
            m['w_out%d' % l] = np.ascontiguousarray(inputs['odd_w_out'][o])
    return m


def kernel(**inputs):
    layers = [0, 1, 2, 3]
    inputs = {k: np.asarray(v) for k, v in inputs.items()}
    nc = build_program(dict(layers=layers, final_norm=True))
    shared = host_inputs(inputs, layers)
    in_maps = []
    for c in range(N_CORES):
        m = dict(shared)
        m['x'] = np.ascontiguousarray(inputs['x'][c % 4])
        in_maps.append(m)
    res = run_bass_kernel_spmd(nc, in_maps, core_ids=list(range(N_CORES)))
    return np.stack([res.results[b]['y'] for b in range(4)], 0).astype(np.float32)
```
